# Optimizing a Trainium2 kernel written in Bass

```python
import jax, jax.numpy as jnp
from jax import lax
import numpy as np

D_MODEL = 2048
BATCH = 8
SEQ = 2048
DEPTH = 2

N_MIXERS = 2
MEM_LEN = 256
EPS = 1e-6

MIX_WIDTH = D_MODEL
TOK_WIDTH = 3 * D_MODEL // 4
XA_HEADS = 4
XA_HEAD_DIM = D_MODEL // 16
XA_WIDTH = XA_HEADS * XA_HEAD_DIM

POOL_WINDOWS = (2, 4, 8, 16)
N_POOL_GROUPS = 4
POOL_GROUP_WIDTH = TOK_WIDTH // N_POOL_GROUPS

HEAD_DIM = 64
N_Q_HEADS = TOK_WIDTH // HEAD_DIM
GQA_RATIO = 8
N_KV_HEADS = N_Q_HEADS // GQA_RATIO
KV_WIDTH = N_KV_HEADS * HEAD_DIM
ATTN_IN_WIDTH = TOK_WIDTH + 2 * KV_WIDTH + XA_WIDTH
WINDOW = 128
BLOCK = 128
ROPE_THETA = 500000.0
ROT_DIM = HEAD_DIM // 4
NEG_INF = -1e30

N_EXPERTS = 16
CAPACITY_FACTOR = 2
D_EXPERT = D_MODEL // 2

kernel_name = "interleaved_pool_swa_memxattn_ecmoe_encoder"


def rmsnorm(x, g):
    x32 = x.astype(jnp.float32)
    y = x32 * lax.rsqrt(jnp.mean(x32 * x32, axis=-1, keepdims=True) + EPS)
    return (y * g.astype(jnp.float32)).astype(x.dtype)


def partial_rotary(t, positions):
    rot, keep = t[..., :ROT_DIM], t[..., ROT_DIM:]
    inv_freq = ROPE_THETA ** (-jnp.arange(0, ROT_DIM, 2, dtype=jnp.float32) / ROT_DIM)
    ang = positions.astype(jnp.float32)[..., None] * inv_freq
    cos = jnp.cos(ang)[:, :, None, :]
    sin = jnp.sin(ang)[:, :, None, :]
    r = rot.astype(jnp.float32)
    r1, r2 = r[..., : ROT_DIM // 2], r[..., ROT_DIM // 2:]
    out = jnp.concatenate([r1 * cos - r2 * sin, r2 * cos + r1 * sin], axis=-1)
    return jnp.concatenate([out.astype(t.dtype), keep], axis=-1)


def multiscale_pool(u, w_group, scale):
    B, S, _ = u.shape
    u32 = u.astype(jnp.float32)
    cs = jnp.concatenate([jnp.zeros((B, 1, TOK_WIDTH), jnp.float32), jnp.cumsum(u32, axis=1)], axis=1)
    t = np.arange(S)
    outs = []
    for g, w in enumerate(POOL_WINDOWS):
        lo = np.maximum(t - w // 2, 0)
        hi = np.minimum(t + w // 2 - 1, S - 1)
        cnt = jnp.asarray((hi - lo + 1).astype(np.float32))[None, :, None]
        sl = slice(g * POOL_GROUP_WIDTH, (g + 1) * POOL_GROUP_WIDTH)
        csg = cs[:, :, sl]
        mean = (csg[:, hi + 1] - csg[:, lo]) / cnt
        outs.append(mean - u32[:, :, sl])
    pooled = jnp.stack(outs, axis=2).astype(u.dtype)
    mixed = jnp.einsum('bsgc,gcd->bsgd', pooled, w_group)
    return mixed.reshape(B, S, TOK_WIDTH) * scale


def window_gqa_sink(q, k, v, sink):
    B, S = q.shape[0], q.shape[1]
    nb = S // BLOCK
    qb = q.reshape(B, nb, BLOCK, N_KV_HEADS, GQA_RATIO, HEAD_DIM)
    pad = ((0, 0), (BLOCK, BLOCK), (0, 0), (0, 0))
    kp = jnp.pad(k, pad).reshape(B, nb + 2, BLOCK, N_KV_HEADS, HEAD_DIM)
    vp = jnp.pad(v, pad).reshape(B, nb + 2, BLOCK, N_KV_HEADS, HEAD_DIM)
    kb = jnp.concatenate([kp[:, :-2], kp[:, 1:-1], kp[:, 2:]], axis=2)
    vb = jnp.concatenate([vp[:, :-2], vp[:, 1:-1], vp[:, 2:]], axis=2)
    blk = np.arange(nb)[:, None, None] * BLOCK
    qpos = blk + np.arange(BLOCK)[None, :, None]
    kpos = blk - BLOCK + np.arange(3 * BLOCK)[None, None, :]
    valid = jnp.asarray((np.abs(kpos - qpos) <= WINDOW) & (kpos >= 0) & (kpos < S))
    scores = jnp.einsum('bnqhgd,bnkhd->bnhgqk', qb, kb).astype(jnp.float32) * (HEAD_DIM ** -0.5)
    scores = jnp.where(valid[None, :, None, None], scores, NEG_INF)
    s = sink.astype(jnp.float32).reshape(N_KV_HEADS, GQA_RATIO)[None, None, :, :, None, None]
    m = jnp.maximum(jnp.max(scores, axis=-1, keepdims=True), s)
    p = jnp.exp(scores - m)
    probs = p / (jnp.sum(p, axis=-1, keepdims=True) + jnp.exp(s - m))
    out = jnp.einsum('bnhgqk,bnkhd->bnqhgd', probs.astype(v.dtype), vb)
    return out.reshape(B, S, N_Q_HEADS * HEAD_DIM)


def memory_cross_attention(qx, mem_kv):
    B, S, _ = qx.shape
    q = qx.reshape(B, S, XA_HEADS, XA_HEAD_DIM)
    k = mem_kv[..., :XA_WIDTH].reshape(B, -1, XA_HEADS, XA_HEAD_DIM)
    v = mem_kv[..., XA_WIDTH:].reshape(B, -1, XA_HEADS, XA_HEAD_DIM)
    scores = jnp.einsum('bshd,bmhd->bhsm', q, k).astype(jnp.float32) * (XA_HEAD_DIM ** -0.5)
    probs = jax.nn.softmax(scores, axis=-1).astype(v.dtype)
    return jnp.einsum('bhsm,bmhd->bshd', probs, v).reshape(B, S, XA_WIDTH)


def expert_choice_ffn(h, w_router, w_gate, w_up, w_down):
    B, S, _ = h.shape
    cap = CAPACITY_FACTOR * S // N_EXPERTS
    aff = jax.nn.softmax(jnp.einsum('bsd,de->bse', h, w_router).astype(jnp.float32), axis=-1)
    gates, idx = lax.top_k(jnp.swapaxes(aff, 1, 2), cap)
    bidx = jnp.arange(B)[:, None, None]
    xg = h[bidx, idx]
    a = jnp.einsum('becd,edf->becf', xg, w_gate)
    u = jnp.einsum('becd,edf->becf', xg, w_up)
    y = jnp.einsum('becf,efd->becd', jax.nn.silu(a) * u, w_down) * gates[..., None].astype(h.dtype)
    return jnp.zeros_like(h).at[bidx, idx].add(y)


def setup_inputs(seed: int = 0) -> dict:
    key = jax.random.key(seed)
    ks = jax.random.split(key, 24)
    n_pool = (DEPTH + 1) // 2
    n_attn = DEPTH // 2
    f32 = jnp.float32

    def w(k, shape, fan_in):
        return jax.random.normal(k, shape, f32) * (fan_in ** -0.5)

    def gain(k, shape):
        return 1.0 + 0.05 * jax.random.normal(k, shape, f32)

    x = jax.random.normal(ks[0], (BATCH, SEQ, D_MODEL), f32)
    mem = jax.random.normal(ks[1], (BATCH, MEM_LEN, D_MODEL), f32)
    offset = jax.random.randint(ks[2], (BATCH, 1), 0, 4096, dtype=jnp.int32)
    positions = offset + jnp.arange(SEQ, dtype=jnp.int32)[None, :]
    return {
        "x": x,
        "mem": mem,
        "positions": positions,
        "norm_mix_g": gain(ks[3], (DEPTH, D_MODEL)),
        "norm_ffn_g": gain(ks[4], (DEPTH, D_MODEL)),
        "mem_norm_g": gain(ks[5], (D_MODEL,)),
        "final_g": gain(ks[6], (D_MODEL,)),
        "mem_w_kv": w(ks[7], (DEPTH, D_MODEL, 2 * XA_WIDTH), D_MODEL),
        "pool_w_in": w(ks[8], (n_pool, D_MODEL, MIX_WIDTH), D_MODEL),
        "pool_group_w": w(ks[9], (n_pool, N_POOL_GROUPS, POOL_GROUP_WIDTH, POOL_GROUP_WIDTH), POOL_GROUP_WIDTH),
        "pool_scale": gain(ks[10], (n_pool, TOK_WIDTH)),
        "pool_w_out": w(ks[11], (n_pool, MIX_WIDTH, D_MODEL), MIX_WIDTH),
        "attn_w_in": w(ks[12], (n_attn, D_MODEL, ATTN_IN_WIDTH), D_MODEL),
        "attn_sink": 0.5 * jax.random.normal(ks[13], (n_attn, N_Q_HEADS), f32),
        "attn_w_out": w(ks[14], (n_attn, MIX_WIDTH, D_MODEL), MIX_WIDTH),
        "router_w": w(ks[15], (DEPTH, D_MODEL, N_EXPERTS), D_MODEL),
        "exp_w_gate": w(ks[16], (DEPTH, N_EXPERTS, D_MODEL, D_EXPERT), D_MODEL),
        "exp_w_up": w(ks[17], (DEPTH, N_EXPERTS, D_MODEL, D_EXPERT), D_MODEL),
        "exp_w_down": w(ks[18], (DEPTH, N_EXPERTS, D_EXPERT, D_MODEL), D_EXPERT),
    }


def reference(x, mem, positions, norm_mix_g, norm_ffn_g, mem_norm_g, final_g, mem_w_kv,
              pool_w_in, pool_group_w, pool_scale, pool_w_out,
              attn_w_in, attn_sink, attn_w_out,
              router_w, exp_w_gate, exp_w_up, exp_w_down):
    B, S, _ = x.shape
    mem_n = rmsnorm(mem, mem_norm_g)
    for layer in range(DEPTH):
        h = rmsnorm(x, norm_mix_g[layer])
        mem_kv = jnp.einsum('bmd,de->bme', mem_n, mem_w_kv[layer])
        j = layer // N_MIXERS
        if layer % N_MIXERS == 0:
            proj = jnp.einsum('bsd,de->bse', h, pool_w_in[j])
            tok = multiscale_pool(proj[..., :TOK_WIDTH], pool_group_w[j], pool_scale[j])
            mem_out = memory_cross_attention(proj[..., TOK_WIDTH:], mem_kv)
            w_out = pool_w_out[j]
        else:
            proj = jnp.einsum('bsd,de->bse', h, attn_w_in[j])
            o1 = TOK_WIDTH
            o2 = o1 + KV_WIDTH
            o3 = o2 + KV_WIDTH
            q = partial_rotary(proj[..., :o1].reshape(B, S, N_Q_HEADS, HEAD_DIM), positions)
            k = partial_rotary(proj[..., o1:o2].reshape(B, S, N_KV_HEADS, HEAD_DIM), positions)
            v = proj[..., o2:o3].reshape(B, S, N_KV_HEADS, HEAD_DIM)
            tok = window_gqa_sink(q, k, v, attn_sink[j])
            mem_out = memory_cross_attention(proj[..., o3:], mem_kv)
            w_out = attn_w_out[j]
        x = x + jnp.einsum('bse,ed->bsd', jnp.concatenate([tok, mem_out], axis=-1), w_out)
        h = rmsnorm(x, norm_ffn_g[layer])
        x = x + expert_choice_ffn(h, router_w[layer], exp_w_gate[layer], exp_w_up[layer], exp_w_down[layer])
    return rmsnorm(x, final_g)
```

```python
import numpy as np
from contextlib import ExitStack
import ml_dtypes
import concourse.bass as bass
import concourse.mybir as mybir
from concourse.bass_utils import run_bass_kernel_spmd

dt = mybir.dt
F32 = dt.float32
BF = dt.bfloat16
I32 = dt.int32
U32 = dt.uint32
AF = mybir.ActivationFunctionType
ALU = mybir.AluOpType
AX = mybir.AxisListType

D = 2048
S_LEN = 2048
NT = 16
NDC = 16
MEM = 256
NE = 16
CAP = 256
DFF = 1024
EPS = 1e-6
ENGS = ("sp", "act", "pool", "dve", "pe")


class Op:
    __slots__ = ("eng", "fn", "deps", "dma", "needed", "sem", "val", "idx")


class Sched:
    def __init__(self, nc):
        self.nc = nc
        self.ops = {e: [] for e in ENGS}
        self.last_w = {}
        self.readers = {}
        self.dma_cnt = {}
        self.dma_last = {}
        self.n = 0

    def _new(self, eng, fn, dma):
        op = Op()
        op.eng = eng
        op.fn = fn
        op.dma = dma
        op.needed = False
        op.sem = None
        op.val = 0
        op.idx = self.n
        op.deps = []
        self.n += 1
        return op

    def add(self, eng, fn, r=(), w=(), dma=None):
        op = self._new(eng, fn, dma)
        deps = {}
        for x in r:
            lw = self.last_w.get(x)
            if lw is not None:
                deps[lw.idx] = lw
        for x in w:
            lw = self.last_w.get(x)
            if lw is not None:
                deps[lw.idx] = lw
            for rd in self.readers.get(x, ()):
                deps[rd.idx] = rd
        best = {}
        out = []
        for d in deps.values():
            if d.dma is not None:
                out.append(d)
            else:
                if d.eng == "pe" and eng == "pe" and dma is None:
                    continue
                b = best.get(d.eng)
                if b is None or d.idx > b.idx:
                    best[d.eng] = d
        out.extend(best.values())
        op.deps = out
        for d in out:
            d.needed = True
        for x in r:
            if not (isinstance(x, str) and x.startswith("_")):
                self.readers.setdefault(x, []).append(op)
        for x in w:
            self.last_w[x] = op
            self.readers[x] = []
        if dma is not None:
            c = self.dma_cnt.get(dma, 0) + 1
            self.dma_cnt[dma] = c
            op.val = 16 * c
            self.dma_last[dma] = op
        self.ops[eng].append(op)
        return op

    def barrier(self):
        lasts = []
        for e in ENGS:
            for op in reversed(self.ops[e]):
                if op.dma is None and op.fn is not None:
                    lasts.append(op)
                    break
        dl = list(self.dma_last.values())
        for e in ENGS:
            op = self._new(e, None, None)
            op.deps = [x for x in lasts] + dl
            for d in op.deps:
                d.needed = True
            self.ops[e].append(op)
        self.last_w = {}
        self.readers = {}
        self.dma_last = {}

    def emit(self, es, final_dma_keys=()):
        nc = self.nc
        esem = {e: es.enter_context(nc.semaphore("s_" + e)) for e in ENGS}
        dsem = {}
        for i, k in enumerate(self.dma_cnt):
            dsem[k] = es.enter_context(nc.semaphore("d%d" % i))
        for e in ENGS:
            c = 0
            for op in self.ops[e]:
                if op.dma is not None:
                    op.sem = dsem[op.dma]
                else:
                    op.sem = esem[e]
                    if op.needed:
                        c += 1
                        op.val = c
        ops = self.ops
        dma_cnt = self.dma_cnt

        def run(e, eng):
            waited = {}
            for op in ops[e]:
                for d in op.deps:
                    key = id(d.sem)
                    if waited.get(key, 0) < d.val:
                        eng.wait_ge(d.sem, d.val)
                        waited[key] = d.val
                if op.fn is None:
                    continue
                ins = op.fn(eng)
                if op.dma is not None:
                    ins.then_inc(op.sem, 16)
                elif op.needed:
                    ins.then_inc(op.sem, 1)
            if e == "sp":
                for k in final_dma_keys:
                    eng.wait_ge(dsem[k], 16 * dma_cnt[k])

        with nc.Block() as block:
            @block.sync
            def _(eng):
                run("sp", eng)

            @block.scalar
            def _(eng):
                run("act", eng)

            @block.gpsimd
            def _(eng):
                run("pool", eng)

            @block.vector
            def _(eng):
                run("dve", eng)

            @block.tensor
            def _(eng):
                run("pe", eng)


ARENA_BYTES = 206 * 1024


class Arena:
    def __init__(self, ap):
        self.ap = ap
        self.off = 0

    def mark(self):
        return self.off

    def reset(self, m):
        self.off = m

    def alloc(self, shape, dtype, parts=128):
        esz = {F32: 4, BF: 2, I32: 4, U32: 4}[dtype]
        n = 1
        for s in shape:
            n *= s
        nbytes = (n * esz + 31) // 32 * 32
        assert self.off + nbytes <= ARENA_BYTES, ("SBUF overflow", self.off, nbytes)
        v = self.ap[0:parts, self.off:self.off + n * esz].bitcast(dtype)
        self.off += nbytes
        if len(shape) == 2:
            v = v.rearrange("p (a b) -> p a b", a=shape[0])
        elif len(shape) == 3:
            v = v.rearrange("p (a b c) -> p a b c", a=shape[0], b=shape[1])
        return v


def build(stop_after=None, debug=False, slim=False, dump=None):
    nc = bass.Bass("TRN2", target_bir_lowering=False)

    def din(name, shape, dtype=F32):
        return nc.dram_tensor(name, list(shape), dtype, kind="ExternalInput").ap()

    x_d = din("x", [S_LEN, D])
    mem_d = din("mem", [MEM, D])
    pos_d = din("pos", [1, S_LEN], I32)
    gains_d = din("gains", [6, D])
    wkv_d = din("wkv", [2, 4, 128, 4 * 1024])
    win0_d = din("win0", [16, 128, 16 * 128])
    wgrp_d = din("wgrp", [4, 128, 3 * 384])
    pscale_d = din("pscale", [128, 12])
    wout_d = din("wout", [2, 16, 128, 2048])
    win1_d = din("win1", [19, 128, 16 * 128])
    wv1_d = din("wv1", [128, 16 * 192])
    sink_d = din("nsink", [128, 24])
    wr_d = din("wr", [2, 128, 16 * 16])
    nl_, ne_ = slim if slim else (2, NE)
    wg_d = din("wg", [nl_, ne_, 2, 128, 16 * 512])
    wu_d = din("wu", [nl_, ne_, 2, 128, 16 * 512])
    wd_d = din("wd", [nl_, ne_, 4, 128, 8 * 512])
    cidb_d = din("c_identb", [128, 128], BF)
    cidf_d = din("c_identf", [128, 128])
    crot_d = din("c_rot", [128, 128])
    cinvf_d = din("c_invf", [128, 2])
    cmask_d = din("c_mask", [128, 2 * 384], BF)
    cpedge_d = din("c_pedge", [128, 4 * 16])

    y_d = nc.dram_tensor("y", [S_LEN, D], F32, kind="ExternalOutput").ap()
    kind_dbg = "ExternalOutput" if debug else "Internal"
    xres_d = nc.dram_tensor("xres", [S_LEN, D], F32, kind=kind_dbg).ap()
    hrow_d = nc.dram_tensor("hrow", [S_LEN, D], BF, kind="Internal").ap()
    cat_d = nc.dram_tensor("catT", [16, 128, S_LEN], BF, kind=kind_dbg).ap()
    if debug:
        dbg_d = nc.dram_tensor("dbg", [128, 4096], F32, kind="ExternalOutput").ap()

    es = ExitStack()
    arena_t = es.enter_context(nc.sbuf_tensor("arena", [128, ARENA_BYTES], dt.uint8))
    A = Arena(arena_t)
    psb = [es.enter_context(nc.psum_tensor("ps%d" % i, [128, 512], F32)) for i in range(8)]
    S = Sched(nc)
    uid = [0]

    def PS(i):
        return ("ps", i)

    def psf(i, n=512):
        return psb[i][:, 0:n]

    def psbf(i):
        return psb[i][:, :].bitcast(BF)

    def dma(q, out, in_, r, w, key):
        return S.add(q, lambda e: e.dma_start(out=out, in_=in_), r=r, w=w, dma=key)

    def dump_buf(ap, ncols, col_off, res):
        st = A.alloc([ncols], F32)
        S.add("dve", lambda e: e.tensor_copy(out=st, in_=ap), r=res, w=["dumpst"])
        dma("sp", dbg_d[:, col_off:col_off + ncols], st, ["dumpst"], ["dbgd"], "dumpd")

    identb = A.alloc([128], BF)
    identf = A.alloc([128], F32)
    mem_nT = A.alloc([16, MEM], BF)
    gbc = A.alloc([D], F32)
    nsink = A.alloc([24], F32)
    pscale = A.alloc([12], F32)
    idxT = A.alloc([2, 16], U32)
    gT = A.alloc([2, 16], F32)
    stat = A.alloc([64], F32)
    dma("sp", identb, cidb_d, [], ["_identb"], "c0")
    dma("sp", identf, cidf_d, [], ["_identf"], "c1")
    dma("sp", nsink, sink_d, [], ["_nsink"], "c2")
    dma("sp", pscale, pscale_d, [], ["_pscale"], "c3")
    PMARK = A.mark()

    def load_gain(i):
        dma("sp", gbc, gains_d[i:i + 1, :].partition_broadcast(128), [], ["gbc"], "gbc")

    def norm_tile(xt, xt_res, h_out, h_res, junk, sidx):
        ss = stat[:, sidx:sidx + 1]
        rs = stat[:, sidx + 1:sidx + 2]
        S.add("act", lambda e: e.activation(out=junk, in_=xt, func=AF.Square, accum_out=ss),
              r=[xt_res], w=["junk", ("stat", sidx)])
        S.add("act", lambda e: e.activation(out=ss, in_=ss, func=AF.Sqrt, scale=1.0 / D, bias=EPS),
              r=[("stat", sidx)], w=[("stat", sidx)])
        S.add("dve", lambda e: e.reciprocal(out=rs, in_=ss), r=[("stat", sidx)], w=[("stat", sidx + 1)])
        S.add("dve", lambda e: e.scalar_tensor_tensor(out=h_out, in0=xt, scalar=rs, in1=gbc,
                                                      op0=ALU.mult, op1=ALU.mult),
              r=[xt_res, ("stat", sidx + 1), "gbc"], w=[h_res])

    def transpose_tile(h_tm, h_res, dst3, dst_res, pbanks, ncols=128, evac="act"):
        for half in range(2):
            bank = pbanks[half]
            pv = psbf(bank)[:, 0:8 * 128].rearrange("p (a b) -> p a b", a=8)

            def tr(e, half=half, pv=pv):
                last = None
                for k in range(8):
                    dc = half * 8 + k
                    last = e.transpose(out=pv[:, k, 0:ncols], in_=h_tm[0:ncols, dc * 128:(dc + 1) * 128],
                                       identity=identb[0:ncols, 0:ncols])
                return last
            S.add("pe", tr, r=[h_res, "_identb"], w=[PS(bank)])
            dsl = dst3[:, half * 8:(half + 1) * 8, :]
            if evac == "act":
                S.add("act", lambda e, dsl=dsl, pv=pv: e.copy(out=dsl, in_=pv[:, :, 0:ncols]),
                      r=[PS(bank)], w=[(dst_res, half)])
            else:
                S.add("dve", lambda e, dsl=dsl, pv=pv: e.tensor_copy(out=dsl, in_=pv[:, :, 0:ncols]),
                      r=[PS(bank)], w=[(dst_res, half)])

    def stage_mem():
        m0 = A.mark()
        xt = A.alloc([D], F32)
        hm = A.alloc([D], BF)
        junk = A.alloc([D], BF)
        load_gain(4)
        for mt in range(2):
            dma("sp", xt, mem_d[mt * 128:(mt + 1) * 128, :], [], ["xt"], "xt")
            norm_tile(xt, "xt", hm, "hm", junk, 0)
            transpose_tile(hm, "hm", mem_nT[:, :, mt * 128:(mt + 1) * 128], ("memT", mt), (0, 1))
        S.barrier()
        A.reset(m0)

    def stage_norm_hT(src_d, gain_idx, hT):
        m0 = A.mark()
        xts = [A.alloc([D], F32) for _ in range(2)]
        hms = [A.alloc([D], BF) for _ in range(2)]
        junk = A.alloc([D], BF)
        load_gain(gain_idx)
        for t in range(NT):
            b = t % 2
            dma("sp", xts[b], src_d[t * 128:(t + 1) * 128, :], [], [("xt", b)], ("xt", b))
            norm_tile(xts[b], ("xt", b), hms[b], ("hm", b), junk, 2 * b)
            transpose_tile(hms[b], ("hm", b), hT[:, :, t * 128:(t + 1) * 128], ("hT", t),
                           (2 * b, 2 * b + 1), evac=("act" if b == 0 else "dve"))
        S.barrier()
        A.reset(m0)

    def mem_kv(layer, kT, V):
        m0 = A.mark()
        wk = [A.alloc([4, 1024], BF) for _ in range(4)]
        for pc in range(4):
            dma("pool", wk[pc], wkv_d[layer, pc].rearrange("p (a b) -> p a b", a=4), [], [("wkv", pc)], ("wkv", pc))
        wres = [("wkv", pc) for pc in range(4)] + [(("memT", m), h) for m in range(2) for h in range(2)]
        for h in range(4):
            def mmk(e, h=h):
                last = None
                o = psb[4 + h // 2][:, (h % 2) * 256:(h % 2) * 256 + 256]
                for dc in range(16):
                    last = e.matmul(o, wk[dc // 4][:, dc % 4, h * 128:(h + 1) * 128], mem_nT[:, dc, :],
                                    start=(dc == 0), stop=(dc == 15))
                return last
            S.add("pe", mmk, r=wres, w=[PS(4 + h // 2)])
        for mt in range(2):
            def mmv(e, mt=mt):
                last = None
                for dc in range(16):
                    last = e.matmul(psb[6 + mt][:, :], mem_nT[:, dc, mt * 128:(mt + 1) * 128],
                                    wk[dc // 4][:, dc % 4, 512:1024], start=(dc == 0), stop=(dc == 15))
                return last
            S.add("pe", mmv, r=wres, w=[PS(6 + mt)])
        for h in range(4):
            if dump in ("hT2a", "hT2b"):
                break
            S.add("act", lambda e, h=h: e.copy(out=kT[:, h, :], in_=psb[4 + h // 2][:, (h % 2) * 256:(h % 2) * 256 + 256]),
                  r=[PS(4 + h // 2)], w=[("kT", h)])
        for mt in range(2):
            if dump in ("hT2a", "hT2b"):
                break
            S.add("dve", lambda e, mt=mt: e.tensor_copy(out=V[:, mt, :], in_=psb[6 + mt][:, :]),
                  r=[PS(6 + mt)], w=[("V", mt)])
        S.barrier()
        A.reset(m0)

    def proj_chunk(wsrc, hT, wbufs, ci, evac, banks):
        b = ci % len(wbufs)
        wb = wbufs[b]
        dma("pool", wb, wsrc.rearrange("p (a b) -> p a b", a=16), [], [("wb", b)], ("wb", b))
        for ts in range(4):
            bank = banks[ts % len(banks)]

            def mm(e, ts=ts, bank=bank, wb=wb):
                last = None
                for dc in range(16):
                    last = e.matmul(psb[bank][:, :], wb[:, dc, :], hT[:, dc, ts * 512:(ts + 1) * 512],
                                    start=(dc == 0), stop=(dc == 15))
                return last
            S.add("pe", mm, r=[("wb", b)] + [(("hT", t), h) for t in range(ts * 4, ts * 4 + 4) for h in range(2)],
                  w=[PS(bank)])
            evac(ts, bank)

    def xattn_head(h, qT, q_res, kT, V, cst, cst_res, bk_s, bk_t):
        scale = 128.0 ** -0.5
        m0 = A.mark()
        pbuf = [A.alloc([MEM], BF) for _ in range(2)]
        pTb = [A.alloc([2, 128], BF) for _ in range(2)]
        for t in range(NT):
            b = t % 2
            sps = psb[bk_s][:, b * 256:(b + 1) * 256]
            S.add("pe", lambda e, t=t, sps=sps: e.matmul(sps, qT[:, t * 128:(t + 1) * 128], kT[:, h, :], start=True, stop=True),
                  r=[q_res, ("kT", h)], w=[(PS(bk_s), b)])
            mx = stat[:, 8 + b:9 + b]
            sm = stat[:, 10 + b:11 + b]
            S.add("dve", lambda e, sps=sps, mx=mx: e.tensor_reduce(out=mx, in_=sps, axis=AX.X, op=ALU.max),
                  r=[(PS(bk_s), b)], w=[("xs", b)])
            S.add("dve", lambda e, mx=mx: e.tensor_scalar(out=mx, in0=mx, scalar1=-scale, scalar2=None, op0=ALU.mult),
                  r=[("xs", b)], w=[("xs", b)])
            S.add("act", lambda e, sps=sps, mx=mx, sm=sm, b=b: e.activation(out=pbuf[b], in_=sps, func=AF.Exp, bias=mx, scale=scale, accum_out=sm),
                  r=[(PS(bk_s), b), ("xs", b)], w=[("xp", b), ("xsm", b)])
            S.add("dve", lambda e, sm=sm: e.reciprocal(out=sm, in_=sm), r=[("xsm", b)], w=[("xsm", b)])
            S.add("dve", lambda e, sm=sm, b=b: e.tensor_scalar(out=pbuf[b], in0=pbuf[b], scalar1=sm, scalar2=None, op0=ALU.mult),
                  r=[("xp", b), ("xsm", b)], w=[("xp", b)])
            pv = psbf(bk_t)[:, b * 512:b * 512 + 256].rearrange("p (a c) -> p a c", a=2)

            def tr(e, b=b, pv=pv):
                last = None
                for mt in range(2):
                    last = e.transpose(out=pv[:, mt, :], in_=pbuf[b][:, mt * 128:(mt + 1) * 128], identity=identb)
                return last
            S.add("pe", tr, r=[("xp", b), "_identb"], w=[(PS(bk_t), b, 0)])
            S.add("act", lambda e, b=b, pv=pv: e.copy(out=pTb[b], in_=pv), r=[(PS(bk_t), b, 0)], w=[("xpT", b)])
            ops_ = psb[bk_t][:, b * 256 + 128:b * 256 + 256]

            def pvmm(e, b=b, ops_=ops_):
                last = None
                for mt in range(2):
                    last = e.matmul(ops_, V[:, mt, h * 128:(h + 1) * 128], pTb[b][:, mt, :], start=(mt == 0), stop=(mt == 1))
                return last
            S.add("pe", pvmm, r=[("xpT", b), ("V", 0), ("V", 1)], w=[(PS(bk_t), b, 1)])
            S.add("dve", lambda e, t=t, ops_=ops_: e.tensor_copy(out=cst[:, t * 128:(t + 1) * 128], in_=ops_),
                  r=[(PS(bk_t), b, 1)], w=[cst_res])
        A.reset(m0)

    def stage_mixer0():
        m0 = A.mark()
        hT = A.alloc([16, S_LEN], BF)
        kT = A.alloc([4, MEM], BF)
        V = A.alloc([2, 512], BF)
        stage_norm_hT(x_d, 0, hT)
        if dump == "hT":
            rr = [(("hT", t), h) for t in range(NT) for h in range(2)]
            dump_buf(hT[:, 0, :], 2048, 0, rr)
            dump_buf(hT[:, 15, :], 2048, 2048, rr)
            S.barrier()
            return
        mem_kv(0, kT, V)
        if dump in ("hT2", "hT2a", "hT2b"):
            rr = []
            dump_buf(hT[:, 0, :], 2048, 0, rr)
            dump_buf(hT[:, 15, :], 2048, 2048, rr)
            S.barrier()
            return
        L = S_LEN + 16
        U = A.alloc([L], F32)
        B1 = A.alloc([L], F32)
        B2 = A.alloc([L], F32)
        pooled = A.alloc([3, S_LEN], BF)
        wbufs = [A.alloc([16, 128], BF) for _ in range(3)]
        wgrp = A.alloc([4, 3 * 384], BF)
        pedge = A.alloc([4, 16], F32)
        tmp8 = A.alloc([16], F32)
        cst = [A.alloc([S_LEN], BF) for _ in range(2)]
        qx = [A.alloc([S_LEN], BF) for _ in range(2)]
        dma("sp", pedge, cpedge_d.rearrange("p (a b) -> p a b", a=4), [], ["_pedge"], "c4")
        for g in range(4):
            dma("pool", wgrp[:, g, :], wgrp_d[g], [], ["_wgrp"], "wgrp")
        S.add("dve", lambda e: e.memset(U[:, 0:8], 0.0), w=["Upad"])
        S.add("dve", lambda e: e.memset(U[:, 8 + S_LEN:L], 0.0), w=["Upad"])
        S.add("dve", lambda e: e.memset(B1[:, L - 8:L], 0.0), w=["Bpad"])
        S.add("dve", lambda e: e.memset(B2[:, L - 8:L], 0.0), w=["Bpad"])
        cat_n = [0]

        def store_cat(chunk, buf_i):
            dma("sp", cat_d[chunk], cst[buf_i], [("cst", buf_i)], [("catd", chunk)], ("cstst", buf_i))

        for ec in range(16):
            g = ec // 3
            if ec < 12:
                def evac(ts, bank):
                    S.add("act", lambda e, ts=ts, bank=bank: e.copy(out=U[:, 8 + ts * 512:8 + (ts + 1) * 512], in_=psb[bank][:, :]),
                          r=[PS(bank)], w=[("U", ts)])
                proj_chunk(win0_d[ec], hT, wbufs, ec, evac, (0, 1, 2, 3))
                if dump == "U" and ec == 0:
                    dump_buf(U[:, 8:8 + S_LEN], 2048, 0, [("U", ts) for ts in range(4)])
                    S.barrier()
                    return
                k = g + 1
                half = 1 << g
                w_ = 2 * half
                bufs = [U, B1, B2]
                src = U
                src_r = [("U", ts) for ts in range(4)] + ["Upad"]
                cur = None
                for step in range(k):
                    sh = 1 << step
                    dst = B1 if (step % 2 == 0) else B2
                    dres = "B1" if (step % 2 == 0) else "B2"
                    n = L - 8 if step > 0 else L - 1
                    n = L - sh - (0 if step == 0 else 0)
                    nn = L - sh
                    eng = "dve" if step % 2 == 0 else "pool"
                    S.add(eng, lambda e, dst=dst, src=src, sh=sh, nn=nn: e.tensor_tensor(out=dst[:, 0:nn], in0=src[:, 0:nn], in1=src[:, sh:sh + nn], op=ALU.add),
                          r=src_r + ["Bpad"], w=[dres])
                    src = dst
                    src_r = [dres]
                off = 8 - half
                cc = ec % 3
                S.add("dve", lambda e, src=src, off=off, w_=w_, cc=cc: e.scalar_tensor_tensor(
                    out=pooled[:, cc, :], in0=src[:, off:off + S_LEN], scalar=1.0 / w_, in1=U[:, 8:8 + S_LEN],
                    op0=ALU.mult, op1=ALU.subtract), r=src_r + [("U", ts) for ts in range(4)], w=[("pooled", cc)])
                for (c0, pe0) in ((0, 0), (S_LEN - 8, 8)):
                    S.add("dve", lambda e, src=src, off=off, c0=c0, pe0=pe0, g=g: e.tensor_tensor(
                        out=tmp8[:, 0:8], in0=src[:, off + c0:off + c0 + 8], in1=pedge[:, g, pe0:pe0 + 8], op=ALU.mult),
                        r=src_r + ["_pedge"], w=["tmp8"])
                    S.add("dve", lambda e, c0=c0, cc=cc: e.tensor_tensor(
                        out=pooled[:, cc, c0:c0 + 8], in0=tmp8[:, 0:8], in1=U[:, 8 + c0:16 + c0], op=ALU.subtract),
                        r=["tmp8"] + [("U", ts) for ts in range(4)], w=[("pooled", cc)])
                if cc == 2:
                    for oc in range(3):
                        bi = cat_n[0] % 2
                        cat_n[0] += 1
                        for ts in range(4):
                            bank = 4 + (ts % 2)

                            def mm(e, oc=oc, ts=ts, bank=bank, g=g):
                                last = None
                                for c3 in range(3):
                                    last = e.matmul(psb[bank][:, :], wgrp[:, g, c3 * 384 + oc * 128:c3 * 384 + (oc + 1) * 128],
                                                    pooled[:, c3, ts * 512:(ts + 1) * 512], start=(c3 == 0), stop=(c3 == 2))
                                return last
                            S.add("pe", mm, r=["_wgrp"] + [("pooled", c3) for c3 in range(3)], w=[PS(bank)])
                            col = g * 3 + oc
                            S.add("act", lambda e, bi=bi, ts=ts, bank=bank, col=col: e.mul(
                                out=cst[bi][:, ts * 512:(ts + 1) * 512], in_=psb[bank][:, :], mul=pscale[:, col:col + 1]),
                                r=[PS(bank), "_pscale"], w=[("cst", bi)])
                        store_cat(g * 3 + oc, bi)
            else:
                h = ec - 12
                qb = qx[h % 2]

                def evac(ts, bank, qb=qb, h=h):
                    S.add("act", lambda e, ts=ts, bank=bank: e.copy(out=qb[:, ts * 512:(ts + 1) * 512], in_=psb[bank][:, :]),
                          r=[PS(bank)], w=[("qx", h % 2)])
                proj_chunk(win0_d[ec], hT, wbufs, ec, evac, (0, 1, 2, 3))
                bi = cat_n[0] % 2
                cat_n[0] += 1
                xattn_head(h, qb, ("qx", h % 2), kT, V, cst[bi], ("cst", bi), 6, 7)
                store_cat(12 + h, bi)
        S.barrier()
        A.reset(m0)

    def stage_outproj(layer, src_d):
        m0 = A.mark()
        catT = A.alloc([16, S_LEN], BF)
        wo = A.alloc([16, D], BF)
        xts = [A.alloc([D], F32) for _ in range(2)]
        for ec in range(16):
            dma("sp", catT[:, ec, :], cat_d[ec], [("catd", ec)], [("catT", ec)], ("catl", ec % 4))
            dma("pool", wo[:, ec, :], wout_d[layer, ec], [], [("wo", ec)], ("wol", ec % 4))
        for t in range(NT):
            b = t % 2
            dma("sp", xts[b], src_d[t * 128:(t + 1) * 128, :], [], [("xo", b)], ("xo", b))
            for ds in range(4):
                bank = 4 * b + ds

                def mm(e, t=t, ds=ds, bank=bank):
                    last = None
                    for ec in range(16):
                        last = e.matmul(psb[bank][:, :], catT[:, ec, t * 128:(t + 1) * 128], wo[:, ec, ds * 512:(ds + 1) * 512],
                                        start=(ec == 0), stop=(ec == 15))
                    return last
                S.add("pe", mm, r=[("catT", ec) for ec in range(16)] + [("wo", ec) for ec in range(16)], w=[PS(bank)])
                S.add("dve", lambda e, b=b, ds=ds, bank=bank: e.tensor_tensor(
                    out=xts[b][:, ds * 512:(ds + 1) * 512], in0=psb[bank][:, :], in1=xts[b][:, ds * 512:(ds + 1) * 512], op=ALU.add),
                    r=[PS(bank), ("xo", b)], w=[("xo", b)])
            dma("sp", xres_d[t * 128:(t + 1) * 128, :], xts[b], [("xo", b)], [("xres", t)], ("xost", b))
        S.barrier()
        A.reset(m0)

    def stage_router(layer):
        m0 = A.mark()
        xts = [A.alloc([D], F32) for _ in range(2)]
        hms = [A.alloc([D], BF) for _ in range(2)]
        junk = A.alloc([D], BF)
        hTt = [A.alloc([16, 128], BF) for _ in range(2)]
        wr = A.alloc([16, 16], BF)
        affT = A.alloc([S_LEN], F32)
        wkb = A.alloc([S_LEN], F32)
        vals = A.alloc([CAP], F32)
        idxu = A.alloc([CAP], U32)
        idxf = A.alloc([CAP], F32)
        lg = [A.alloc([16], F32) for _ in range(2)]
        load_gain(1 + 2 * layer)
        dma("pool", wr, wr_d[layer].rearrange("p (a b) -> p a b", a=16), [], ["_wr"], "wr")
        for t in range(NT):
            b = t % 2
            dma("sp", xts[b], xres_d[t * 128:(t + 1) * 128, :], [("xres", t)], [("xt", b)], ("xt", b))
            norm_tile(xts[b], ("xt", b), hms[b], ("hm", b), junk, 2 * b)
            dma("sp", hrow_d[t * 128:(t + 1) * 128, :], hms[b], [("hm", b)], ["hrow"], ("hst", b))
            transpose_tile(hms[b], ("hm", b), hTt[b], ("hTt", b), (2 * b, 2 * b + 1), evac=("act" if b == 0 else "dve"))
            bank = 4 + b

            def mm(e, b=b, bank=bank):
                last = None
                for dc in range(16):
                    last = e.matmul(psb[bank][:, 0:16], hTt[b][:, dc, :], wr[:, dc, :], start=(dc == 0), stop=(dc == 15))
                return last
            S.add("pe", mm, r=[(("hTt", b), 0), (("hTt", b), 1), "_wr"], w=[PS(bank)])
            mx = stat[:, 16 + b:17 + b]
            sm = stat[:, 18 + b:19 + b]
            S.add("dve", lambda e, bank=bank, mx=mx: e.tensor_reduce(out=mx, in_=psb[bank][:, 0:16], axis=AX.X, op=ALU.max),
                  r=[PS(bank)], w=[("rs", b)])
            S.add("dve", lambda e, mx=mx: e.tensor_scalar(out=mx, in0=mx, scalar1=-1.0, scalar2=None, op0=ALU.mult),
                  r=[("rs", b)], w=[("rs", b)])
            S.add("act", lambda e, b=b, bank=bank, mx=mx, sm=sm: e.activation(out=lg[b], in_=psb[bank][:, 0:16], func=AF.Exp, bias=mx, scale=1.0, accum_out=sm),
                  r=[PS(bank), ("rs", b)], w=[("lg", b), ("rsm", b)])
            S.add("dve", lambda e, sm=sm: e.reciprocal(out=sm, in_=sm), r=[("rsm", b)], w=[("rsm", b)])
            S.add("dve", lambda e, b=b, sm=sm: e.tensor_scalar(out=lg[b], in0=lg[b], scalar1=sm, scalar2=None, op0=ALU.mult),
                  r=[("lg", b), ("rsm", b)], w=[("lg", b)])
            tb = 6 + b
            S.add("pe", lambda e, b=b, tb=tb: e.transpose(out=psb[tb][0:16, 0:128], in_=lg[b], identity=identf),
                  r=[("lg", b), "_identf"], w=[PS(tb)])
            S.add("act", lambda e, t=t, tb=tb: e.copy(out=affT[0:16, t * 128:(t + 1) * 128], in_=psb[tb][0:16, 0:128]),
                  r=[PS(tb)], w=["affT"])
        cur = affT
        for it in range(CAP // 8):
            v8 = vals[0:16, it * 8:it * 8 + 8]
            i8 = idxu[0:16, it * 8:it * 8 + 8]
            S.add("dve", lambda e, v8=v8, cur=cur: e.max(out=v8, in_=cur[0:16, :]), r=["affT", "wkb"], w=[("v8", it)])
            S.add("dve", lambda e, v8=v8, i8=i8, cur=cur: e.max_index(out=i8, in_max=v8, in_values=cur[0:16, :]),
                  r=["affT", "wkb", ("v8", it)], w=[("i8", it)])
            if it < CAP // 8 - 1:
                S.add("dve", lambda e, v8=v8, cur=cur: e.match_replace(out=wkb[0:16, :], in_to_replace=v8, in_values=cur[0:16, :], imm_value=-1.0),
                      r=["affT", ("v8", it)], w=["wkb"])
                cur = wkb
        S.add("dve", lambda e: e.tensor_copy(out=idxf[0:16, :], in_=idxu[0:16, :]), r=[("i8", it) for it in range(CAP // 8)], w=["idxf"])
        for ct in range(2):
            S.add("pe", lambda e, ct=ct: e.transpose(out=psb[0][:, ct * 16:(ct + 1) * 16], in_=idxf[0:16, ct * 128:(ct + 1) * 128], identity=identf[0:16, 0:16]),
                  r=["idxf", "_identf"], w=[(PS(0), ct)])
            S.add("pe", lambda e, ct=ct: e.transpose(out=psb[1][:, ct * 16:(ct + 1) * 16], in_=vals[0:16, ct * 128:(ct + 1) * 128], identity=identf[0:16, 0:16]),
                  r=[("v8", it) for it in range(CAP // 8)] + ["_identf"], w=[(PS(1), ct)])
        S.add("dve", lambda e: e.tensor_copy(out=idxT.rearrange("p a b -> p (a b)"), in_=psb[0][:, 0:32]), r=[(PS(0), 0), (PS(0), 1)], w=["idxT"])
        S.add("dve", lambda e: e.tensor_copy(out=gT.rearrange("p a b -> p (a b)"), in_=psb[1][:, 0:32]), r=[(PS(1), 0), (PS(1), 1)], w=["gT"])
        if dump == "rt":
            dump_buf(idxT.rearrange("p a b -> p (a b)"), 32, 0, ["idxT"])
            dump_buf(gT.rearrange("p a b -> p (a b)"), 32, 32, ["gT"])
        S.barrier()
        A.reset(m0)

    def stage_experts(layer):
        m0 = A.mark()
        NBIG, NSM = 4, 5
        big = [A.alloc([16, 512], BF) for _ in range(NBIG)]
        sml = [A.alloc([8, 512], BF) for _ in range(NSM)]
        xg = [[A.alloc([D], BF) for _ in range(2)] for _ in range(2)]
        xgT = [A.alloc([16, CAP], BF) for _ in range(2)]
        act_tm = A.alloc([2, DFF], BF)
        actT = A.alloc([8, CAP], BF)
        sa = [A.alloc([512], F32) for _ in range(2)]
        yo = [A.alloc([D], F32) for _ in range(2)]
        nb = [0]
        ns = [0]
        def gather(e_):
            pb = e_ % 2
            for ct in range(2):
                S.add("pool", lambda e, pb=pb, ct=ct, e_=e_: e.indirect_dma_start(
                    out=xg[pb][ct], out_offset=None, in_=hrow_d,
                    in_offset=bass.IndirectOffsetOnAxis(ap=idxT[:, ct, e_:e_ + 1], axis=0)),
                    r=["hrow", "idxT"], w=[("xg", pb, ct)], dma=("xg", pb, ct))

        def load_w(e_):
            slots = {}
            for fs in range(2):
                for nm, src in (("g", wg_d), ("u", wu_d)):
                    bi = nb[0] % NBIG
                    nb[0] += 1
                    dma("pool", big[bi], src[layer, e_, fs].rearrange("p (a b) -> p a b", a=16), [], [("big", bi)], ("big", bi))
                    slots[(nm, fs)] = bi
            for ds in range(4):
                si = ns[0] % NSM
                ns[0] += 1
                dma("pool", sml[si], wd_d[layer, e_, ds].rearrange("p (a b) -> p a b", a=8), [], [("sml", si)], ("sml", si))
                slots[("d", ds)] = si
            return slots

        gather(0)
        for e_ in range(NE):
            pb = e_ % 2
            slots = load_w(e_)
            if e_ + 1 < NE:
                gather(e_ + 1)
            for ct in range(2):
                transpose_tile(xg[pb][ct], ("xg", pb, ct), xgT[pb][:, :, ct * 128:(ct + 1) * 128], ("xgT", pb, ct),
                               (6, 7), evac=("act" if ct == 0 else "dve"))
            xg_r = [(("xgT", pb, ct), h) for ct in range(2) for h in range(2)]
            for fs in range(2):
                bg = slots[("g", fs)]
                bu = slots[("u", fs)]
                for ct in range(2):
                    pa = ct
                    pu = 2 + ct

                    def mm(e, bw, bank, ct=ct, pb=pb):
                        last = None
                        for dc in range(16):
                            last = e.matmul(psb[bank][:, :], xgT[pb][:, dc, ct * 128:(ct + 1) * 128], big[bw][:, dc, :],
                                            start=(dc == 0), stop=(dc == 15))
                        return last
                    S.add("pe", lambda e, bg=bg, pa=pa, mm=mm: mm(e, bg, pa), r=xg_r + [("big", bg)], w=[PS(pa)])
                    S.add("pe", lambda e, bu=bu, pu=pu, mm=mm: mm(e, bu, pu), r=xg_r + [("big", bu)], w=[PS(pu)])
                    S.add("act", lambda e, ct=ct, pa=pa: e.activation(out=sa[ct], in_=psb[pa][:, :], func=AF.Silu),
                          r=[PS(pa)], w=[("sa", ct)])
                    S.add("dve", lambda e, ct=ct, pu=pu, fs=fs: e.tensor_tensor(out=act_tm[:, ct, fs * 512:(fs + 1) * 512], in0=sa[ct], in1=psb[pu][:, :], op=ALU.mult),
                          r=[("sa", ct), PS(pu)], w=[("act_tm", ct, fs)])
            for ct in range(2):
                pv = psbf(6 + ct)[:, :].rearrange("p (a b) -> p a b", a=8)

                def tr(e, ct=ct, pv=pv):
                    last = None
                    for fc in range(8):
                        last = e.transpose(out=pv[:, fc, :], in_=act_tm[:, ct, fc * 128:(fc + 1) * 128], identity=identb)
                    return last
                S.add("pe", tr, r=[("act_tm", ct, 0), ("act_tm", ct, 1), "_identb"], w=[PS(6 + ct)])
                if ct == 0:
                    S.add("act", lambda e, ct=ct, pv=pv: e.copy(out=actT[:, :, ct * 128:(ct + 1) * 128], in_=pv), r=[PS(6 + ct)], w=[("actT", ct)])
                else:
                    S.add("dve", lambda e, ct=ct, pv=pv: e.tensor_copy(out=actT[:, :, ct * 128:(ct + 1) * 128], in_=pv), r=[PS(6 + ct)], w=[("actT", ct)])
            for ct in range(2):
                for ds in range(4):
                    si = slots[("d", ds)]
                    bank = 4 + (ds % 2)

                    def mmd(e, ct=ct, si=si, bank=bank):
                        last = None
                        for fc in range(8):
                            last = e.matmul(psb[bank][:, :], actT[:, fc, ct * 128:(ct + 1) * 128], sml[si][:, fc, :],
                                            start=(fc == 0), stop=(fc == 7))
                        return last
                    S.add("pe", mmd, r=[("actT", 0), ("actT", 1), ("sml", si)], w=[PS(bank)])
                    if ds % 2 == 0:
                        S.add("act", lambda e, ct=ct, ds=ds, bank=bank, e_=e_: e.mul(
                            out=yo[ct][:, ds * 512:(ds + 1) * 512], in_=psb[bank][:, :], mul=gT[:, ct, e_:e_ + 1]),
                            r=[PS(bank), "gT"], w=[("yo", ct)])
                    else:
                        S.add("dve", lambda e, ct=ct, ds=ds, bank=bank, e_=e_: e.tensor_scalar(
                            out=yo[ct][:, ds * 512:(ds + 1) * 512], in0=psb[bank][:, :], scalar1=gT[:, ct, e_:e_ + 1], scalar2=None, op0=ALU.mult),
                            r=[PS(bank), "gT"], w=[("yo", ct)])
                S.add("pool", lambda e, ct=ct, e_=e_: e.indirect_dma_start(
                    out=xres_d, out_offset=bass.IndirectOffsetOnAxis(ap=idxT[:, ct, e_:e_ + 1], axis=0),
                    in_=yo[ct], in_offset=None, compute_op=ALU.add),
                    r=[("yo", ct), "idxT"], w=["xres_all"], dma=("sc", ct))
        S.barrier()
        A.reset(m0)

    def stage_mixer1():
        m0 = A.mark()
        hT = A.alloc([16, S_LEN], BF)
        kT = A.alloc([4, MEM], BF)
        V = A.alloc([2, 512], BF)
        Ct = A.alloc([S_LEN], F32)
        St = A.alloc([S_LEN], F32)
        invf = A.alloc([2], F32)
        rotT = A.alloc([128], F32)
        stage_norm_hT(xres_d, 2, hT)
        m1 = A.mark()
        posi = A.alloc([S_LEN], I32)
        posf = A.alloc([S_LEN], F32)
        dma("sp", posi, pos_d.partition_broadcast(128), [], ["posi"], "posi")
        dma("sp", invf, cinvf_d, [], ["_invf"], "c5")
        dma("sp", rotT, crot_d, [], ["_rotT"], "c6")
        S.add("dve", lambda e: e.tensor_copy(out=posf, in_=posi), r=["posi"], w=["posf"])
        ki = A.alloc([S_LEN], I32)
        mk = A.alloc([S_LEN], F32)
        SC = 6.283179

        def reduce_(T, res):
            S.add("dve", lambda e: e.tensor_copy(out=ki, in_=T), r=[res], w=["ki"])
            S.add("dve", lambda e: e.tensor_copy(out=mk, in_=ki), r=["ki"], w=["mk"])
            S.add("dve", lambda e: e.tensor_tensor(out=T, in0=T, in1=mk, op=ALU.subtract), r=[res, "mk"], w=[res])
            S.add("dve", lambda e: e.tensor_scalar(out=mk, in0=T, scalar1=0.5, scalar2=None, op0=ALU.is_gt), r=[res], w=["mk"])
            S.add("dve", lambda e: e.tensor_tensor(out=T, in0=T, in1=mk, op=ALU.subtract), r=[res, "mk"], w=[res])
            S.add("dve", lambda e: e.tensor_scalar(out=mk, in0=T, scalar1=-0.5, scalar2=None, op0=ALU.is_lt), r=[res], w=["mk"])
            S.add("dve", lambda e: e.tensor_tensor(out=T, in0=T, in1=mk, op=ALU.add), r=[res, "mk"], w=[res])
            S.add("act", lambda e: e.activation(out=T, in_=T, func=AF.Sin, scale=SC), r=[res], w=[res])
        S.add("dve", lambda e: e.tensor_scalar(out=St, in0=posf, scalar1=invf[:, 0:1], scalar2=None, op0=ALU.mult),
              r=["posf", "_invf"], w=["St"])
        S.add("dve", lambda e: e.tensor_scalar(out=Ct, in0=posf, scalar1=invf[:, 0:1], scalar2=0.25, op0=ALU.mult, op1=ALU.add),
              r=["posf", "_invf"], w=["Ct"])
        reduce_(St, "St")
        reduce_(Ct, "Ct")
        S.add("dve", lambda e: e.tensor_scalar(out=St, in0=St, scalar1=invf[:, 1:2], scalar2=None, op0=ALU.mult), r=["St", "_invf"], w=["St"])
        S.add("dve", lambda e: e.tensor_scalar(out=Ct, in0=Ct, scalar1=-1.0, scalar2=invf[:, 1:2], op0=ALU.add, op1=ALU.mult), r=["Ct", "_invf"], w=["Ct"])
        S.add("dve", lambda e: e.tensor_scalar(out=Ct, in0=Ct, scalar1=1.0, scalar2=None, op0=ALU.add), r=["Ct"], w=["Ct"])
        S.barrier()
        A.reset(m1)
        mem_kv(1, kT, V)
        wbufs = [A.alloc([16, 128], BF) for _ in range(3)]
        wv = A.alloc([16, 192], BF)
        v_tm = A.alloc([NT, 192], BF)
        kT2 = A.alloc([3, S_LEN], BF)
        qf = A.alloc([S_LEN], F32)
        t1 = A.alloc([512], F32)
        qTb = [A.alloc([S_LEN], BF) for _ in range(2)]
        mask = A.alloc([2, 384], BF)
        pb_ = [A.alloc([2, 384], BF) for _ in range(2)]
        pm_ = [A.alloc([2, 384], BF) for _ in range(2)]
        pTs = [A.alloc([6, 128], BF) for _ in range(2)]
        otm = [A.alloc([128], BF) for _ in range(2)]
        cst = [A.alloc([S_LEN], BF) for _ in range(2)]
        qx = [A.alloc([S_LEN], BF) for _ in range(2)]
        dma("sp", mask, cmask_d.rearrange("p (a b) -> p a b", a=2), [], ["_mask"], "c7")
        dma("pool", wv, wv1_d.rearrange("p (a b) -> p a b", a=16), [], ["_wv"], "wv")
        hT_all = [(("hT", t), h) for t in range(NT) for h in range(2)]
        for t in range(NT):
            bank = t % 2

            def mm(e, t=t, bank=bank):
                last = None
                for dc in range(16):
                    last = e.matmul(psb[bank][:, 0:192], hT[:, dc, t * 128:(t + 1) * 128], wv[:, dc, :], start=(dc == 0), stop=(dc == 15))
                return last
            S.add("pe", mm, r=["_wv", (("hT", t), 0), (("hT", t), 1)], w=[PS(bank)])
            S.add("act", lambda e, t=t, bank=bank: e.copy(out=v_tm[:, t, :], in_=psb[bank][:, 0:192]), r=[PS(bank)], w=[("v_tm", t)])

        def rotary_chunk(dst, dst_res):
            for ts in range(4):
                for hh in range(2):
                    c0 = ts * 512 + hh * 256
                    S.add("pe", lambda e, c0=c0: e.matmul(psb[2][:, 0:256], rotT, qf[:, c0:c0 + 256], start=True, stop=True),
                          r=[("qf", ts), "_rotT"], w=[(PS(2), 0)])
                    S.add("dve", lambda e, c0=c0: e.tensor_tensor(out=t1[:, 0:256], in0=psb[2][:, 0:256], in1=St[:, c0:c0 + 256], op=ALU.mult),
                          r=[(PS(2), 0), "St"], w=["t1a"])
                    S.add("pool", lambda e, c0=c0: e.tensor_tensor(out=t1[:, 256:512], in0=qf[:, c0:c0 + 256], in1=Ct[:, c0:c0 + 256], op=ALU.mult),
                          r=[("qf", ts), "Ct"], w=["t1b"])
                    S.add("dve", lambda e, c0=c0: e.tensor_tensor(out=dst[:, c0:c0 + 256], in0=t1[:, 0:256], in1=t1[:, 256:512], op=ALU.add),
                          r=["t1a", "t1b"], w=[dst_res])

        def evac_qf(ts, bank):
            S.add("act", lambda e, ts=ts, bank=bank: e.copy(out=qf[:, ts * 512:(ts + 1) * 512], in_=psb[bank][:, :]),
                  r=[PS(bank)], w=[("qf", ts)])

        ci = [0]
        for g in range(3):
            proj_chunk(win1_d[g], hT, wbufs, ci[0], evac_qf, (0, 1))
            ci[0] += 1
            rotary_chunk(kT2[:, g, :], ("kT2", g))
        cat_n = [0]

        def store_cat(chunk, buf_i):
            dma("sp", cat_d[chunk], cst[buf_i], [("cst", buf_i)], [("catd", chunk)], ("cstst", buf_i))

        inst = [0]
        for j in range(12):
            g = j // 4
            qb = qTb[j % 2]
            qres = ("qTb", j % 2)
            proj_chunk(win1_d[3 + j], hT, wbufs, ci[0], evac_qf, (0, 1))
            ci[0] += 1
            rotary_chunk(qb, qres)
            bi = cat_n[0] % 2
            cat_n[0] += 1
            for n in range(NT):
                ib = inst[0] % 2
                inst[0] += 1
                kb0 = max(n - 1, 0)
                kb1 = min(n + 1, NT - 1)
                nk = (kb1 - kb0 + 1) * 128
                moff = 0 if n > 0 else 128
                sb0 = 3 + 2 * ib
                for hh in range(2):
                    S.add("pe", lambda e, hh=hh, n=n, kb0=kb0, nk=nk, sb0=sb0, qb=qb, g=g: e.matmul(
                        psb[sb0 + hh][:, 0:nk], qb[hh * 64:(hh + 1) * 64, n * 128:(n + 1) * 128],
                        kT2[hh * 64:(hh + 1) * 64, g, kb0 * 128:kb0 * 128 + nk], start=True, stop=True),
                        r=[qres, ("kT2", g)], w=[PS(sb0 + hh)])
                mx = stat[:, 24 + 2 * ib:26 + 2 * ib]
                sm = stat[:, 28 + 2 * ib:30 + 2 * ib]
                es_ = stat[:, 32 + 2 * ib:34 + 2 * ib]
                for hh in range(2):
                    S.add("dve", lambda e, hh=hh, nk=nk, sb0=sb0, mx=mx: e.tensor_reduce(out=mx[:, hh:hh + 1], in_=psb[sb0 + hh][:, 0:nk], axis=AX.X, op=ALU.max),
                          r=[PS(sb0 + hh)], w=[("amx", ib, hh)])
                S.add("dve", lambda e, mx=mx, j=j: e.scalar_tensor_tensor(out=mx, in0=mx, scalar=-0.125, in1=nsink[:, 2 * j:2 * j + 2], op0=ALU.mult, op1=ALU.min),
                      r=[("amx", ib, 0), ("amx", ib, 1), "_nsink"], w=[("anm", ib)])
                for hh in range(2):
                    S.add("act", lambda e, hh=hh, nk=nk, sb0=sb0, mx=mx, ib=ib: e.activation(
                        out=pb_[ib][:, hh, 0:nk], in_=psb[sb0 + hh][:, 0:nk], func=AF.Exp, bias=mx[:, hh:hh + 1], scale=0.125),
                        r=[PS(sb0 + hh), ("anm", ib)], w=[("ap", ib, hh)])
                    S.add("dve", lambda e, hh=hh, nk=nk, ib=ib, moff=moff, sm=sm: e.scalar_tensor_tensor(
                        out=pm_[ib][:, hh, 0:nk], in0=pb_[ib][:, hh, 0:nk], scalar=1.0, in1=mask[:, 0, moff:moff + nk],
                        op0=ALU.mult, op1=ALU.mult, accum_out=sm[:, hh:hh + 1]),
                        r=[("ap", ib, hh), "_mask"], w=[("apm", ib, hh), ("asm", ib, hh)])
                S.add("dve", lambda e, mx=mx, es_=es_, j=j: e.tensor_tensor(out=es_, in0=mx, in1=nsink[:, 2 * j:2 * j + 2], op=ALU.subtract),
                      r=[("anm", ib), "_nsink"], w=[("aes", ib)])
                S.add("act", lambda e, es_=es_: e.activation(out=es_, in_=es_, func=AF.Exp), r=[("aes", ib)], w=[("aes", ib)])
                S.add("dve", lambda e, es_=es_, sm=sm: e.tensor_tensor(out=sm, in0=sm, in1=es_, op=ALU.add),
                      r=[("aes", ib), ("asm", ib, 0), ("asm", ib, 1)], w=[("ard", ib)])
                S.add("dve", lambda e, sm=sm: e.reciprocal(out=sm, in_=sm), r=[("ard", ib)], w=[("ard", ib)])
                nkb = nk // 128
                pv = psbf(7)[:, 0:768].rearrange("p (a b) -> p a b", a=6)

                def tr(e, ib=ib, nkb=nkb, pv=pv):
                    last = None
                    for hh in range(2):
                        for kb in range(nkb):
                            last = e.transpose(out=pv[:, hh * 3 + kb, :], in_=pm_[ib][:, hh, kb * 128:(kb + 1) * 128], identity=identb)
                    return last
                S.add("pe", tr, r=[("apm", ib, 0), ("apm", ib, 1), "_identb"], w=[(PS(7), 0)])
                S.add("act", lambda e, ib=ib, pv=pv: e.copy(out=pTs[ib], in_=pv), r=[(PS(7), 0)], w=[("apT", ib)])
                po = psb[7][:, 384:512]

                def pvm(e, ib=ib, nkb=nkb, kb0=kb0, g=g, po=po):
                    last = None
                    for hh in range(2):
                        for kb in range(nkb):
                            last = e.matmul(po[:, hh * 64:(hh + 1) * 64], pTs[ib][:, hh * 3 + kb, :], v_tm[:, kb0 + kb, g * 64:(g + 1) * 64],
                                            start=(kb == 0), stop=(kb == nkb - 1))
                    return last
                S.add("pe", pvm, r=[("apT", ib)] + [("v_tm", kb0 + kb) for kb in range(nkb)], w=[(PS(7), 1)])
                for hh in range(2):
                    S.add("act", lambda e, hh=hh, ib=ib, po=po, sm=sm: e.mul(
                        out=otm[ib][:, hh * 64:(hh + 1) * 64], in_=po[:, hh * 64:(hh + 1) * 64], mul=sm[:, hh:hh + 1]),
                        r=[(PS(7), 1), ("ard", ib)], w=[("otm", ib)])
                ft = psbf(2)[:, 512:640]
                S.add("pe", lambda e, ib=ib, ft=ft: e.transpose(out=ft, in_=otm[ib], identity=identb), r=[("otm", ib), "_identb"], w=[(PS(2), 1)])
                S.add("dve", lambda e, n=n, bi=bi, ft=ft: e.tensor_copy(out=cst[bi][:, n * 128:(n + 1) * 128], in_=ft), r=[(PS(2), 1)], w=[("cst", bi)])
            store_cat(j, bi)
        for h in range(4):
            qb = qx[h % 2]

            def evac(ts, bank, qb=qb, h=h):
                S.add("act", lambda e, ts=ts, bank=bank: e.copy(out=qb[:, ts * 512:(ts + 1) * 512], in_=psb[bank][:, :]),
                      r=[PS(bank)], w=[("qx", h % 2)])
            proj_chunk(win1_d[15 + h], hT, wbufs, ci[0], evac, (0, 1))
            ci[0] += 1
            bi = cat_n[0] % 2
            cat_n[0] += 1
            xattn_head(h, qb, ("qx", h % 2), kT, V, cst[bi], ("cst", bi), 5, 6)
            store_cat(12 + h, bi)
        S.barrier()
        A.reset(m0)

    def stage_final():
        m0 = A.mark()
        xts = [A.alloc([D], F32) for _ in range(2)]
        outs = [A.alloc([D], F32) for _ in range(2)]
        junk = A.alloc([D], BF)
        load_gain(5)
        for t in range(NT):
            b = t % 2
            dma("sp", xts[b], xres_d[t * 128:(t + 1) * 128, :], [], [("xt", b)], ("xt", b))
            norm_tile(xts[b], ("xt", b), outs[b], ("of", b), junk, 2 * b)
            dma("sp", y_d[t * 128:(t + 1) * 128, :], outs[b], [("of", b)], ["y"], ("yst", b))
        A.reset(m0)

    stages = [
        ("mem", stage_mem),
        ("mix0", stage_mixer0),
        ("out0", lambda: stage_outproj(0, x_d)),
        ("rt0", lambda: stage_router(0)),
        ("ex0", lambda: stage_experts(0)),
        ("mix1", stage_mixer1),
        ("out1", lambda: stage_outproj(1, xres_d)),
        ("rt1", lambda: stage_router(1)),
        ("ex1", lambda: stage_experts(1)),
        ("final", stage_final),
    ]
    for name, fn in stages:
        fn()
        if stop_after == name:
            break
    if stop_after is not None and stop_after != "final":
        S.barrier()
    finals = [k for k in S.dma_cnt if isinstance(k, tuple) and k[0] in ("yst", "xost", "cstst", "sc")]
    S.emit(es, final_dma_keys=finals)
    es.close()
    return nc


def _consts():
    identb = np.eye(128, dtype=np.float32).astype(ml_dtypes.bfloat16)
    identf = np.eye(128, dtype=np.float32)
    rot = np.zeros((128, 128), np.float32)
    for b in (0, 64):
        for r in range(8):
            rot[b + r + 8, b + r] = -1.0
            rot[b + r, b + r + 8] = 1.0
    inv_freq = (500000.0 ** (-np.arange(0, 16, 2, dtype=np.float32) / 16)).astype(np.float32)
    invf = np.zeros((128, 2), np.float32)
    for p in range(128):
        r = p % 64
        if r < 16:
            invf[p, 0] = np.float32(inv_freq[r % 8] / (2.0 * np.pi))
            invf[p, 1] = 1.0
    qq = np.arange(128)[:, None]
    kk = np.arange(128)[None, :]
    m = np.concatenate([(kk >= qq), np.ones((128, 128), bool), (kk <= qq)], axis=1).astype(np.float32)
    mask = np.concatenate([m, m], axis=1).astype(ml_dtypes.bfloat16)
    pedge = np.zeros((4, 16), np.float32)
    t = np.arange(S_LEN)
    for g, w in enumerate((2, 4, 8, 16)):
        lo = np.maximum(t - w // 2, 0)
        hi = np.minimum(t + w // 2 - 1, S_LEN - 1)
        cnt = (hi - lo + 1).astype(np.float32)
        pedge[g, 0:8] = 1.0 / cnt[0:8]
        pedge[g, 8:16] = 1.0 / cnt[S_LEN - 8:]
    pedge = np.broadcast_to(pedge.reshape(1, 64), (128, 64)).copy()
    return identb, identf, rot, invf, mask, pedge


def _pieces(w, pc):
    R, C = w.shape
    n = R // 128
    return np.ascontiguousarray(w.reshape(n, 128, C // pc, pc).transpose(2, 1, 0, 3)).reshape(C // pc, 128, n * pc)


def _host_layout(inp, slim=False):
    f = lambda a: np.ascontiguousarray(np.asarray(a, dtype=np.float32))
    shared = {}
    shared["gains"] = np.stack([f(inp["norm_mix_g"])[0], f(inp["norm_ffn_g"])[0], f(inp["norm_mix_g"])[1],
                                f(inp["norm_ffn_g"])[1], f(inp["mem_norm_g"]), f(inp["final_g"])], axis=0)
    wkv = f(inp["mem_w_kv"])
    shared["wkv"] = np.ascontiguousarray(wkv.reshape(2, 4, 4, 128, 1024).transpose(0, 1, 3, 2, 4)).reshape(2, 4, 128, 4096)
    shared["win0"] = _pieces(f(inp["pool_w_in"])[0], 128)
    gw = f(inp["pool_group_w"])[0]
    shared["wgrp"] = np.ascontiguousarray(gw.reshape(4, 3, 128, 384).transpose(0, 2, 1, 3)).reshape(4, 128, 1152)
    shared["pscale"] = np.ascontiguousarray(f(inp["pool_scale"])[0].reshape(12, 128).T)
    shared["wout"] = np.stack([f(inp["pool_w_out"])[0].reshape(16, 128, 2048), f(inp["attn_w_out"])[0].reshape(16, 128, 2048)], axis=0)
    w1 = f(inp["attn_w_in"])[0]
    kcols = []
    for g in range(3):
        kc = w1[:, 1536 + g * 64:1536 + (g + 1) * 64]
        kcols.append(np.concatenate([kc, kc], axis=1))
    w1c = np.concatenate(kcols + [w1[:, 0:1536], w1[:, 1920:2432]], axis=1)
    shared["win1"] = _pieces(w1c, 128)
    wv = w1[:, 1728:1920]
    shared["wv1"] = np.ascontiguousarray(wv.reshape(16, 128, 192).transpose(1, 0, 2)).reshape(128, 16 * 192)
    shared["nsink"] = np.ascontiguousarray(np.broadcast_to(-f(inp["attn_sink"])[0][None, :], (128, 24)))
    wr = f(inp["router_w"])
    shared["wr"] = np.ascontiguousarray(wr.reshape(2, 16, 128, 16).transpose(0, 2, 1, 3)).reshape(2, 128, 256)
    wg = inp["exp_w_gate"]
    wu = inp["exp_w_up"]
    wd = inp["exp_w_down"]
    if slim:
        wg, wu, wd = wg[:slim[0], :slim[1]], wu[:slim[0], :slim[1]], wd[:slim[0], :slim[1]]
    wg, wu, wd = f(wg), f(wu), f(wd)
    nl_, ne_ = wg.shape[0], wg.shape[1]
    shared["wg"] = np.ascontiguousarray(wg.reshape(nl_, ne_, 16, 128, 2, 512).transpose(0, 1, 4, 3, 2, 5)).reshape(nl_, ne_, 2, 128, 8192)
    shared["wu"] = np.ascontiguousarray(wu.reshape(nl_, ne_, 16, 128, 2, 512).transpose(0, 1, 4, 3, 2, 5)).reshape(nl_, ne_, 2, 128, 8192)
    shared["wd"] = np.ascontiguousarray(wd.reshape(nl_, ne_, 8, 128, 4, 512).transpose(0, 1, 4, 3, 2, 5)).reshape(nl_, ne_, 4, 128, 4096)
    identb, identf, rot, invf, mask, pedge = _consts()
    shared["c_identb"] = identb
    shared["c_identf"] = identf
    shared["c_rot"] = rot
    shared["c_invf"] = invf
    shared["c_mask"] = mask
    shared["c_pedge"] = pedge
    return shared


_NC_CACHE = {}


def kernel(x, mem, positions, norm_mix_g, norm_ffn_g, mem_norm_g, final_g, mem_w_kv,
           pool_w_in, pool_group_w, pool_scale, pool_w_out,
           attn_w_in, attn_sink, attn_w_out,
           router_w, exp_w_gate, exp_w_up, exp_w_down):
    inp = dict(norm_mix_g=norm_mix_g, norm_ffn_g=norm_ffn_g, mem_norm_g=mem_norm_g, final_g=final_g,
               mem_w_kv=mem_w_kv, pool_w_in=pool_w_in, pool_group_w=pool_group_w, pool_scale=pool_scale,
               pool_w_out=pool_w_out, attn_w_in=attn_w_in, attn_sink=attn_sink, attn_w_out=attn_w_out,
               router_w=router_w, exp_w_gate=exp_w_gate, exp_w_up=exp_w_up, exp_w_down=exp_w_down)
    shared = _host_layout(inp)
    x = np.asarray(x, dtype=np.float32)
    mem = np.asarray(mem, dtype=np.float32)
    positions = np.asarray(positions, dtype=np.int32)
    if "nc" not in _NC_CACHE:
        _NC_CACHE["nc"] = build()
    nc = _NC_CACHE["nc"]
    n = 8
    in_maps = []
    for b in range(n):
        m = dict(shared)
        m["x"] = np.ascontiguousarray(x[b])
        m["mem"] = np.ascontiguousarray(mem[b])
        m["pos"] = np.ascontiguousarray(positions[b].reshape(1, S_LEN))
        in_maps.append(m)
    res = run_bass_kernel_spmd(nc, in_maps, core_ids=list(range(n)))
    return np.stack([r["y"] for r in res.results], axis=0)
```

```python
import numpy as np
from contextlib import ExitStack
import ml_dtypes
import concourse.bass as bass
import concourse.mybir as mybir
from concourse.bass_utils import run_bass_kernel_spmd

dt = mybir.dt
F32 = dt.float32
BF = dt.bfloat16
I32 = dt.int32
U32 = dt.uint32
AF = mybir.ActivationFunctionType
ALU = mybir.AluOpType
AX = mybir.AxisListType

D = 2048
S_LEN = 2048
NT = 16
NDC = 16
MEM = 256
NE = 16
CAP = 256
DFF = 1024
EPS = 1e-6
ENGS = ("sp", "act", "pool", "dve", "pe")


class Op:
    __slots__ = ("eng", "fn", "deps", "dma", "needed", "sem", "val", "idx")


class Sched:
    def __init__(self, nc):
        self.nc = nc
        self.ops = {e: [] for e in ENGS}
        self.last_w = {}
        self.readers = {}
        self.dma_cnt = {}
        self.dma_last = {}
        self.n = 0

    def _new(self, eng, fn, dma):
        op = Op()
        op.eng = eng
        op.fn = fn
        op.dma = dma
        op.needed = False
        op.sem = None
        op.val = 0
        op.idx = self.n
        op.deps = []
        self.n += 1
        return op

    def add(self, eng, fn, r=(), w=(), dma=None):
        op = self._new(eng, fn, dma)
        deps = {}
        for x in r:
            lw = self.last_w.get(x)
            if lw is not None:
                deps[lw.idx] = lw
        for x in w:
            lw = self.last_w.get(x)
            if lw is not None:
                deps[lw.idx] = lw
            for rd in self.readers.get(x, ()):
                deps[rd.idx] = rd
        best = {}
        out = []
        for d in deps.values():
            if d.dma is not None:
                out.append(d)
            else:
                if d.eng == "pe" and eng == "pe" and dma is None:
                    continue
                b = best.get(d.eng)
                if b is None or d.idx > b.idx:
                    best[d.eng] = d
        out.extend(best.values())
        op.deps = out
        for d in out:
            d.needed = True
        for x in r:
            if not (isinstance(x, str) and x.startswith("_")):
                self.readers.setdefault(x, []).append(op)
        for x in w:
            self.last_w[x] = op
            self.readers[x] = []
        if dma is not None:
            c = self.dma_cnt.get(dma, 0) + 1
            self.dma_cnt[dma] = c
            op.val = 16 * c
            self.dma_last[dma] = op
        self.ops[eng].append(op)
        return op

    def barrier(self):
        lasts = []
        for e in ENGS:
            for op in reversed(self.ops[e]):
                if op.dma is None and op.fn is not None:
                    lasts.append(op)
                    break
        dl = list(self.dma_last.values())
        for e in ENGS:
            op = self._new(e, None, None)
            op.deps = [x for x in lasts] + dl
            for d in op.deps:
                d.needed = True
            self.ops[e].append(op)
        self.last_w = {}
        self.readers = {}
        self.dma_last = {}

    def emit(self, es, final_dma_keys=()):
        nc = self.nc
        esem = {e: es.enter_context(nc.semaphore("s_" + e)) for e in ENGS}
        dsem = {}
        for i, k in enumerate(self.dma_cnt):
            dsem[k] = es.enter_context(nc.semaphore("d%d" % i))
        for e in ENGS:
            c = 0
            for op in self.ops[e]:
                if op.dma is not None:
                    op.sem = dsem[op.dma]
                else:
                    op.sem = esem[e]
                    if op.needed:
                        c += 1
                        op.val = c
        ops = self.ops
        dma_cnt = self.dma_cnt

        def run(e, eng):
            waited = {}
            for op in ops[e]:
                for d in op.deps:
                    key = id(d.sem)
                    if waited.get(key, 0) < d.val:
                        eng.wait_ge(d.sem, d.val)
                        waited[key] = d.val
                if op.fn is None:
                    continue
                ins = op.fn(eng)
                if op.dma is not None:
                    ins.then_inc(op.sem, 16)
                elif op.needed:
                    ins.then_inc(op.sem, 1)
            if e == "sp":
                for k in final_dma_keys:
                    eng.wait_ge(dsem[k], 16 * dma_cnt[k])

        with nc.Block() as block:
            @block.sync
            def _(eng):
                run("sp", eng)

            @block.scalar
            def _(eng):
                run("act", eng)

            @block.gpsimd
            def _(eng):
                run("pool", eng)

            @block.vector
            def _(eng):
                run("dve", eng)

            @block.tensor
            def _(eng):
                run("pe", eng)


ARENA_BYTES = 206 * 1024


class Arena:
    def __init__(self, ap):
        self.ap = ap
        self.off = 0

    def mark(self):
        return self.off

    def reset(self, m):
        self.off = m

    def alloc(self, shape, dtype, parts=128):
        esz = {F32: 4, BF: 2, I32: 4, U32: 4}[dtype]
        n = 1
        for s in shape:
            n *= s
        nbytes = (n * esz + 31) // 32 * 32
        assert self.off + nbytes <= ARENA_BYTES, ("SBUF overflow", self.off, nbytes)
        v = self.ap[0:parts, self.off:self.off + n * esz].bitcast(dtype)
        self.off += nbytes
        if len(shape) == 2:
            v = v.rearrange("p (a b) -> p a b", a=shape[0])
        elif len(shape) == 3:
            v = v.rearrange("p (a b c) -> p a b c", a=shape[0], b=shape[1])
        return v


def build(stop_after=None, debug=False, slim=False, dump=None):
    nc = bass.Bass("TRN2", target_bir_lowering=False)

    def din(name, shape, dtype=F32):
        return nc.dram_tensor(name, list(shape), dtype, kind="ExternalInput").ap()

    x_d = din("x", [S_LEN, D])
    mem_d = din("mem", [MEM, D])
    pos_d = din("pos", [1, S_LEN], I32)
    gains_d = din("gains", [6, D])
    wkv_d = din("wkv", [2, 4, 128, 4 * 1024])
    win0_d = din("win0", [16, 128, 16 * 128])
    wgrp_d = din("wgrp", [4, 128, 3 * 384])
    pscale_d = din("pscale", [128, 12])
    wout_d = din("wout", [2, 16, 128, 2048])
    win1_d = din("win1", [19, 128, 16 * 128])
    wv1_d = din("wv1", [128, 16 * 192])
    sink_d = din("nsink", [128, 24])
    wr_d = din("wr", [2, 128, 16 * 16])
    nl_, ne_ = slim if slim else (2, NE)
    wg_d = din("wg", [nl_, ne_, 2, 128, 16 * 512])
    wu_d = din("wu", [nl_, ne_, 2, 128, 16 * 512])
    wd_d = din("wd", [nl_, ne_, 4, 128, 8 * 512])
    cidb_d = din("c_identb", [128, 128], BF)
    cidf_d = din("c_identf", [128, 128])
    crot_d = din("c_rot", [128, 128])
    cinvf_d = din("c_invf", [128, 2])
    cmask_d = din("c_mask", [128, 2 * 384], BF)
    cpedge_d = din("c_pedge", [128, 4 * 16])

    y_d = nc.dram_tensor("y", [S_LEN, D], F32, kind="ExternalOutput").ap()
    kind_dbg = "ExternalOutput" if debug else "Internal"
    xres_d = nc.dram_tensor("xres", [S_LEN, D], F32, kind=kind_dbg).ap()
    hrow_d = nc.dram_tensor("hrow", [S_LEN, D], BF, kind="Internal").ap()
    cat_d = nc.dram_tensor("catT", [16, 128, S_LEN], BF, kind=kind_dbg).ap()
    if debug:
        dbg_d = nc.dram_tensor("dbg", [128, 4096], F32, kind="ExternalOutput").ap()

    es = ExitStack()
    arena_t = es.enter_context(nc.sbuf_tensor("arena", [128, ARENA_BYTES], dt.uint8))
    A = Arena(arena_t)
    psb = [es.enter_context(nc.psum_tensor("ps%d" % i, [128, 512], F32)) for i in range(8)]
    S = Sched(nc)
    uid = [0]

    def PS(i):
        return ("ps", i)

    def psf(i, n=512):
        return psb[i][:, 0:n]

    def psbf(i):
        return psb[i][:, :].bitcast(BF)

    def dma(q, out, in_, r, w, key):
        return S.add(q, lambda e: e.dma_start(out=out, in_=in_), r=r, w=w, dma=key)

    def dump_buf(ap, ncols, col_off, res):
        st = A.alloc([ncols], F32)
        S.add("dve", lambda e: e.tensor_copy(out=st, in_=ap), r=res, w=["dumpst"])
        dma("sp", dbg_d[:, col_off:col_off + ncols], st, ["dumpst"], ["dbgd"], "dumpd")

    identb = A.alloc([128], BF)
    identf = A.alloc([128], F32)
    mem_nT = A.alloc([16, MEM], BF)
    gbc = A.alloc([D], F32)
    nsink = A.alloc([24], F32)
    pscale = A.alloc([12], F32)
    idxT = A.alloc([2, 16], U32)
    gT = A.alloc([2, 16], F32)
    stat = A.alloc([64], F32)
    dma("sp", identb, cidb_d, [], ["_identb"], "c0")
    dma("sp", identf, cidf_d, [], ["_identf"], "c1")
    dma("sp", nsink, sink_d, [], ["_nsink"], "c2")
    dma("sp", pscale, pscale_d, [], ["_pscale"], "c3")
    PMARK = A.mark()

    def load_gain(i):
        dma("sp", gbc, gains_d[i:i + 1, :].partition_broadcast(128), [], ["gbc"], "gbc")

    def norm_tile(xt, xt_res, h_out, h_res, junk, sidx):
        ss = stat[:, sidx:sidx + 1]
        rs = stat[:, sidx + 1:sidx + 2]
        S.add("act", lambda e: e.activation(out=junk, in_=xt, func=AF.Square, accum_out=ss),
              r=[xt_res], w=["junk", ("stat", sidx)])
        S.add("act", lambda e: e.activation(out=ss, in_=ss, func=AF.Sqrt, scale=1.0 / D, bias=EPS),
              r=[("stat", sidx)], w=[("stat", sidx)])
        S.add("dve", lambda e: e.reciprocal(out=rs, in_=ss), r=[("stat", sidx)], w=[("stat", sidx + 1)])
        S.add("dve", lambda e: e.scalar_tensor_tensor(out=h_out, in0=xt, scalar=rs, in1=gbc,
                                                      op0=ALU.mult, op1=ALU.mult),
              r=[xt_res, ("stat", sidx + 1), "gbc"], w=[h_res])

    def transpose_tile(h_tm, h_res, dst3, dst_res, pbanks, ncols=128, evac="act"):
        for half in range(2):
            bank = pbanks[half]
            pv = psbf(bank)[:, 0:8 * 128].rearrange("p (a b) -> p a b", a=8)

            def tr(e, half=half, pv=pv):
                last = None
                for k in range(8):
                    dc = half * 8 + k
                    last = e.transpose(out=pv[:, k, 0:ncols], in_=h_tm[0:ncols, dc * 128:(dc + 1) * 128],
                                       identity=identb[0:ncols, 0:ncols])
                return last
            S.add("pe", tr, r=[h_res, "_identb"], w=[PS(bank)])
            dsl = dst3[:, half * 8:(half + 1) * 8, :]
            if evac == "act":
                S.add("act", lambda e, dsl=dsl, pv=pv: e.copy(out=dsl, in_=pv[:, :, 0:ncols]),
                      r=[PS(bank)], w=[(dst_res, half)])
            else:
                S.add("dve", lambda e, dsl=dsl, pv=pv: e.tensor_copy(out=dsl, in_=pv[:, :, 0:ncols]),
                      r=[PS(bank)], w=[(dst_res, half)])

    def stage_mem():
        m0 = A.mark()
        xt = A.alloc([D], F32)
        hm = A.alloc([D], BF)
        junk = A.alloc([D], BF)
        load_gain(4)
        for mt in range(2):
            dma("sp", xt, mem_d[mt * 128:(mt + 1) * 128, :], [], ["xt"], "xt")
            norm_tile(xt, "xt", hm, "hm", junk, 0)
            transpose_tile(hm, "hm", mem_nT[:, :, mt * 128:(mt + 1) * 128], ("memT", mt), (0, 1))
        S.barrier()
        A.reset(m0)

    def stage_norm_hT(src_d, gain_idx, hT):
        m0 = A.mark()
        xts = [A.alloc([D], F32) for _ in range(2)]
        hms = [A.alloc([D], BF) for _ in range(2)]
        junk = A.alloc([D], BF)
        load_gain(gain_idx)
        for t in range(NT):
            b = t % 2
            dma("sp", xts[b], src_d[t * 128:(t + 1) * 128, :], [], [("xt", b)], ("xt", b))
            norm_tile(xts[b], ("xt", b), hms[b], ("hm", b), junk, 2 * b)
            transpose_tile(hms[b], ("hm", b), hT[:, :, t * 128:(t + 1) * 128], ("hT", t),
                           (2 * b, 2 * b + 1), evac=("act" if b == 0 else "dve"))
        S.barrier()
        A.reset(m0)

    def mem_kv(layer, kT, V):
        m0 = A.mark()
        wk = [A.alloc([4, 1024], BF) for _ in range(4)]
        for pc in range(4):
            dma("pool", wk[pc], wkv_d[layer, pc].rearrange("p (a b) -> p a b", a=4), [], [("wkv", pc)], ("wkv", pc))
        wres = [("wkv", pc) for pc in range(4)] + [(("memT", m), h) for m in range(2) for h in range(2)]
        for h in range(4):
            def mmk(e, h=h):
                last = None
                o = psb[4 + h // 2][:, (h % 2) * 256:(h % 2) * 256 + 256]
                for dc in range(16):
                    last = e.matmul(o, wk[dc // 4][:, dc % 4, h * 128:(h + 1) * 128], mem_nT[:, dc, :],
                                    start=(dc == 0), stop=(dc == 15))
                return last
            S.add("pe", mmk, r=wres, w=[PS(4 + h // 2)])
        for mt in range(2):
            def mmv(e, mt=mt):
                last = None
                for dc in range(16):
                    last = e.matmul(psb[6 + mt][:, :], mem_nT[:, dc, mt * 128:(mt + 1) * 128],
                                    wk[dc // 4][:, dc % 4, 512:1024], start=(dc == 0), stop=(dc == 15))
                return last
            S.add("pe", mmv, r=wres, w=[PS(6 + mt)])
        for h in range(4):
            if dump in ("hT2a", "hT2b"):
                break
            S.add("act", lambda e, h=h: e.copy(out=kT[:, h, :], in_=psb[4 + h // 2][:, (h % 2) * 256:(h % 2) * 256 + 256]),
                  r=[PS(4 + h // 2)], w=[("kT", h)])
        for mt in range(2):
            if dump in ("hT2a", "hT2b"):
                break
            S.add("dve", lambda e, mt=mt: e.tensor_copy(out=V[:, mt, :], in_=psb[6 + mt][:, :]),
                  r=[PS(6 + mt)], w=[("V", mt)])
        S.barrier()
        A.reset(m0)

    def proj_chunk(wsrc, hT, wbufs, ci, evac, banks):
        b = ci % len(wbufs)
        wb = wbufs[b]
        dma("pool", wb, wsrc.rearrange("p (a b) -> p a b", a=16), [], [("wb", b)], ("wb", b))
        for ts in range(4):
            bank = banks[ts % len(banks)]

            def mm(e, ts=ts, bank=bank, wb=wb):
                last = None
                for dc in range(16):
                    last = e.matmul(psb[bank][:, :], wb[:, dc, :], hT[:, dc, ts * 512:(ts + 1) * 512],
                                    start=(dc == 0), stop=(dc == 15))
                return last
            S.add("pe", mm, r=[("wb", b)] + [(("hT", t), h) for t in range(ts * 4, ts * 4 + 4) for h in range(2)],
                  w=[PS(bank)])
            evac(ts, bank)

    def xattn_head(h, qT, q_res, kT, V, cst, cst_res, bk_s, bk_t):
        scale = 128.0 ** -0.5
        m0 = A.mark()
        pbuf = [A.alloc([MEM], BF) for _ in range(2)]
        pTb = [A.alloc([2, 128], BF) for _ in range(2)]
        for t in range(NT):
            b = t % 2
            sps = psb[bk_s][:, b * 256:(b + 1) * 256]
            S.add("pe", lambda e, t=t, sps=sps: e.matmul(sps, qT[:, t * 128:(t + 1) * 128], kT[:, h, :], start=True, stop=True),
                  r=[q_res, ("kT", h)], w=[(PS(bk_s), b)])
            mx = stat[:, 8 + b:9 + b]
            sm = stat[:, 10 + b:11 + b]
            S.add("dve", lambda e, sps=sps, mx=mx: e.tensor_reduce(out=mx, in_=sps, axis=AX.X, op=ALU.max),
                  r=[(PS(bk_s), b)], w=[("xs", b)])
            S.add("dve", lambda e, mx=mx: e.tensor_scalar(out=mx, in0=mx, scalar1=-scale, scalar2=None, op0=ALU.mult),
                  r=[("xs", b)], w=[("xs", b)])
            S.add("act", lambda e, sps=sps, mx=mx, sm=sm, b=b: e.activation(out=pbuf[b], in_=sps, func=AF.Exp, bias=mx, scale=scale, accum_out=sm),
                  r=[(PS(bk_s), b), ("xs", b)], w=[("xp", b), ("xsm", b)])
            S.add("dve", lambda e, sm=sm: e.reciprocal(out=sm, in_=sm), r=[("xsm", b)], w=[("xsm", b)])
            S.add("dve", lambda e, sm=sm, b=b: e.tensor_scalar(out=pbuf[b], in0=pbuf[b], scalar1=sm, scalar2=None, op0=ALU.mult),
                  r=[("xp", b), ("xsm", b)], w=[("xp", b)])
            pv = psbf(bk_t)[:, b * 512:b * 512 + 256].rearrange("p (a c) -> p a c", a=2)

            def tr(e, b=b, pv=pv):
                last = None
                for mt in range(2):
                    last = e.transpose(out=pv[:, mt, :], in_=pbuf[b][:, mt * 128:(mt + 1) * 128], identity=identb)
                return last
            S.add("pe", tr, r=[("xp", b), "_identb"], w=[(PS(bk_t), b, 0)])
            S.add("act", lambda e, b=b, pv=pv: e.copy(out=pTb[b], in_=pv), r=[(PS(bk_t), b, 0)], w=[("xpT", b)])
            ops_ = psb[bk_t][:, b * 256 + 128:b * 256 + 256]

            def pvmm(e, b=b, ops_=ops_):
                last = None
                for mt in range(2):
                    last = e.matmul(ops_, V[:, mt, h * 128:(h + 1) * 128], pTb[b][:, mt, :], start=(mt == 0), stop=(mt == 1))
                return last
            S.add("pe", pvmm, r=[("xpT", b), ("V", 0), ("V", 1)], w=[(PS(bk_t), b, 1)])
            S.add("dve", lambda e, t=t, ops_=ops_: e.tensor_copy(out=cst[:, t * 128:(t + 1) * 128], in_=ops_),
                  r=[(PS(bk_t), b, 1)], w=[cst_res])
        A.reset(m0)

    def stage_mixer0():
        m0 = A.mark()
        hT = A.alloc([16, S_LEN], BF)
        kT = A.alloc([4, MEM], BF)
        V = A.alloc([2, 512], BF)
        stage_norm_hT(x_d, 0, hT)
        if dump == "hT":
            rr = [(("hT", t), h) for t in range(NT) for h in range(2)]
            dump_buf(hT[:, 0, :], 2048, 0, rr)
            dump_buf(hT[:, 15, :], 2048, 2048, rr)
            S.barrier()
            return
        mem_kv(0, kT, V)
        if dump in ("hT2", "hT2a", "hT2b"):
            rr = []
            dump_buf(hT[:, 0, :], 2048, 0, rr)
            dump_buf(hT[:, 15, :], 2048, 2048, rr)
            S.barrier()
            return
        L = S_LEN + 16
        U = A.alloc([L], F32)
        B1 = A.alloc([L], F32)
        B2 = A.alloc([L], F32)
        pooled = A.alloc([3, S_LEN], BF)
        wbufs = [A.alloc([16, 128], BF) for _ in range(3)]
        wgrp = A.alloc([4, 3 * 384], BF)
        pedge = A.alloc([4, 16], F32)
        tmp8 = A.alloc([16], F32)
        cst = [A.alloc([S_LEN], BF) for _ in range(2)]
        qx = [A.alloc([S_LEN], BF) for _ in range(2)]
        dma("sp", pedge, cpedge_d.rearrange("p (a b) -> p a b", a=4), [], ["_pedge"], "c4")
        for g in range(4):
            dma("pool", wgrp[:, g, :], wgrp_d[g], [], ["_wgrp"], "wgrp")
        S.add("dve", lambda e: e.memset(U[:, 0:8], 0.0), w=["Upad"])
        S.add("dve", lambda e: e.memset(U[:, 8 + S_LEN:L], 0.0), w=["Upad"])
        S.add("dve", lambda e: e.memset(B1[:, L - 8:L], 0.0), w=["Bpad"])
        S.add("dve", lambda e: e.memset(B2[:, L - 8:L], 0.0), w=["Bpad"])
        cat_n = [0]

        def store_cat(chunk, buf_i):
            dma("sp", cat_d[chunk], cst[buf_i], [("cst", buf_i)], [("catd", chunk)], ("cstst", buf_i))

        for ec in range(16):
            g = ec // 3
            if ec < 12:
                def evac(ts, bank):
                    S.add("act", lambda e, ts=ts, bank=bank: e.copy(out=U[:, 8 + ts * 512:8 + (ts + 1) * 512], in_=psb[bank][:, :]),
                          r=[PS(bank)], w=[("U", ts)])
                proj_chunk(win0_d[ec], hT, wbufs, ec, evac, (0, 1, 2, 3))
                if dump == "U" and ec == 0:
                    dump_buf(U[:, 8:8 + S_LEN], 2048, 0, [("U", ts) for ts in range(4)])
                    S.barrier()
                    return
                k = g + 1
                half = 1 << g
                w_ = 2 * half
                bufs = [U, B1, B2]
                src = U
                src_r = [("U", ts) for ts in range(4)] + ["Upad"]
                cur = None
                for step in range(k):
                    sh = 1 << step
                    dst = B1 if (step % 2 == 0) else B2
                    dres = "B1" if (step % 2 == 0) else "B2"
                    n = L - 8 if step > 0 else L - 1
                    n = L - sh - (0 if step == 0 else 0)
                    nn = L - sh
                    eng = "dve" if step % 2 == 0 else "pool"
                    S.add(eng, lambda e, dst=dst, src=src, sh=sh, nn=nn: e.tensor_tensor(out=dst[:, 0:nn], in0=src[:, 0:nn], in1=src[:, sh:sh + nn], op=ALU.add),
                          r=src_r + ["Bpad"], w=[dres])
                    src = dst
                    src_r = [dres]
                off = 8 - half
                cc = ec % 3
                S.add("dve", lambda e, src=src, off=off, w_=w_, cc=cc: e.scalar_tensor_tensor(
                    out=pooled[:, cc, :], in0=src[:, off:off + S_LEN], scalar=1.0 / w_, in1=U[:, 8:8 + S_LEN],
                    op0=ALU.mult, op1=ALU.subtract), r=src_r + [("U", ts) for ts in range(4)], w=[("pooled", cc)])
                for (c0, pe0) in ((0, 0), (S_LEN - 8, 8)):
                    S.add("dve", lambda e, src=src, off=off, c0=c0, pe0=pe0, g=g: e.tensor_tensor(
                        out=tmp8[:, 0:8], in0=src[:, off + c0:off + c0 + 8], in1=pedge[:, g, pe0:pe0 + 8], op=ALU.mult),
                        r=src_r + ["_pedge"], w=["tmp8"])
                    S.add("dve", lambda e, c0=c0, cc=cc: e.tensor_tensor(
                        out=pooled[:, cc, c0:c0 + 8], in0=tmp8[:, 0:8], in1=U[:, 8 + c0:16 + c0], op=ALU.subtract),
                        r=["tmp8"] + [("U", ts) for ts in range(4)], w=[("pooled", cc)])
                if cc == 2:
                    for oc in range(3):
                        bi = cat_n[0] % 2
                        cat_n[0] += 1
                        for ts in range(4):
                            bank = 4 + (ts % 2)

                            def mm(e, oc=oc, ts=ts, bank=bank, g=g):
                                last = None
                                for c3 in range(3):
                                    last = e.matmul(psb[bank][:, :], wgrp[:, g, c3 * 384 + oc * 128:c3 * 384 + (oc + 1) * 128],
                                                    pooled[:, c3, ts * 512:(ts + 1) * 512], start=(c3 == 0), stop=(c3 == 2))
                                return last
                            S.add("pe", mm, r=["_wgrp"] + [("pooled", c3) for c3 in range(3)], w=[PS(bank)])
                            col = g * 3 + oc
                            S.add("act", lambda e, bi=bi, ts=ts, bank=bank, col=col: e.mul(
                                out=cst[bi][:, ts * 512:(ts + 1) * 512], in_=psb[bank][:, :], mul=pscale[:, col:col + 1]),
                                r=[PS(bank), "_pscale"], w=[("cst", bi)])
                        store_cat(g * 3 + oc, bi)
            else:
                h = ec - 12
                qb = qx[h % 2]

                def evac(ts, bank, qb=qb, h=h):
                    S.add("act", lambda e, ts=ts, bank=bank: e.copy(out=qb[:, ts * 512:(ts + 1) * 512], in_=psb[bank][:, :]),
                          r=[PS(bank)], w=[("qx", h % 2)])
                proj_chunk(win0_d[ec], hT, wbufs, ec, evac, (0, 1, 2, 3))
                bi = cat_n[0] % 2
                cat_n[0] += 1
                xattn_head(h, qb, ("qx", h % 2), kT, V, cst[bi], ("cst", bi), 6, 7)
                store_cat(12 + h, bi)
        S.barrier()
        A.reset(m0)

    def stage_outproj(layer, src_d):
        m0 = A.mark()
        catT = A.alloc([16, S_LEN], BF)
        wo = A.alloc([16, D], BF)
        xts = [A.alloc([D], F32) for _ in range(2)]
        for ec in range(16):
            dma("sp", catT[:, ec, :], cat_d[ec], [("catd", ec)], [("catT", ec)], ("catl", ec % 4))
            dma("pool", wo[:, ec, :], wout_d[layer, ec], [], [("wo", ec)], ("wol", ec % 4))
        for t in range(NT):
            b = t % 2
            dma("sp", xts[b], src_d[t * 128:(t + 1) * 128, :], [], [("xo", b)], ("xo", b))
            for ds in range(4):
                bank = 4 * b + ds

                def mm(e, t=t, ds=ds, bank=bank):
                    last = None
                    for ec in range(16):
                        last = e.matmul(psb[bank][:, :], catT[:, ec, t * 128:(t + 1) * 128], wo[:, ec, ds * 512:(ds + 1) * 512],
                                        start=(ec == 0), stop=(ec == 15))
                    return last
                S.add("pe", mm, r=[("catT", ec) for ec in range(16)] + [("wo", ec) for ec in range(16)], w=[PS(bank)])
                S.add("dve", lambda e, b=b, ds=ds, bank=bank: e.tensor_tensor(
                    out=xts[b][:, ds * 512:(ds + 1) * 512], in0=psb[bank][:, :], in1=xts[b][:, ds * 512:(ds + 1) * 512], op=ALU.add),
                    r=[PS(bank), ("xo", b)], w=[("xo", b)])
            dma("sp", xres_d[t * 128:(t + 1) * 128, :], xts[b], [("xo", b)], [("xres", t)], ("xost", b))
        S.barrier()
        A.reset(m0)

    def stage_router(layer, pre=None):
        m0 = A.mark()
        if pre is not None:
            pre()
        xts = [A.alloc([D], F32) for _ in range(2)]
        hms = [A.alloc([D], BF) for _ in range(2)]
        junk = A.alloc([D], BF)
        hTt = [A.alloc([16, 128], BF) for _ in range(2)]
        wr = A.alloc([16, 16], BF)
        affT = A.alloc([S_LEN], F32)
        wkb = A.alloc([S_LEN], F32)
        vals = A.alloc([CAP], F32)
        idxu = A.alloc([CAP], U32)
        idxf = A.alloc([CAP], F32)
        lg = [A.alloc([16], F32) for _ in range(2)]
        load_gain(1 + 2 * layer)
        dma("pool", wr, wr_d[layer].rearrange("p (a b) -> p a b", a=16), [], ["_wr"], "wr")
        for t in range(NT):
            b = t % 2
            dma("sp", xts[b], xres_d[t * 128:(t + 1) * 128, :], [("xres", t)], [("xt", b)], ("xt", b))
            norm_tile(xts[b], ("xt", b), hms[b], ("hm", b), junk, 2 * b)
            dma("sp", hrow_d[t * 128:(t + 1) * 128, :], hms[b], [("hm", b)], ["hrow"], ("hst", b))
            transpose_tile(hms[b], ("hm", b), hTt[b], ("hTt", b), (2 * b, 2 * b + 1), evac=("act" if b == 0 else "dve"))
            bank = 4 + b

            def mm(e, b=b, bank=bank):
                last = None
                for dc in range(16):
                    last = e.matmul(psb[bank][:, 0:16], hTt[b][:, dc, :], wr[:, dc, :], start=(dc == 0), stop=(dc == 15))
                return last
            S.add("pe", mm, r=[(("hTt", b), 0), (("hTt", b), 1), "_wr"], w=[PS(bank)])
            mx = stat[:, 16 + b:17 + b]
            sm = stat[:, 18 + b:19 + b]
            S.add("dve", lambda e, bank=bank, mx=mx: e.tensor_reduce(out=mx, in_=psb[bank][:, 0:16], axis=AX.X, op=ALU.max),
                  r=[PS(bank)], w=[("rs", b)])
            S.add("dve", lambda e, mx=mx: e.tensor_scalar(out=mx, in0=mx, scalar1=-1.0, scalar2=None, op0=ALU.mult),
                  r=[("rs", b)], w=[("rs", b)])
            S.add("act", lambda e, b=b, bank=bank, mx=mx, sm=sm: e.activation(out=lg[b], in_=psb[bank][:, 0:16], func=AF.Exp, bias=mx, scale=1.0, accum_out=sm),
                  r=[PS(bank), ("rs", b)], w=[("lg", b), ("rsm", b)])
            S.add("dve", lambda e, sm=sm: e.reciprocal(out=sm, in_=sm), r=[("rsm", b)], w=[("rsm", b)])
            S.add("dve", lambda e, b=b, sm=sm: e.tensor_scalar(out=lg[b], in0=lg[b], scalar1=sm, scalar2=None, op0=ALU.mult),
                  r=[("lg", b), ("rsm", b)], w=[("lg", b)])
            tb = 6 + b
            S.add("pe", lambda e, b=b, tb=tb: e.transpose(out=psb[tb][0:16, 0:128], in_=lg[b], identity=identf),
                  r=[("lg", b), "_identf"], w=[PS(tb)])
            S.add("act", lambda e, t=t, tb=tb: e.copy(out=affT[0:16, t * 128:(t + 1) * 128], in_=psb[tb][0:16, 0:128]),
                  r=[PS(tb)], w=["affT"])
        cur = affT
        for it in range(CAP // 8):
            v8 = vals[0:16, it * 8:it * 8 + 8]
            i8 = idxu[0:16, it * 8:it * 8 + 8]
            S.add("dve", lambda e, v8=v8, cur=cur: e.max(out=v8, in_=cur[0:16, :]), r=["affT", "wkb"], w=[("v8", it)])
            S.add("dve", lambda e, v8=v8, i8=i8, cur=cur: e.max_index(out=i8, in_max=v8, in_values=cur[0:16, :]),
                  r=["affT", "wkb", ("v8", it)], w=[("i8", it)])
            if it < CAP // 8 - 1:
                S.add("dve", lambda e, v8=v8, cur=cur: e.match_replace(out=wkb[0:16, :], in_to_replace=v8, in_values=cur[0:16, :], imm_value=-1.0),
                      r=["affT", ("v8", it)], w=["wkb"])
                cur = wkb
        S.add("dve", lambda e: e.tensor_copy(out=idxf[0:16, :], in_=idxu[0:16, :]), r=[("i8", it) for it in range(CAP // 8)], w=["idxf"])
        for ct in range(2):
            S.add("pe", lambda e, ct=ct: e.transpose(out=psb[0][:, ct * 16:(ct + 1) * 16], in_=idxf[0:16, ct * 128:(ct + 1) * 128], identity=identf[0:16, 0:16]),
                  r=["idxf", "_identf"], w=[(PS(0), ct)])
            S.add("pe", lambda e, ct=ct: e.transpose(out=psb[1][:, ct * 16:(ct + 1) * 16], in_=vals[0:16, ct * 128:(ct + 1) * 128], identity=identf[0:16, 0:16]),
                  r=[("v8", it) for it in range(CAP // 8)] + ["_identf"], w=[(PS(1), ct)])
        S.add("dve", lambda e: e.tensor_copy(out=idxT.rearrange("p a b -> p (a b)"), in_=psb[0][:, 0:32]), r=[(PS(0), 0), (PS(0), 1)], w=["idxT"])
        S.add("dve", lambda e: e.tensor_copy(out=gT.rearrange("p a b -> p (a b)"), in_=psb[1][:, 0:32]), r=[(PS(1), 0), (PS(1), 1)], w=["gT"])
        if dump == "rt":
            dump_buf(idxT.rearrange("p a b -> p (a b)"), 32, 0, ["idxT"])
            dump_buf(gT.rearrange("p a b -> p (a b)"), 32, 32, ["gT"])
        S.barrier()
        A.reset(m0)

    def stage_moe(layer):
        m0 = A.mark()
        NBIG, NSM = 4, 5
        big = [A.alloc([16, 512], BF) for _ in range(NBIG)]
        sml = [A.alloc([8, 512], BF) for _ in range(NSM)]
        nb = [0]
        ns = [0]
        slots_of = {}

        def load_big(e_):
            slots = slots_of.setdefault(e_, {})
            for fs in range(2):
                for nm, src in (("g", wg_d), ("u", wu_d)):
                    bi = nb[0] % NBIG
                    nb[0] += 1
                    dma("pool", big[bi], src[layer, e_, fs].rearrange("p (a b) -> p a b", a=16), [], [("big", bi)], ("big", bi))
                    slots[(nm, fs)] = bi

        def load_sml(e_):
            slots = slots_of.setdefault(e_, {})
            for ds in range(4):
                si = ns[0] % NSM
                ns[0] += 1
                dma("pool", sml[si], wd_d[layer, e_, ds].rearrange("p (a b) -> p a b", a=8), [], [("sml", si)], ("sml", si))
                slots[("d", ds)] = si

        stage_router(layer, pre=lambda: (load_big(0), load_sml(0)))
        xg = [[A.alloc([D], BF) for _ in range(2)] for _ in range(2)]
        xgT = [A.alloc([16, CAP], BF) for _ in range(2)]
        act_tm = A.alloc([2, DFF], BF)
        actT = A.alloc([8, CAP], BF)
        sa = [A.alloc([512], F32) for _ in range(2)]
        yo = [A.alloc([D], F32) for _ in range(2)]

        def gather(e_):
            pb = e_ % 2
            for ct in range(2):
                S.add("pool", lambda e, pb=pb, ct=ct, e_=e_: e.indirect_dma_start(
                    out=xg[pb][ct], out_offset=None, in_=hrow_d,
                    in_offset=bass.IndirectOffsetOnAxis(ap=idxT[:, ct, e_:e_ + 1], axis=0)),
                    r=["hrow", "idxT"], w=[("xg", pb, ct)], dma=("xg", pb, ct))

        gather(0)
        for e_ in range(NE):
            pb = e_ % 2
            slots = slots_of[e_]
            if e_ + 1 < NE:
                gather(e_ + 1)
            for ct in range(2):
                transpose_tile(xg[pb][ct], ("xg", pb, ct), xgT[pb][:, :, ct * 128:(ct + 1) * 128], ("xgT", pb, ct),
                               (6, 7), evac=("act" if ct == 0 else "dve"))
            xg_r = [(("xgT", pb, ct), h) for ct in range(2) for h in range(2)]
            for fs in range(2):
                bg = slots[("g", fs)]
                bu = slots[("u", fs)]
                for ct in range(2):
                    pa = ct
                    pu = 2 + ct

                    def mm(e, bw, bank, ct=ct, pb=pb):
                        last = None
                        for dc in range(16):
                            last = e.matmul(psb[bank][:, :], xgT[pb][:, dc, ct * 128:(ct + 1) * 128], big[bw][:, dc, :],
                                            start=(dc == 0), stop=(dc == 15))
                        return last
                    S.add("pe", lambda e, bg=bg, pa=pa, mm=mm: mm(e, bg, pa), r=xg_r + [("big", bg)], w=[PS(pa)])
                    S.add("pe", lambda e, bu=bu, pu=pu, mm=mm: mm(e, bu, pu), r=xg_r + [("big", bu)], w=[PS(pu)])
                    S.add("act", lambda e, ct=ct, pa=pa: e.activation(out=sa[ct], in_=psb[pa][:, :], func=AF.Silu),
                          r=[PS(pa)], w=[("sa", ct)])
                    S.add("dve", lambda e, ct=ct, pu=pu, fs=fs: e.tensor_tensor(out=act_tm[:, ct, fs * 512:(fs + 1) * 512], in0=sa[ct], in1=psb[pu][:, :], op=ALU.mult),
                          r=[("sa", ct), PS(pu)], w=[("act_tm", ct, fs)])
            if e_ + 1 < NE:
                load_big(e_ + 1)
            for ct in range(2):
                pv = psbf(6 + ct)[:, :].rearrange("p (a b) -> p a b", a=8)

                def tr(e, ct=ct, pv=pv):
                    last = None
                    for fc in range(8):
                        last = e.transpose(out=pv[:, fc, :], in_=act_tm[:, ct, fc * 128:(fc + 1) * 128], identity=identb)
                    return last
                S.add("pe", tr, r=[("act_tm", ct, 0), ("act_tm", ct, 1), "_identb"], w=[PS(6 + ct)])
                if ct == 0:
                    S.add("act", lambda e, ct=ct, pv=pv: e.copy(out=actT[:, :, ct * 128:(ct + 1) * 128], in_=pv), r=[PS(6 + ct)], w=[("actT", ct)])
                else:
                    S.add("dve", lambda e, ct=ct, pv=pv: e.tensor_copy(out=actT[:, :, ct * 128:(ct + 1) * 128], in_=pv), r=[PS(6 + ct)], w=[("actT", ct)])
            k = 0
            for ds in range(4):
                si = slots[("d", ds)]
                for ct in range(2):
                    bank = 4 + (k % 2)
                    k += 1

                    def mmd(e, ct=ct, si=si, bank=bank):
                        last = None
                        for fc in range(8):
                            last = e.matmul(psb[bank][:, :], actT[:, fc, ct * 128:(ct + 1) * 128], sml[si][:, fc, :],
                                            start=(fc == 0), stop=(fc == 7))
                        return last
                    S.add("pe", mmd, r=[("actT", 0), ("actT", 1), ("sml", si)], w=[PS(bank)])
                    if ct == 0:
                        S.add("act", lambda e, ct=ct, ds=ds, bank=bank, e_=e_: e.mul(
                            out=yo[ct][:, ds * 512:(ds + 1) * 512], in_=psb[bank][:, :], mul=gT[:, ct, e_:e_ + 1]),
                            r=[PS(bank), "gT"], w=[("yo", ct)])
                    else:
                        S.add("dve", lambda e, ct=ct, ds=ds, bank=bank, e_=e_: e.tensor_scalar(
                            out=yo[ct][:, ds * 512:(ds + 1) * 512], in0=psb[bank][:, :], scalar1=gT[:, ct, e_:e_ + 1], scalar2=None, op0=ALU.mult),
                            r=[PS(bank), "gT"], w=[("yo", ct)])
            if e_ + 1 < NE:
                load_sml(e_ + 1)
            for ct in range(2):
                S.add("pool", lambda e, ct=ct, e_=e_: e.indirect_dma_start(
                    out=xres_d, out_offset=bass.IndirectOffsetOnAxis(ap=idxT[:, ct, e_:e_ + 1], axis=0),
                    in_=yo[ct], in_offset=None, compute_op=ALU.add),
                    r=[("yo", ct), "idxT"], w=["xres_all"], dma=("sc", ct))
        S.barrier()
        A.reset(m0)

    def stage_mixer1():
        m0 = A.mark()
        hT = A.alloc([16, S_LEN], BF)
        kT = A.alloc([4, MEM], BF)
        V = A.alloc([2, 512], BF)
        Ct = A.alloc([S_LEN], F32)
        St = A.alloc([S_LEN], F32)
        invf = A.alloc([2], F32)
        rotT = A.alloc([128], F32)
        stage_norm_hT(xres_d, 2, hT)
        m1 = A.mark()
        posi = A.alloc([S_LEN], I32)
        posf = A.alloc([S_LEN], F32)
        dma("sp", posi, pos_d.partition_broadcast(128), [], ["posi"], "posi")
        dma("sp", invf, cinvf_d, [], ["_invf"], "c5")
        dma("sp", rotT, crot_d, [], ["_rotT"], "c6")
        S.add("dve", lambda e: e.tensor_copy(out=posf, in_=posi), r=["posi"], w=["posf"])
        ki = A.alloc([S_LEN], I32)
        mk = A.alloc([S_LEN], F32)
        SC = 6.283179

        def reduce_(T, res):
            S.add("dve", lambda e: e.tensor_copy(out=ki, in_=T), r=[res], w=["ki"])
            S.add("dve", lambda e: e.tensor_copy(out=mk, in_=ki), r=["ki"], w=["mk"])
            S.add("dve", lambda e: e.tensor_tensor(out=T, in0=T, in1=mk, op=ALU.subtract), r=[res, "mk"], w=[res])
            S.add("dve", lambda e: e.tensor_scalar(out=mk, in0=T, scalar1=0.5, scalar2=None, op0=ALU.is_gt), r=[res], w=["mk"])
            S.add("dve", lambda e: e.tensor_tensor(out=T, in0=T, in1=mk, op=ALU.subtract), r=[res, "mk"], w=[res])
            S.add("dve", lambda e: e.tensor_scalar(out=mk, in0=T, scalar1=-0.5, scalar2=None, op0=ALU.is_lt), r=[res], w=["mk"])
            S.add("dve", lambda e: e.tensor_tensor(out=T, in0=T, in1=mk, op=ALU.add), r=[res, "mk"], w=[res])
            S.add("act", lambda e: e.activation(out=T, in_=T, func=AF.Sin, scale=SC), r=[res], w=[res])
        S.add("dve", lambda e: e.tensor_scalar(out=St, in0=posf, scalar1=invf[:, 0:1], scalar2=None, op0=ALU.mult),
              r=["posf", "_invf"], w=["St"])
        S.add("dve", lambda e: e.tensor_scalar(out=Ct, in0=posf, scalar1=invf[:, 0:1], scalar2=0.25, op0=ALU.mult, op1=ALU.add),
              r=["posf", "_invf"], w=["Ct"])
        reduce_(St, "St")
        reduce_(Ct, "Ct")
        S.add("dve", lambda e: e.tensor_scalar(out=St, in0=St, scalar1=invf[:, 1:2], scalar2=None, op0=ALU.mult), r=["St", "_invf"], w=["St"])
        S.add("dve", lambda e: e.tensor_scalar(out=Ct, in0=Ct, scalar1=-1.0, scalar2=invf[:, 1:2], op0=ALU.add, op1=ALU.mult), r=["Ct", "_invf"], w=["Ct"])
        S.add("dve", lambda e: e.tensor_scalar(out=Ct, in0=Ct, scalar1=1.0, scalar2=None, op0=ALU.add), r=["Ct"], w=["Ct"])
        S.barrier()
        A.reset(m1)
        mem_kv(1, kT, V)
        wbufs = [A.alloc([16, 128], BF) for _ in range(3)]
        wv = A.alloc([16, 192], BF)
        v_tm = A.alloc([NT, 192], BF)
        kT2 = A.alloc([3, S_LEN], BF)
        qf = A.alloc([S_LEN], F32)
        t1 = A.alloc([512], F32)
        qTb = [A.alloc([S_LEN], BF) for _ in range(2)]
        mask = A.alloc([2, 384], BF)
        pb_ = [A.alloc([2, 384], BF) for _ in range(2)]
        pm_ = [A.alloc([2, 384], BF) for _ in range(2)]
        pTs = [A.alloc([6, 128], BF) for _ in range(2)]
        otm = [A.alloc([128], BF) for _ in range(2)]
        cst = [A.alloc([S_LEN], BF) for _ in range(2)]
        qx = [A.alloc([S_LEN], BF) for _ in range(2)]
        dma("sp", mask, cmask_d.rearrange("p (a b) -> p a b", a=2), [], ["_mask"], "c7")
        dma("pool", wv, wv1_d.rearrange("p (a b) -> p a b", a=16), [], ["_wv"], "wv")
        hT_all = [(("hT", t), h) for t in range(NT) for h in range(2)]
        for t in range(NT):
            bank = t % 2

            def mm(e, t=t, bank=bank):
                last = None
                for dc in range(16):
                    last = e.matmul(psb[bank][:, 0:192], hT[:, dc, t * 128:(t + 1) * 128], wv[:, dc, :], start=(dc == 0), stop=(dc == 15))
                return last
            S.add("pe", mm, r=["_wv", (("hT", t), 0), (("hT", t), 1)], w=[PS(bank)])
            S.add("act", lambda e, t=t, bank=bank: e.copy(out=v_tm[:, t, :], in_=psb[bank][:, 0:192]), r=[PS(bank)], w=[("v_tm", t)])

        def rotary_chunk(dst, dst_res):
            for ts in range(4):
                for hh in range(2):
                    c0 = ts * 512 + hh * 256
                    S.add("pe", lambda e, c0=c0: e.matmul(psb[2][:, 0:256], rotT, qf[:, c0:c0 + 256], start=True, stop=True),
                          r=[("qf", ts), "_rotT"], w=[(PS(2), 0)])
                    S.add("dve", lambda e, c0=c0: e.tensor_tensor(out=t1[:, 0:256], in0=psb[2][:, 0:256], in1=St[:, c0:c0 + 256], op=ALU.mult),
                          r=[(PS(2), 0), "St"], w=["t1a"])
                    S.add("pool", lambda e, c0=c0: e.tensor_tensor(out=t1[:, 256:512], in0=qf[:, c0:c0 + 256], in1=Ct[:, c0:c0 + 256], op=ALU.mult),
                          r=[("qf", ts), "Ct"], w=["t1b"])
                    S.add("dve", lambda e, c0=c0: e.tensor_tensor(out=dst[:, c0:c0 + 256], in0=t1[:, 0:256], in1=t1[:, 256:512], op=ALU.add),
                          r=["t1a", "t1b"], w=[dst_res])

        def evac_qf(ts, bank):
            S.add("act", lambda e, ts=ts, bank=bank: e.copy(out=qf[:, ts * 512:(ts + 1) * 512], in_=psb[bank][:, :]),
                  r=[PS(bank)], w=[("qf", ts)])

        ci = [0]
        for g in range(3):
            proj_chunk(win1_d[g], hT, wbufs, ci[0], evac_qf, (0, 1))
            ci[0] += 1
            rotary_chunk(kT2[:, g, :], ("kT2", g))
        cat_n = [0]

        def store_cat(chunk, buf_i):
            dma("sp", cat_d[chunk], cst[buf_i], [("cst", buf_i)], [("catd", chunk)], ("cstst", buf_i))

        inst = [0]
        for j in range(12):
            g = j // 4
            qb = qTb[j % 2]
            qres = ("qTb", j % 2)
            proj_chunk(win1_d[3 + j], hT, wbufs, ci[0], evac_qf, (0, 1))
            ci[0] += 1
            rotary_chunk(qb, qres)
            bi = cat_n[0] % 2
            cat_n[0] += 1
            for n in range(NT):
                ib = inst[0] % 2
                inst[0] += 1
                kb0 = max(n - 1, 0)
                kb1 = min(n + 1, NT - 1)
                nk = (kb1 - kb0 + 1) * 128
                moff = 0 if n > 0 else 128
                sb0 = 3 + 2 * ib
                for hh in range(2):
                    S.add("pe", lambda e, hh=hh, n=n, kb0=kb0, nk=nk, sb0=sb0, qb=qb, g=g: e.matmul(
                        psb[sb0 + hh][:, 0:nk], qb[hh * 64:(hh + 1) * 64, n * 128:(n + 1) * 128],
                        kT2[hh * 64:(hh + 1) * 64, g, kb0 * 128:kb0 * 128 + nk], start=True, stop=True),
                        r=[qres, ("kT2", g)], w=[PS(sb0 + hh)])
                mx = stat[:, 24 + 2 * ib:26 + 2 * ib]
                sm = stat[:, 28 + 2 * ib:30 + 2 * ib]
                es_ = stat[:, 32 + 2 * ib:34 + 2 * ib]
                for hh in range(2):
                    S.add("dve", lambda e, hh=hh, nk=nk, sb0=sb0, mx=mx: e.tensor_reduce(out=mx[:, hh:hh + 1], in_=psb[sb0 + hh][:, 0:nk], axis=AX.X, op=ALU.max),
                          r=[PS(sb0 + hh)], w=[("amx", ib, hh)])
                S.add("dve", lambda e, mx=mx, j=j: e.scalar_tensor_tensor(out=mx, in0=mx, scalar=-0.125, in1=nsink[:, 2 * j:2 * j + 2], op0=ALU.mult, op1=ALU.min),
                      r=[("amx", ib, 0), ("amx", ib, 1), "_nsink"], w=[("anm", ib)])
                for hh in range(2):
                    S.add("act", lambda e, hh=hh, nk=nk, sb0=sb0, mx=mx, ib=ib: e.activation(
                        out=pb_[ib][:, hh, 0:nk], in_=psb[sb0 + hh][:, 0:nk], func=AF.Exp, bias=mx[:, hh:hh + 1], scale=0.125),
                        r=[PS(sb0 + hh), ("anm", ib)], w=[("ap", ib, hh)])
                    S.add("dve", lambda e, hh=hh, nk=nk, ib=ib, moff=moff, sm=sm: e.scalar_tensor_tensor(
                        out=pm_[ib][:, hh, 0:nk], in0=pb_[ib][:, hh, 0:nk], scalar=1.0, in1=mask[:, 0, moff:moff + nk],
                        op0=ALU.mult, op1=ALU.mult, accum_out=sm[:, hh:hh + 1]),
                        r=[("ap", ib, hh), "_mask"], w=[("apm", ib, hh), ("asm", ib, hh)])
                S.add("dve", lambda e, mx=mx, es_=es_, j=j: e.tensor_tensor(out=es_, in0=mx, in1=nsink[:, 2 * j:2 * j + 2], op=ALU.subtract),
                      r=[("anm", ib), "_nsink"], w=[("aes", ib)])
                S.add("act", lambda e, es_=es_: e.activation(out=es_, in_=es_, func=AF.Exp), r=[("aes", ib)], w=[("aes", ib)])
                S.add("dve", lambda e, es_=es_, sm=sm: e.tensor_tensor(out=sm, in0=sm, in1=es_, op=ALU.add),
                      r=[("aes", ib), ("asm", ib, 0), ("asm", ib, 1)], w=[("ard", ib)])
                S.add("dve", lambda e, sm=sm: e.reciprocal(out=sm, in_=sm), r=[("ard", ib)], w=[("ard", ib)])
                nkb = nk // 128
                pv = psbf(7)[:, 0:768].rearrange("p (a b) -> p a b", a=6)

                def tr(e, ib=ib, nkb=nkb, pv=pv):
                    last = None
                    for hh in range(2):
                        for kb in range(nkb):
                            last = e.transpose(out=pv[:, hh * 3 + kb, :], in_=pm_[ib][:, hh, kb * 128:(kb + 1) * 128], identity=identb)
                    return last
                S.add("pe", tr, r=[("apm", ib, 0), ("apm", ib, 1), "_identb"], w=[(PS(7), 0)])
                S.add("act", lambda e, ib=ib, pv=pv: e.copy(out=pTs[ib], in_=pv), r=[(PS(7), 0)], w=[("apT", ib)])
                po = psb[7][:, 384:512]

                def pvm(e, ib=ib, nkb=nkb, kb0=kb0, g=g, po=po):
                    last = None
                    for hh in range(2):
                        for kb in range(nkb):
                            last = e.matmul(po[:, hh * 64:(hh + 1) * 64], pTs[ib][:, hh * 3 + kb, :], v_tm[:, kb0 + kb, g * 64:(g + 1) * 64],
                                            start=(kb == 0), stop=(kb == nkb - 1))
                    return last
                S.add("pe", pvm, r=[("apT", ib)] + [("v_tm", kb0 + kb) for kb in range(nkb)], w=[(PS(7), 1)])
                for hh in range(2):
                    S.add("act", lambda e, hh=hh, ib=ib, po=po, sm=sm: e.mul(
                        out=otm[ib][:, hh * 64:(hh + 1) * 64], in_=po[:, hh * 64:(hh + 1) * 64], mul=sm[:, hh:hh + 1]),
                        r=[(PS(7), 1), ("ard", ib)], w=[("otm", ib)])
                ft = psbf(2)[:, 512:640]
                S.add("pe", lambda e, ib=ib, ft=ft: e.transpose(out=ft, in_=otm[ib], identity=identb), r=[("otm", ib), "_identb"], w=[(PS(2), 1)])
                S.add("dve", lambda e, n=n, bi=bi, ft=ft: e.tensor_copy(out=cst[bi][:, n * 128:(n + 1) * 128], in_=ft), r=[(PS(2), 1)], w=[("cst", bi)])
            store_cat(j, bi)
        for h in range(4):
            qb = qx[h % 2]

            def evac(ts, bank, qb=qb, h=h):
                S.add("act", lambda e, ts=ts, bank=bank: e.copy(out=qb[:, ts * 512:(ts + 1) * 512], in_=psb[bank][:, :]),
                      r=[PS(bank)], w=[("qx", h % 2)])
            proj_chunk(win1_d[15 + h], hT, wbufs, ci[0], evac, (0, 1))
            ci[0] += 1
            bi = cat_n[0] % 2
            cat_n[0] += 1
            xattn_head(h, qb, ("qx", h % 2), kT, V, cst[bi], ("cst", bi), 5, 6)
            store_cat(12 + h, bi)
        S.barrier()
        A.reset(m0)

    def stage_final():
        m0 = A.mark()
        xts = [A.alloc([D], F32) for _ in range(2)]
        outs = [A.alloc([D], F32) for _ in range(2)]
        junk = A.alloc([D], BF)
        load_gain(5)
        for t in range(NT):
            b = t % 2
            dma("sp", xts[b], xres_d[t * 128:(t + 1) * 128, :], [], [("xt", b)], ("xt", b))
            norm_tile(xts[b], ("xt", b), outs[b], ("of", b), junk, 2 * b)
            dma("sp", y_d[t * 128:(t + 1) * 128, :], outs[b], [("of", b)], ["y"], ("yst", b))
        A.reset(m0)

    stages = [
        ("mem", stage_mem),
        ("mix0", stage_mixer0),
        ("out0", lambda: stage_outproj(0, x_d)),
        ("ex0", lambda: stage_moe(0)),
        ("mix1", stage_mixer1),
        ("out1", lambda: stage_outproj(1, xres_d)),
        ("ex1", lambda: stage_moe(1)),
        ("final", stage_final),
    ]
    for name, fn in stages:
        fn()
        if stop_after == name:
            break
    if stop_after is not None and stop_after != "final":
        S.barrier()
    finals = [k for k in S.dma_cnt if isinstance(k, tuple) and k[0] in ("yst", "xost", "cstst", "sc")]
    S.emit(es, final_dma_keys=finals)
    es.close()
    return nc


def _consts():
    identb = np.eye(128, dtype=np.float32).astype(ml_dtypes.bfloat16)
    identf = np.eye(128, dtype=np.float32)
    rot = np.zeros((128, 128), np.float32)
    for b in (0, 64):
        for r in range(8):
            rot[b + r + 8, b + r] = -1.0
            rot[b + r, b + r + 8] = 1.0
    inv_freq = (500000.0 ** (-np.arange(0, 16, 2, dtype=np.float32) / 16)).astype(np.float32)
    invf = np.zeros((128, 2), np.float32)
    for p in range(128):
        r = p % 64
        if r < 16:
            invf[p, 0] = np.float32(inv_freq[r % 8] / (2.0 * np.pi))
            invf[p, 1] = 1.0
    qq = np.arange(128)[:, None]
    kk = np.arange(128)[None, :]
    m = np.concatenate([(kk >= qq), np.ones((128, 128), bool), (kk <= qq)], axis=1).astype(np.float32)
    mask = np.concatenate([m, m], axis=1).astype(ml_dtypes.bfloat16)
    pedge = np.zeros((4, 16), np.float32)
    t = np.arange(S_LEN)
    for g, w in enumerate((2, 4, 8, 16)):
        lo = np.maximum(t - w // 2, 0)
        hi = np.minimum(t + w // 2 - 1, S_LEN - 1)
        cnt = (hi - lo + 1).astype(np.float32)
        pedge[g, 0:8] = 1.0 / cnt[0:8]
        pedge[g, 8:16] = 1.0 / cnt[S_LEN - 8:]
    pedge = np.broadcast_to(pedge.reshape(1, 64), (128, 64)).copy()
    return identb, identf, rot, invf, mask, pedge


def _pieces(w, pc):
    R, C = w.shape
    n = R // 128
    return np.ascontiguousarray(w.reshape(n, 128, C // pc, pc).transpose(2, 1, 0, 3)).reshape(C // pc, 128, n * pc)


def _host_layout(inp, slim=False):
    f = lambda a: np.ascontiguousarray(np.asarray(a, dtype=np.float32))
    shared = {}
    shared["gains"] = np.stack([f(inp["norm_mix_g"])[0], f(inp["norm_ffn_g"])[0], f(inp["norm_mix_g"])[1],
                                f(inp["norm_ffn_g"])[1], f(inp["mem_norm_g"]), f(inp["final_g"])], axis=0)
    wkv = f(inp["mem_w_kv"])
    shared["wkv"] = np.ascontiguousarray(wkv.reshape(2, 4, 4, 128, 1024).transpose(0, 1, 3, 2, 4)).reshape(2, 4, 128, 4096)
    shared["win0"] = _pieces(f(inp["pool_w_in"])[0], 128)
    gw = f(inp["pool_group_w"])[0]
    shared["wgrp"] = np.ascontiguousarray(gw.reshape(4, 3, 128, 384).transpose(0, 2, 1, 3)).reshape(4, 128, 1152)
    shared["pscale"] = np.ascontiguousarray(f(inp["pool_scale"])[0].reshape(12, 128).T)
    shared["wout"] = np.stack([f(inp["pool_w_out"])[0].reshape(16, 128, 2048), f(inp["attn_w_out"])[0].reshape(16, 128, 2048)], axis=0)
    w1 = f(inp["attn_w_in"])[0]
    kcols = []
    for g in range(3):
        kc = w1[:, 1536 + g * 64:1536 + (g + 1) * 64]
        kcols.append(np.concatenate([kc, kc], axis=1))
    w1c = np.concatenate(kcols + [w1[:, 0:1536], w1[:, 1920:2432]], axis=1)
    shared["win1"] = _pieces(w1c, 128)
    wv = w1[:, 1728:1920]
    shared["wv1"] = np.ascontiguousarray(wv.reshape(16, 128, 192).transpose(1, 0, 2)).reshape(128, 16 * 192)
    shared["nsink"] = np.ascontiguousarray(np.broadcast_to(-f(inp["attn_sink"])[0][None, :], (128, 24)))
    wr = f(inp["router_w"])
    shared["wr"] = np.ascontiguousarray(wr.reshape(2, 16, 128, 16).transpose(0, 2, 1, 3)).reshape(2, 128, 256)
    wg = inp["exp_w_gate"]
    wu = inp["exp_w_up"]
    wd = inp["exp_w_down"]
    if slim:
        wg, wu, wd = wg[:slim[0], :slim[1]], wu[:slim[0], :slim[1]], wd[:slim[0], :slim[1]]
    wg, wu, wd = f(wg), f(wu), f(wd)
    nl_, ne_ = wg.shape[0], wg.shape[1]
    shared["wg"] = np.ascontiguousarray(wg.reshape(nl_, ne_, 16, 128, 2, 512).transpose(0, 1, 4, 3, 2, 5)).reshape(nl_, ne_, 2, 128, 8192)
    shared["wu"] = np.ascontiguousarray(wu.reshape(nl_, ne_, 16, 128, 2, 512).transpose(0, 1, 4, 3, 2, 5)).reshape(nl_, ne_, 2, 128, 8192)
    shared["wd"] = np.ascontiguousarray(wd.reshape(nl_, ne_, 8, 128, 4, 512).transpose(0, 1, 4, 3, 2, 5)).reshape(nl_, ne_, 4, 128, 4096)
    identb, identf, rot, invf, mask, pedge = _consts()
    shared["c_identb"] = identb
    shared["c_identf"] = identf
    shared["c_rot"] = rot
    shared["c_invf"] = invf
    shared["c_mask"] = mask
    shared["c_pedge"] = pedge
    return shared


_NC_CACHE = {}


def kernel(x, mem, positions, norm_mix_g, norm_ffn_g, mem_norm_g, final_g, mem_w_kv,
           pool_w_in, pool_group_w, pool_scale, pool_w_out,
           attn_w_in, attn_sink, attn_w_out,
           router_w, exp_w_gate, exp_w_up, exp_w_down):
    inp = dict(norm_mix_g=norm_mix_g, norm_ffn_g=norm_ffn_g, mem_norm_g=mem_norm_g, final_g=final_g,
               mem_w_kv=mem_w_kv, pool_w_in=pool_w_in, pool_group_w=pool_group_w, pool_scale=pool_scale,
               pool_w_out=pool_w_out, attn_w_in=attn_w_in, attn_sink=attn_sink, attn_w_out=attn_w_out,
               router_w=router_w, exp_w_gate=exp_w_gate, exp_w_up=exp_w_up, exp_w_down=exp_w_down)
    shared = _host_layout(inp)
    x = np.asarray(x, dtype=np.float32)
    mem = np.asarray(mem, dtype=np.float32)
    positions = np.asarray(positions, dtype=np.int32)
    if "nc" not in _NC_CACHE:
        _NC_CACHE["nc"] = build()
    nc = _NC_CACHE["nc"]
    n = 8
    in_maps = []
    for b in range(n):
        m = dict(shared)
        m["x"] = np.ascontiguousarray(x[b])
        m["mem"] = np.ascontiguousarray(mem[b])
        m["pos"] = np.ascontiguousarray(positions[b].reshape(1, S_LEN))
        in_maps.append(m)
    res = run_bass_kernel_spmd(nc, in_maps, core_ids=list(range(n)))
    return np.stack([r["y"] for r in res.results], axis=0)
```

```python
import numpy as np
from contextlib import ExitStack
import ml_dtypes
import concourse.bass as bass
import concourse.mybir as mybir
from concourse.bass_utils import run_bass_kernel_spmd

dt = mybir.dt
F32 = dt.float32
BF = dt.bfloat16
I32 = dt.int32
U32 = dt.uint32
AF = mybir.ActivationFunctionType
ALU = mybir.AluOpType
AX = mybir.AxisListType

D = 2048
S_LEN = 2048
NT = 16
NDC = 16
MEM = 256
NE = 16
CAP = 256
DFF = 1024
EPS = 1e-6
ENGS = ("sp", "act", "pool", "dve", "pe")


STATS = {}


class Op:
    __slots__ = ("eng", "fn", "deps", "dma", "needed", "sem", "val", "idx")


class Sched:
    def __init__(self, nc):
        self.nc = nc
        self.ops = {e: [] for e in ENGS}
        self.last_w = {}
        self.readers = {}
        self.dma_cnt = {}
        self.dma_last = {}
        self.dma_prev = {}
        self.n = 0

    def _new(self, eng, fn, dma):
        op = Op()
        op.eng = eng
        op.fn = fn
        op.dma = dma
        op.needed = False
        op.sem = None
        op.val = 0
        op.idx = self.n
        op.deps = []
        self.n += 1
        return op

    def add(self, eng, fn, r=(), w=(), dma=None):
        op = self._new(eng, fn, dma)
        deps = {}
        for x in r:
            lw = self.last_w.get(x)
            if lw is not None:
                deps[lw.idx] = lw
        for x in w:
            lw = self.last_w.get(x)
            if lw is not None:
                deps[lw.idx] = lw
            for rd in self.readers.get(x, ()):
                deps[rd.idx] = rd
        best = {}
        out = []
        for d in deps.values():
            if d.dma is not None:
                out.append(d)
            else:
                if d.eng == "pe" and eng == "pe" and dma is None:
                    continue
                b = best.get(d.eng)
                if b is None or d.idx > b.idx:
                    best[d.eng] = d
        out.extend(best.values())
        if dma is not None:
            pv_ = self.dma_prev.get(dma)
            if pv_ is not None and all(pv_ is not d for d in out):
                out.append(pv_)
            self.dma_prev[dma] = op
        op.deps = out
        for d in out:
            d.needed = True
        for x in r:
            if not (isinstance(x, str) and x.startswith("_")):
                self.readers.setdefault(x, []).append(op)
        for x in w:
            self.last_w[x] = op
            self.readers[x] = []
        if dma is not None:
            c = self.dma_cnt.get(dma, 0) + 1
            self.dma_cnt[dma] = c
            op.val = 16 * c
            self.dma_last[dma] = op
        self.ops[eng].append(op)
        return op

    def barrier(self):
        lasts = []
        for e in ENGS:
            for op in reversed(self.ops[e]):
                if op.dma is None and op.fn is not None:
                    lasts.append(op)
                    break
        dl = list(self.dma_last.values())
        for e in ENGS:
            op = self._new(e, None, None)
            op.deps = [x for x in lasts] + dl
            for d in op.deps:
                d.needed = True
            self.ops[e].append(op)
        self.last_w = {}
        self.readers = {}
        self.dma_last = {}

    def emit(self, es, final_dma_keys=()):
        nc = self.nc
        esem = {e: es.enter_context(nc.semaphore("s_" + e)) for e in ENGS}
        dsem = {}
        for i, k in enumerate(self.dma_cnt):
            dsem[k] = es.enter_context(nc.semaphore("d%d" % i))
        for e in ENGS:
            c = 0
            for op in self.ops[e]:
                if op.dma is not None:
                    op.sem = dsem[op.dma]
                else:
                    op.sem = esem[e]
                    if op.needed:
                        c += 1
                        op.val = c
            STATS[e] = (c, len(self.ops[e]))
        STATS["dma_max"] = max(16 * v for v in self.dma_cnt.values())
        STATS["n_dma_keys"] = len(self.dma_cnt)
        ops = self.ops
        dma_cnt = self.dma_cnt

        def run(e, eng):
            waited = {}
            for op in ops[e]:
                for d in op.deps:
                    key = id(d.sem)
                    if waited.get(key, 0) < d.val:
                        eng.wait_ge(d.sem, d.val)
                        waited[key] = d.val
                if op.fn is None:
                    continue
                ins = op.fn(eng)
                if op.dma is not None:
                    ins.then_inc(op.sem, 16)
                elif op.needed:
                    ins.then_inc(op.sem, 1)
            if e == "sp":
                for k in final_dma_keys:
                    eng.wait_ge(dsem[k], 16 * dma_cnt[k])

        with nc.Block() as block:
            @block.sync
            def _(eng):
                run("sp", eng)

            @block.scalar
            def _(eng):
                run("act", eng)

            @block.gpsimd
            def _(eng):
                run("pool", eng)

            @block.vector
            def _(eng):
                run("dve", eng)

            @block.tensor
            def _(eng):
                run("pe", eng)


ARENA_BYTES = 206 * 1024


class Arena:
    def __init__(self, ap):
        self.ap = ap
        self.off = 0

    def mark(self):
        return self.off

    def reset(self, m):
        self.off = m

    def alloc(self, shape, dtype, parts=128):
        esz = {F32: 4, BF: 2, I32: 4, U32: 4}[dtype]
        n = 1
        for s in shape:
            n *= s
        nbytes = (n * esz + 31) // 32 * 32
        assert self.off + nbytes <= ARENA_BYTES, ("SBUF overflow", self.off, nbytes)
        v = self.ap[0:parts, self.off:self.off + n * esz].bitcast(dtype)
        self.off += nbytes
        if len(shape) == 2:
            v = v.rearrange("p (a b) -> p a b", a=shape[0])
        elif len(shape) == 3:
            v = v.rearrange("p (a b c) -> p a b c", a=shape[0], b=shape[1])
        return v


def build(stop_after=None, debug=False, slim=False, dump=None, only=None):
    nc = bass.Bass("TRN2", target_bir_lowering=False)

    def din(name, shape, dtype=F32):
        return nc.dram_tensor(name, list(shape), dtype, kind="ExternalInput").ap()

    x_d = din("x", [S_LEN, D])
    mem_d = din("mem", [MEM, D])
    pos_d = din("pos", [1, S_LEN], I32)
    gains_d = din("gains", [6, D])
    wkv_d = din("wkv", [2, 4, 128, 4 * 1024])
    win0_d = din("win0", [16, 128, 16 * 128])
    wgrp_d = din("wgrp", [4, 128, 3 * 384])
    pscale_d = din("pscale", [128, 12])
    wout_d = din("wout", [2, 16, 128, 2048])
    win1_d = din("win1", [19, 128, 16 * 128])
    wv1_d = din("wv1", [128, 16 * 192])
    sink_d = din("nsink", [128, 24])
    wr_d = din("wr", [2, 128, 16 * 16])
    nl_, ne_ = slim if slim else (2, NE)
    wg_d = din("wg", [nl_, ne_, 2, 128, 16 * 512])
    wu_d = din("wu", [nl_, ne_, 2, 128, 16 * 512])
    wd_d = din("wd", [nl_, ne_, 4, 128, 8 * 512])
    cidb_d = din("c_identb", [128, 128], BF)
    cidf_d = din("c_identf", [128, 128])
    crot_d = din("c_rot", [128, 128])
    cinvf_d = din("c_invf", [128, 2])
    cmask_d = din("c_mask", [128, 2 * 384], BF)
    cpedge_d = din("c_pedge", [128, 4 * 16])

    y_d = nc.dram_tensor("y", [S_LEN, D], F32, kind="ExternalOutput").ap()
    kind_dbg = "ExternalOutput" if debug else "Internal"
    xres_d = nc.dram_tensor("xres", [S_LEN, D], F32, kind=kind_dbg).ap()
    hrow_d = nc.dram_tensor("hrow", [S_LEN, D], BF, kind="Internal").ap()
    cat_d = nc.dram_tensor("catT", [16, 128, S_LEN], BF, kind=kind_dbg).ap()
    if debug:
        dbg_d = nc.dram_tensor("dbg", [128, 4096], F32, kind="ExternalOutput").ap()

    es = ExitStack()
    arena_t = es.enter_context(nc.sbuf_tensor("arena", [128, ARENA_BYTES], dt.uint8))
    A = Arena(arena_t)
    psb = [es.enter_context(nc.psum_tensor("ps%d" % i, [128, 512], F32)) for i in range(8)]
    S = Sched(nc)
    uid = [0]

    def PS(i):
        return ("ps", i)

    def psf(i, n=512):
        return psb[i][:, 0:n]

    def psbf(i):
        return psb[i][:, :].bitcast(BF)

    def dma(q, out, in_, r, w, key):
        return S.add(q, lambda e: e.dma_start(out=out, in_=in_), r=r, w=w, dma=key)

    def dump_buf(ap, ncols, col_off, res):
        st = A.alloc([ncols], F32)
        S.add("dve", lambda e: e.tensor_copy(out=st, in_=ap), r=res, w=["dumpst"])
        dma("sp", dbg_d[:, col_off:col_off + ncols], st, ["dumpst"], ["dbgd"], "dumpd")

    identb = A.alloc([128], BF)
    identf = A.alloc([128], F32)
    mem_nT = A.alloc([16, MEM], BF)
    gbc = A.alloc([D], F32)
    nsink = A.alloc([24], F32)
    pscale = A.alloc([12], F32)
    idxT = A.alloc([2, 16], U32)
    gT = A.alloc([2, 16], F32)
    stat = A.alloc([64], F32)
    dma("sp", identb, cidb_d, [], ["_identb"], "c0")
    dma("sp", identf, cidf_d, [], ["_identf"], "c1")
    dma("sp", nsink, sink_d, [], ["_nsink"], "c2")
    dma("sp", pscale, pscale_d, [], ["_pscale"], "c3")
    PMARK = A.mark()

    def load_gain(i):
        dma("sp", gbc, gains_d[i:i + 1, :].partition_broadcast(128), [], ["gbc"], "gbc")

    def norm_tile(xt, xt_res, h_out, h_res, junk, sidx):
        ss = stat[:, sidx:sidx + 1]
        rs = stat[:, sidx + 1:sidx + 2]
        S.add("act", lambda e: e.activation(out=junk, in_=xt, func=AF.Square, accum_out=ss),
              r=[xt_res], w=["junk", ("stat", sidx)])
        S.add("act", lambda e: e.activation(out=ss, in_=ss, func=AF.Sqrt, scale=1.0 / D, bias=EPS),
              r=[("stat", sidx)], w=[("stat", sidx)])
        S.add("dve", lambda e: e.reciprocal(out=rs, in_=ss), r=[("stat", sidx)], w=[("stat", sidx + 1)])
        S.add("dve", lambda e: e.scalar_tensor_tensor(out=h_out, in0=xt, scalar=rs, in1=gbc,
                                                      op0=ALU.mult, op1=ALU.mult),
              r=[xt_res, ("stat", sidx + 1), "gbc"], w=[h_res])

    def transpose_tile(h_tm, h_res, dst3, dst_res, pbanks, ncols=128, evac="act"):
        for half in range(2):
            bank = pbanks[half]
            pv = psbf(bank)[:, 0:8 * 128].rearrange("p (a b) -> p a b", a=8)

            def tr(e, half=half, pv=pv):
                last = None
                for k in range(8):
                    dc = half * 8 + k
                    last = e.transpose(out=pv[:, k, 0:ncols], in_=h_tm[0:ncols, dc * 128:(dc + 1) * 128],
                                       identity=identb[0:ncols, 0:ncols])
                return last
            S.add("pe", tr, r=[h_res, "_identb"], w=[PS(bank)])
            dsl = dst3[:, half * 8:(half + 1) * 8, :]
            if evac == "act":
                S.add("act", lambda e, dsl=dsl, pv=pv: e.copy(out=dsl, in_=pv[:, :, 0:ncols]),
                      r=[PS(bank)], w=[(dst_res, half)])
            else:
                S.add("dve", lambda e, dsl=dsl, pv=pv: e.tensor_copy(out=dsl, in_=pv[:, :, 0:ncols]),
                      r=[PS(bank)], w=[(dst_res, half)])

    def stage_mem():
        m0 = A.mark()
        xt = A.alloc([D], F32)
        hm = A.alloc([D], BF)
        junk = A.alloc([D], BF)
        load_gain(4)
        for mt in range(2):
            dma("sp", xt, mem_d[mt * 128:(mt + 1) * 128, :], [], ["xt"], "xt")
            norm_tile(xt, "xt", hm, "hm", junk, 0)
            transpose_tile(hm, "hm", mem_nT[:, :, mt * 128:(mt + 1) * 128], ("memT", mt), (0, 1))
        S.barrier()
        A.reset(m0)

    def stage_norm_hT(src_d, gain_idx, hT):
        m0 = A.mark()
        xts = [A.alloc([D], F32) for _ in range(2)]
        hms = [A.alloc([D], BF) for _ in range(2)]
        junk = A.alloc([D], BF)
        load_gain(gain_idx)
        for t in range(NT):
            b = t % 2
            dma("sp", xts[b], src_d[t * 128:(t + 1) * 128, :], [], [("xt", b)], ("xt", b))
            norm_tile(xts[b], ("xt", b), hms[b], ("hm", b), junk, 2 * b)
            transpose_tile(hms[b], ("hm", b), hT[:, :, t * 128:(t + 1) * 128], ("hT", t),
                           (2 * b, 2 * b + 1), evac=("act" if b == 0 else "dve"))
        S.barrier()
        A.reset(m0)

    def mem_kv(layer, kT, V):
        m0 = A.mark()
        wk = [A.alloc([4, 1024], BF) for _ in range(4)]
        for pc in range(4):
            dma("pool", wk[pc], wkv_d[layer, pc].rearrange("p (a b) -> p a b", a=4), [], [("wkv", pc)], ("wkv", pc))
        wres = [("wkv", pc) for pc in range(4)] + [(("memT", m), h) for m in range(2) for h in range(2)]
        for h in range(4):
            def mmk(e, h=h):
                last = None
                o = psb[4 + h // 2][:, (h % 2) * 256:(h % 2) * 256 + 256]
                for dc in range(16):
                    last = e.matmul(o, wk[dc // 4][:, dc % 4, h * 128:(h + 1) * 128], mem_nT[:, dc, :],
                                    start=(dc == 0), stop=(dc == 15))
                return last
            S.add("pe", mmk, r=wres, w=[PS(4 + h // 2)])
        for mt in range(2):
            def mmv(e, mt=mt):
                last = None
                for dc in range(16):
                    last = e.matmul(psb[6 + mt][:, :], mem_nT[:, dc, mt * 128:(mt + 1) * 128],
                                    wk[dc // 4][:, dc % 4, 512:1024], start=(dc == 0), stop=(dc == 15))
                return last
            S.add("pe", mmv, r=wres, w=[PS(6 + mt)])
        for h in range(4):
            if dump in ("hT2a", "hT2b"):
                break
            S.add("act", lambda e, h=h: e.copy(out=kT[:, h, :], in_=psb[4 + h // 2][:, (h % 2) * 256:(h % 2) * 256 + 256]),
                  r=[PS(4 + h // 2)], w=[("kT", h)])
        for mt in range(2):
            if dump in ("hT2a", "hT2b"):
                break
            S.add("dve", lambda e, mt=mt: e.tensor_copy(out=V[:, mt, :], in_=psb[6 + mt][:, :]),
                  r=[PS(6 + mt)], w=[("V", mt)])
        S.barrier()
        A.reset(m0)

    def proj_chunk(wsrc, hT, wbufs, ci, evac, banks):
        b = ci % len(wbufs)
        wb = wbufs[b]
        dma("pool", wb, wsrc.rearrange("p (a b) -> p a b", a=16), [], [("wb", b)], ("wb", b))
        for ts in range(4):
            bank = banks[ts % len(banks)]

            def mm(e, ts=ts, bank=bank, wb=wb):
                last = None
                for dc in range(16):
                    last = e.matmul(psb[bank][:, :], wb[:, dc, :], hT[:, dc, ts * 512:(ts + 1) * 512],
                                    start=(dc == 0), stop=(dc == 15))
                return last
            S.add("pe", mm, r=[("wb", b)] + [(("hT", t), h) for t in range(ts * 4, ts * 4 + 4) for h in range(2)],
                  w=[PS(bank)])
            evac(ts, bank)

    def xattn_head(h, qT, q_res, kT, V, cst, cst_res, bk_s, bk_t):
        scale = 128.0 ** -0.5
        m0 = A.mark()
        pbuf = [A.alloc([MEM], BF) for _ in range(2)]
        pTb = [A.alloc([2, 128], BF) for _ in range(2)]
        for t in range(NT):
            b = t % 2
            sps = psb[bk_s][:, b * 256:(b + 1) * 256]
            S.add("pe", lambda e, t=t, sps=sps: e.matmul(sps, qT[:, t * 128:(t + 1) * 128], kT[:, h, :], start=True, stop=True),
                  r=[q_res, ("kT", h)], w=[(PS(bk_s), b)])
            mx = stat[:, 8 + b:9 + b]
            sm = stat[:, 10 + b:11 + b]
            S.add("dve", lambda e, sps=sps, mx=mx: e.tensor_reduce(out=mx, in_=sps, axis=AX.X, op=ALU.max),
                  r=[(PS(bk_s), b)], w=[("xs", b)])
            S.add("dve", lambda e, mx=mx: e.tensor_scalar(out=mx, in0=mx, scalar1=-scale, scalar2=None, op0=ALU.mult),
                  r=[("xs", b)], w=[("xs", b)])
            S.add("act", lambda e, sps=sps, mx=mx, sm=sm, b=b: e.activation(out=pbuf[b], in_=sps, func=AF.Exp, bias=mx, scale=scale, accum_out=sm),
                  r=[(PS(bk_s), b), ("xs", b)], w=[("xp", b), ("xsm", b)])
            S.add("dve", lambda e, sm=sm: e.reciprocal(out=sm, in_=sm), r=[("xsm", b)], w=[("xsm", b)])
            S.add("dve", lambda e, sm=sm, b=b: e.tensor_scalar(out=pbuf[b], in0=pbuf[b], scalar1=sm, scalar2=None, op0=ALU.mult),
                  r=[("xp", b), ("xsm", b)], w=[("xp", b)])
            pv = psbf(bk_t)[:, b * 512:b * 512 + 256].rearrange("p (a c) -> p a c", a=2)

            def tr(e, b=b, pv=pv):
                last = None
                for mt in range(2):
                    last = e.transpose(out=pv[:, mt, :], in_=pbuf[b][:, mt * 128:(mt + 1) * 128], identity=identb)
                return last
            S.add("pe", tr, r=[("xp", b), "_identb"], w=[(PS(bk_t), b, 0)])
            S.add("act", lambda e, b=b, pv=pv: e.copy(out=pTb[b], in_=pv), r=[(PS(bk_t), b, 0)], w=[("xpT", b)])
            ops_ = psb[bk_t][:, b * 256 + 128:b * 256 + 256]

            def pvmm(e, b=b, ops_=ops_):
                last = None
                for mt in range(2):
                    last = e.matmul(ops_, V[:, mt, h * 128:(h + 1) * 128], pTb[b][:, mt, :], start=(mt == 0), stop=(mt == 1))
                return last
            S.add("pe", pvmm, r=[("xpT", b), ("V", 0), ("V", 1)], w=[(PS(bk_t), b, 1)])
            S.add("dve", lambda e, t=t, ops_=ops_: e.tensor_copy(out=cst[:, t * 128:(t + 1) * 128], in_=ops_),
                  r=[(PS(bk_t), b, 1)], w=[cst_res])
        A.reset(m0)

    def stage_mixer0():
        m0 = A.mark()
        hT = A.alloc([16, S_LEN], BF)
        kT = A.alloc([4, MEM], BF)
        V = A.alloc([2, 512], BF)
        stage_norm_hT(x_d, 0, hT)
        if dump == "hT":
            rr = [(("hT", t), h) for t in range(NT) for h in range(2)]
            dump_buf(hT[:, 0, :], 2048, 0, rr)
            dump_buf(hT[:, 15, :], 2048, 2048, rr)
            S.barrier()
            return
        mem_kv(0, kT, V)
        if dump in ("hT2", "hT2a", "hT2b"):
            rr = []
            dump_buf(hT[:, 0, :], 2048, 0, rr)
            dump_buf(hT[:, 15, :], 2048, 2048, rr)
            S.barrier()
            return
        L = S_LEN + 16
        U = A.alloc([L], F32)
        B1 = A.alloc([L], F32)
        B2 = A.alloc([L], F32)
        pooled = A.alloc([3, S_LEN], BF)
        wbufs = [A.alloc([16, 128], BF) for _ in range(3)]
        wgrp = A.alloc([4, 3 * 384], BF)
        pedge = A.alloc([4, 16], F32)
        tmp8 = A.alloc([16], F32)
        cst = [A.alloc([S_LEN], BF) for _ in range(2)]
        qx = [A.alloc([S_LEN], BF) for _ in range(2)]
        dma("sp", pedge, cpedge_d.rearrange("p (a b) -> p a b", a=4), [], ["_pedge"], "c4")
        for g in range(4):
            dma("pool", wgrp[:, g, :], wgrp_d[g], [], ["_wgrp"], "wgrp")
        S.add("dve", lambda e: e.memset(U[:, 0:8], 0.0), w=["Upad"])
        S.add("dve", lambda e: e.memset(U[:, 8 + S_LEN:L], 0.0), w=["Upad"])
        S.add("dve", lambda e: e.memset(B1[:, L - 8:L], 0.0), w=["Bpad"])
        S.add("dve", lambda e: e.memset(B2[:, L - 8:L], 0.0), w=["Bpad"])
        cat_n = [0]

        def store_cat(chunk, buf_i):
            dma("sp", cat_d[chunk], cst[buf_i], [("cst", buf_i)], [("catd", chunk)], ("cstst", buf_i))

        for ec in range(16):
            g = ec // 3
            if ec < 12:
                def evac(ts, bank):
                    S.add("act", lambda e, ts=ts, bank=bank: e.copy(out=U[:, 8 + ts * 512:8 + (ts + 1) * 512], in_=psb[bank][:, :]),
                          r=[PS(bank)], w=[("U", ts)])
                proj_chunk(win0_d[ec], hT, wbufs, ec, evac, (0, 1, 2, 3))
                if dump == "U" and ec == 0:
                    dump_buf(U[:, 8:8 + S_LEN], 2048, 0, [("U", ts) for ts in range(4)])
                    S.barrier()
                    return
                k = g + 1
                half = 1 << g
                w_ = 2 * half
                bufs = [U, B1, B2]
                src = U
                src_r = [("U", ts) for ts in range(4)] + ["Upad"]
                cur = None
                for step in range(k):
                    sh = 1 << step
                    dst = B1 if (step % 2 == 0) else B2
                    dres = "B1" if (step % 2 == 0) else "B2"
                    n = L - 8 if step > 0 else L - 1
                    n = L - sh - (0 if step == 0 else 0)
                    nn = L - sh
                    eng = "dve" if step % 2 == 0 else "pool"
                    S.add(eng, lambda e, dst=dst, src=src, sh=sh, nn=nn: e.tensor_tensor(out=dst[:, 0:nn], in0=src[:, 0:nn], in1=src[:, sh:sh + nn], op=ALU.add),
                          r=src_r + ["Bpad"], w=[dres])
                    src = dst
                    src_r = [dres]
                off = 8 - half
                cc = ec % 3
                S.add("dve", lambda e, src=src, off=off, w_=w_, cc=cc: e.scalar_tensor_tensor(
                    out=pooled[:, cc, :], in0=src[:, off:off + S_LEN], scalar=1.0 / w_, in1=U[:, 8:8 + S_LEN],
                    op0=ALU.mult, op1=ALU.subtract), r=src_r + [("U", ts) for ts in range(4)], w=[("pooled", cc)])
                for (c0, pe0) in ((0, 0), (S_LEN - 8, 8)):
                    S.add("dve", lambda e, src=src, off=off, c0=c0, pe0=pe0, g=g: e.tensor_tensor(
                        out=tmp8[:, 0:8], in0=src[:, off + c0:off + c0 + 8], in1=pedge[:, g, pe0:pe0 + 8], op=ALU.mult),
                        r=src_r + ["_pedge"], w=["tmp8"])
                    S.add("dve", lambda e, c0=c0, cc=cc: e.tensor_tensor(
                        out=pooled[:, cc, c0:c0 + 8], in0=tmp8[:, 0:8], in1=U[:, 8 + c0:16 + c0], op=ALU.subtract),
                        r=["tmp8"] + [("U", ts) for ts in range(4)], w=[("pooled", cc)])
                if cc == 2:
                    for oc in range(3):
                        bi = cat_n[0] % 2
                        cat_n[0] += 1
                        for ts in range(4):
                            bank = 4 + (ts % 2)

                            def mm(e, oc=oc, ts=ts, bank=bank, g=g):
                                last = None
                                for c3 in range(3):
                                    last = e.matmul(psb[bank][:, :], wgrp[:, g, c3 * 384 + oc * 128:c3 * 384 + (oc + 1) * 128],
                                                    pooled[:, c3, ts * 512:(ts + 1) * 512], start=(c3 == 0), stop=(c3 == 2))
                                return last
                            S.add("pe", mm, r=["_wgrp"] + [("pooled", c3) for c3 in range(3)], w=[PS(bank)])
                            col = g * 3 + oc
                            S.add("act", lambda e, bi=bi, ts=ts, bank=bank, col=col: e.mul(
                                out=cst[bi][:, ts * 512:(ts + 1) * 512], in_=psb[bank][:, :], mul=pscale[:, col:col + 1]),
                                r=[PS(bank), "_pscale"], w=[("cst", bi)])
                        store_cat(g * 3 + oc, bi)
            else:
                h = ec - 12
                qb = qx[h % 2]

                def evac(ts, bank, qb=qb, h=h):
                    S.add("act", lambda e, ts=ts, bank=bank: e.copy(out=qb[:, ts * 512:(ts + 1) * 512], in_=psb[bank][:, :]),
                          r=[PS(bank)], w=[("qx", h % 2)])
                proj_chunk(win0_d[ec], hT, wbufs, ec, evac, (0, 1, 2, 3))
                bi = cat_n[0] % 2
                cat_n[0] += 1
                xattn_head(h, qb, ("qx", h % 2), kT, V, cst[bi], ("cst", bi), 6, 7)
                store_cat(12 + h, bi)
        S.barrier()
        A.reset(m0)

    def stage_outproj(layer, src_d):
        m0 = A.mark()
        catT = A.alloc([16, S_LEN], BF)
        wo = A.alloc([16, D], BF)
        xts = [A.alloc([D], F32) for _ in range(2)]
        for ec in range(16):
            dma("sp", catT[:, ec, :], cat_d[ec], [("catd", ec)], [("catT", ec)], ("catl", ec % 4))
            dma("pool", wo[:, ec, :], wout_d[layer, ec], [], [("wo", ec)], ("wol", ec % 4))
        for t in range(NT):
            b = t % 2
            dma("sp", xts[b], src_d[t * 128:(t + 1) * 128, :], [], [("xo", b)], ("xo", b))
            for ds in range(4):
                bank = 4 * b + ds

                def mm(e, t=t, ds=ds, bank=bank):
                    last = None
                    for ec in range(16):
                        last = e.matmul(psb[bank][:, :], catT[:, ec, t * 128:(t + 1) * 128], wo[:, ec, ds * 512:(ds + 1) * 512],
                                        start=(ec == 0), stop=(ec == 15))
                    return last
                S.add("pe", mm, r=[("catT", ec) for ec in range(16)] + [("wo", ec) for ec in range(16)], w=[PS(bank)])
                S.add("dve", lambda e, b=b, ds=ds, bank=bank: e.tensor_tensor(
                    out=xts[b][:, ds * 512:(ds + 1) * 512], in0=psb[bank][:, :], in1=xts[b][:, ds * 512:(ds + 1) * 512], op=ALU.add),
                    r=[PS(bank), ("xo", b)], w=[("xo", b)])
            dma("sp", xres_d[t * 128:(t + 1) * 128, :], xts[b], [("xo", b)], [("xres", t)], ("xost", b))
        S.barrier()
        A.reset(m0)

    def stage_router(layer, pre=None):
        m0 = A.mark()
        if pre is not None:
            pre()
        xts = [A.alloc([D], F32) for _ in range(2)]
        hms = [A.alloc([D], BF) for _ in range(2)]
        junk = A.alloc([D], BF)
        hTt = [A.alloc([16, 128], BF) for _ in range(2)]
        wr = A.alloc([16, 16], BF)
        affT = A.alloc([S_LEN], F32)
        wkb = A.alloc([S_LEN], F32)
        vals = A.alloc([CAP], F32)
        idxu = A.alloc([CAP], U32)
        idxf = A.alloc([CAP], F32)
        lg = [A.alloc([16], F32) for _ in range(2)]
        load_gain(1 + 2 * layer)
        dma("pool", wr, wr_d[layer].rearrange("p (a b) -> p a b", a=16), [], ["_wr"], "wr")
        for t in range(NT):
            b = t % 2
            dma("sp", xts[b], xres_d[t * 128:(t + 1) * 128, :], [("xres", t)], [("xt", b)], ("xt", b))
            norm_tile(xts[b], ("xt", b), hms[b], ("hm", b), junk, 2 * b)
            dma("sp", hrow_d[t * 128:(t + 1) * 128, :], hms[b], [("hm", b)], ["hrow"], ("hst", b))
            transpose_tile(hms[b], ("hm", b), hTt[b], ("hTt", b), (2 * b, 2 * b + 1), evac=("act" if b == 0 else "dve"))
            bank = 4 + b

            def mm(e, b=b, bank=bank):
                last = None
                for dc in range(16):
                    last = e.matmul(psb[bank][:, 0:16], hTt[b][:, dc, :], wr[:, dc, :], start=(dc == 0), stop=(dc == 15))
                return last
            S.add("pe", mm, r=[(("hTt", b), 0), (("hTt", b), 1), "_wr"], w=[PS(bank)])
            mx = stat[:, 16 + b:17 + b]
            sm = stat[:, 18 + b:19 + b]
            S.add("dve", lambda e, bank=bank, mx=mx: e.tensor_reduce(out=mx, in_=psb[bank][:, 0:16], axis=AX.X, op=ALU.max),
                  r=[PS(bank)], w=[("rs", b)])
            S.add("dve", lambda e, mx=mx: e.tensor_scalar(out=mx, in0=mx, scalar1=-1.0, scalar2=None, op0=ALU.mult),
                  r=[("rs", b)], w=[("rs", b)])
            S.add("act", lambda e, b=b, bank=bank, mx=mx, sm=sm: e.activation(out=lg[b], in_=psb[bank][:, 0:16], func=AF.Exp, bias=mx, scale=1.0, accum_out=sm),
                  r=[PS(bank), ("rs", b)], w=[("lg", b), ("rsm", b)])
            S.add("dve", lambda e, sm=sm: e.reciprocal(out=sm, in_=sm), r=[("rsm", b)], w=[("rsm", b)])
            S.add("dve", lambda e, b=b, sm=sm: e.tensor_scalar(out=lg[b], in0=lg[b], scalar1=sm, scalar2=None, op0=ALU.mult),
                  r=[("lg", b), ("rsm", b)], w=[("lg", b)])
            tb = 6 + b
            S.add("pe", lambda e, b=b, tb=tb: e.transpose(out=psb[tb][0:16, 0:128], in_=lg[b], identity=identf),
                  r=[("lg", b), "_identf"], w=[PS(tb)])
            S.add("act", lambda e, t=t, tb=tb: e.copy(out=affT[0:16, t * 128:(t + 1) * 128], in_=psb[tb][0:16, 0:128]),
                  r=[PS(tb)], w=["affT"])
        cur = affT
        for it in range(CAP // 8):
            v8 = vals[0:16, it * 8:it * 8 + 8]
            i8 = idxu[0:16, it * 8:it * 8 + 8]
            S.add("dve", lambda e, v8=v8, cur=cur: e.max(out=v8, in_=cur[0:16, :]), r=["affT", "wkb"], w=[("v8", it)])
            S.add("dve", lambda e, v8=v8, i8=i8, cur=cur: e.max_index(out=i8, in_max=v8, in_values=cur[0:16, :]),
                  r=["affT", "wkb", ("v8", it)], w=[("i8", it)])
            if it < CAP // 8 - 1:
                S.add("dve", lambda e, v8=v8, cur=cur: e.match_replace(out=wkb[0:16, :], in_to_replace=v8, in_values=cur[0:16, :], imm_value=-1.0),
                      r=["affT", ("v8", it)], w=["wkb"])
                cur = wkb
        S.add("dve", lambda e: e.tensor_copy(out=idxf[0:16, :], in_=idxu[0:16, :]), r=[("i8", it) for it in range(CAP // 8)], w=["idxf"])
        for ct in range(2):
            S.add("pe", lambda e, ct=ct: e.transpose(out=psb[0][:, ct * 16:(ct + 1) * 16], in_=idxf[0:16, ct * 128:(ct + 1) * 128], identity=identf[0:16, 0:16]),
                  r=["idxf", "_identf"], w=[(PS(0), ct)])
            S.add("pe", lambda e, ct=ct: e.transpose(out=psb[1][:, ct * 16:(ct + 1) * 16], in_=vals[0:16, ct * 128:(ct + 1) * 128], identity=identf[0:16, 0:16]),
                  r=[("v8", it) for it in range(CAP // 8)] + ["_identf"], w=[(PS(1), ct)])
        S.add("dve", lambda e: e.tensor_copy(out=idxT.rearrange("p a b -> p (a b)"), in_=psb[0][:, 0:32]), r=[(PS(0), 0), (PS(0), 1)], w=["idxT"])
        S.add("dve", lambda e: e.tensor_copy(out=gT.rearrange("p a b -> p (a b)"), in_=psb[1][:, 0:32]), r=[(PS(1), 0), (PS(1), 1)], w=["gT"])
        if dump == "rt":
            dump_buf(idxT.rearrange("p a b -> p (a b)"), 32, 0, ["idxT"])
            dump_buf(gT.rearrange("p a b -> p (a b)"), 32, 32, ["gT"])
        S.barrier()
        A.reset(m0)

    def stage_moe(layer):
        m0 = A.mark()
        NBIG, NSM = 4, 5
        big = [A.alloc([16, 512], BF) for _ in range(NBIG)]
        sml = [A.alloc([8, 512], BF) for _ in range(NSM)]
        nb = [0]
        ns = [0]
        slots_of = {}

        def load_big(e_):
            slots = slots_of.setdefault(e_, {})
            for fs in range(2):
                for nm, src in (("g", wg_d), ("u", wu_d)):
                    bi = nb[0] % NBIG
                    nb[0] += 1
                    dma("pool", big[bi], src[layer, e_, fs].rearrange("p (a b) -> p a b", a=16), [], [("big", bi)], ("big", bi))
                    slots[(nm, fs)] = bi

        def load_sml(e_):
            slots = slots_of.setdefault(e_, {})
            for ds in range(4):
                si = ns[0] % NSM
                ns[0] += 1
                dma("pool", sml[si], wd_d[layer, e_, ds].rearrange("p (a b) -> p a b", a=8), [], [("sml", si)], ("sml", si))
                slots[("d", ds)] = si

        stage_router(layer, pre=lambda: (load_big(0), load_sml(0)))
        xg = [[A.alloc([D], BF) for _ in range(2)] for _ in range(2)]
        xgT = [A.alloc([16, CAP], BF) for _ in range(2)]
        act_tm = A.alloc([2, DFF], BF)
        actT = A.alloc([8, CAP], BF)
        sa = [A.alloc([512], F32) for _ in range(2)]
        yo = [A.alloc([D], F32) for _ in range(2)]

        def gather(e_):
            pb = e_ % 2
            for ct in range(2):
                S.add("pool", lambda e, pb=pb, ct=ct, e_=e_: e.indirect_dma_start(
                    out=xg[pb][ct], out_offset=None, in_=hrow_d,
                    in_offset=bass.IndirectOffsetOnAxis(ap=idxT[:, ct, e_:e_ + 1], axis=0)),
                    r=["hrow", "idxT"], w=[("xg", pb, ct)], dma=("xg", pb, ct))

        gather(0)
        for e_ in range(NE):
            pb = e_ % 2
            slots = slots_of[e_]
            if e_ + 1 < NE:
                gather(e_ + 1)
            for ct in range(2):
                transpose_tile(xg[pb][ct], ("xg", pb, ct), xgT[pb][:, :, ct * 128:(ct + 1) * 128], ("xgT", pb, ct),
                               (6, 7), evac=("act" if ct == 0 else "dve"))
            xg_r = [(("xgT", pb, ct), h) for ct in range(2) for h in range(2)]
            for fs in range(2):
                bg = slots[("g", fs)]
                bu = slots[("u", fs)]
                for ct in range(2):
                    pa = ct
                    pu = 2 + ct

                    def mm(e, bw, bank, ct=ct, pb=pb):
                        last = None
                        for dc in range(16):
                            last = e.matmul(psb[bank][:, :], xgT[pb][:, dc, ct * 128:(ct + 1) * 128], big[bw][:, dc, :],
                                            start=(dc == 0), stop=(dc == 15))
                        return last
                    S.add("pe", lambda e, bg=bg, pa=pa, mm=mm: mm(e, bg, pa), r=xg_r + [("big", bg)], w=[PS(pa)])
                    S.add("pe", lambda e, bu=bu, pu=pu, mm=mm: mm(e, bu, pu), r=xg_r + [("big", bu)], w=[PS(pu)])
                    S.add("act", lambda e, ct=ct, pa=pa: e.activation(out=sa[ct], in_=psb[pa][:, :], func=AF.Silu),
                          r=[PS(pa)], w=[("sa", ct)])
                    S.add("dve", lambda e, ct=ct, pu=pu, fs=fs: e.tensor_tensor(out=act_tm[:, ct, fs * 512:(fs + 1) * 512], in0=sa[ct], in1=psb[pu][:, :], op=ALU.mult),
                          r=[("sa", ct), PS(pu)], w=[("act_tm", ct, fs)])
            if e_ + 1 < NE:
                load_big(e_ + 1)
            for ct in range(2):
                pv = psbf(6 + ct)[:, :].rearrange("p (a b) -> p a b", a=8)

                def tr(e, ct=ct, pv=pv):
                    last = None
                    for fc in range(8):
                        last = e.transpose(out=pv[:, fc, :], in_=act_tm[:, ct, fc * 128:(fc + 1) * 128], identity=identb)
                    return last
                S.add("pe", tr, r=[("act_tm", ct, 0), ("act_tm", ct, 1), "_identb"], w=[PS(6 + ct)])
                if ct == 0:
                    S.add("act", lambda e, ct=ct, pv=pv: e.copy(out=actT[:, :, ct * 128:(ct + 1) * 128], in_=pv), r=[PS(6 + ct)], w=[("actT", ct)])
                else:
                    S.add("dve", lambda e, ct=ct, pv=pv: e.tensor_copy(out=actT[:, :, ct * 128:(ct + 1) * 128], in_=pv), r=[PS(6 + ct)], w=[("actT", ct)])
            k = 0
            for ds in range(4):
                si = slots[("d", ds)]
                for ct in range(2):
                    bank = 4 + (k % 2)
                    k += 1

                    def mmd(e, ct=ct, si=si, bank=bank):
                        last = None
                        for fc in range(8):
                            last = e.matmul(psb[bank][:, :], actT[:, fc, ct * 128:(ct + 1) * 128], sml[si][:, fc, :],
                                            start=(fc == 0), stop=(fc == 7))
                        return last
                    S.add("pe", mmd, r=[("actT", 0), ("actT", 1), ("sml", si)], w=[PS(bank)])
                    if ct == 0:
                        S.add("act", lambda e, ct=ct, ds=ds, bank=bank, e_=e_: e.mul(
                            out=yo[ct][:, ds * 512:(ds + 1) * 512], in_=psb[bank][:, :], mul=gT[:, ct, e_:e_ + 1]),
                            r=[PS(bank), "gT"], w=[("yo", ct)])
                    else:
                        S.add("dve", lambda e, ct=ct, ds=ds, bank=bank, e_=e_: e.tensor_scalar(
                            out=yo[ct][:, ds * 512:(ds + 1) * 512], in0=psb[bank][:, :], scalar1=gT[:, ct, e_:e_ + 1], scalar2=None, op0=ALU.mult),
                            r=[PS(bank), "gT"], w=[("yo", ct)])
            if e_ + 1 < NE:
                load_sml(e_ + 1)
            for ct in range(2):
                S.add("pool", lambda e, ct=ct, e_=e_: e.indirect_dma_start(
                    out=xres_d, out_offset=bass.IndirectOffsetOnAxis(ap=idxT[:, ct, e_:e_ + 1], axis=0),
                    in_=yo[ct], in_offset=None, compute_op=ALU.add),
                    r=[("yo", ct), "idxT"], w=["xres_all"], dma=("sc", ct))
        S.barrier()
        A.reset(m0)

    def stage_mixer1():
        m0 = A.mark()
        hT = A.alloc([16, S_LEN], BF)
        kT = A.alloc([4, MEM], BF)
        V = A.alloc([2, 512], BF)
        Ct = A.alloc([S_LEN], F32)
        St = A.alloc([S_LEN], F32)
        invf = A.alloc([2], F32)
        rotT = A.alloc([128], F32)
        stage_norm_hT(xres_d, 2, hT)
        m1 = A.mark()
        posi = A.alloc([S_LEN], I32)
        posf = A.alloc([S_LEN], F32)
        dma("sp", posi, pos_d.partition_broadcast(128), [], ["posi"], "posi")
        dma("sp", invf, cinvf_d, [], ["_invf"], "c5")
        dma("sp", rotT, crot_d, [], ["_rotT"], "c6")
        S.add("dve", lambda e: e.tensor_copy(out=posf, in_=posi), r=["posi"], w=["posf"])
        ki = A.alloc([S_LEN], I32)
        mk = A.alloc([S_LEN], F32)
        SC = 6.283179

        def reduce_(T, res):
            S.add("dve", lambda e: e.tensor_copy(out=ki, in_=T), r=[res], w=["ki"])
            S.add("dve", lambda e: e.tensor_copy(out=mk, in_=ki), r=["ki"], w=["mk"])
            S.add("dve", lambda e: e.tensor_tensor(out=T, in0=T, in1=mk, op=ALU.subtract), r=[res, "mk"], w=[res])
            S.add("dve", lambda e: e.tensor_scalar(out=mk, in0=T, scalar1=0.5, scalar2=None, op0=ALU.is_gt), r=[res], w=["mk"])
            S.add("dve", lambda e: e.tensor_tensor(out=T, in0=T, in1=mk, op=ALU.subtract), r=[res, "mk"], w=[res])
            S.add("dve", lambda e: e.tensor_scalar(out=mk, in0=T, scalar1=-0.5, scalar2=None, op0=ALU.is_lt), r=[res], w=["mk"])
            S.add("dve", lambda e: e.tensor_tensor(out=T, in0=T, in1=mk, op=ALU.add), r=[res, "mk"], w=[res])
            S.add("act", lambda e: e.activation(out=T, in_=T, func=AF.Sin, scale=SC), r=[res], w=[res])
        S.add("dve", lambda e: e.tensor_scalar(out=St, in0=posf, scalar1=invf[:, 0:1], scalar2=None, op0=ALU.mult),
              r=["posf", "_invf"], w=["St"])
        S.add("dve", lambda e: e.tensor_scalar(out=Ct, in0=posf, scalar1=invf[:, 0:1], scalar2=0.25, op0=ALU.mult, op1=ALU.add),
              r=["posf", "_invf"], w=["Ct"])
        reduce_(St, "St")
        reduce_(Ct, "Ct")
        S.add("dve", lambda e: e.tensor_scalar(out=St, in0=St, scalar1=invf[:, 1:2], scalar2=None, op0=ALU.mult), r=["St", "_invf"], w=["St"])
        S.add("dve", lambda e: e.tensor_scalar(out=Ct, in0=Ct, scalar1=-1.0, scalar2=invf[:, 1:2], op0=ALU.add, op1=ALU.mult), r=["Ct", "_invf"], w=["Ct"])
        S.add("dve", lambda e: e.tensor_scalar(out=Ct, in0=Ct, scalar1=1.0, scalar2=None, op0=ALU.add), r=["Ct"], w=["Ct"])
        S.barrier()
        A.reset(m1)
        mem_kv(1, kT, V)
        wbufs = [A.alloc([16, 128], BF) for _ in range(3)]
        wv = A.alloc([16, 192], BF)
        v_tm = A.alloc([NT, 192], BF)
        kT2 = A.alloc([3, S_LEN], BF)
        qf = A.alloc([S_LEN], F32)
        t1 = A.alloc([512], F32)
        qTb = [A.alloc([S_LEN], BF) for _ in range(2)]
        mask = A.alloc([2, 384], BF)
        pb_ = [A.alloc([2, 384], BF) for _ in range(2)]
        pm_ = [A.alloc([2, 384], BF) for _ in range(2)]
        pTs = [A.alloc([6, 128], BF) for _ in range(2)]
        otm = [A.alloc([128], BF) for _ in range(2)]
        cst = [A.alloc([S_LEN], BF) for _ in range(2)]
        qx = [A.alloc([S_LEN], BF) for _ in range(2)]
        dma("sp", mask, cmask_d.rearrange("p (a b) -> p a b", a=2), [], ["_mask"], "c7")
        dma("pool", wv, wv1_d.rearrange("p (a b) -> p a b", a=16), [], ["_wv"], "wv")
        hT_all = [(("hT", t), h) for t in range(NT) for h in range(2)]
        for t in range(NT):
            bank = t % 2

            def mm(e, t=t, bank=bank):
                last = None
                for dc in range(16):
                    last = e.matmul(psb[bank][:, 0:192], hT[:, dc, t * 128:(t + 1) * 128], wv[:, dc, :], start=(dc == 0), stop=(dc == 15))
                return last
            S.add("pe", mm, r=["_wv", (("hT", t), 0), (("hT", t), 1)], w=[PS(bank)])
            S.add("act", lambda e, t=t, bank=bank: e.copy(out=v_tm[:, t, :], in_=psb[bank][:, 0:192]), r=[PS(bank)], w=[("v_tm", t)])

        def rotary_chunk(dst, dst_res):
            for ts in range(4):
                for hh in range(2):
                    c0 = ts * 512 + hh * 256
                    S.add("pe", lambda e, c0=c0: e.matmul(psb[2][:, 0:256], rotT, qf[:, c0:c0 + 256], start=True, stop=True),
                          r=[("qf", ts), "_rotT"], w=[(PS(2), 0)])
                    S.add("dve", lambda e, c0=c0: e.tensor_tensor(out=t1[:, 0:256], in0=psb[2][:, 0:256], in1=St[:, c0:c0 + 256], op=ALU.mult),
                          r=[(PS(2), 0), "St"], w=["t1a"])
                    S.add("pool", lambda e, c0=c0: e.tensor_tensor(out=t1[:, 256:512], in0=qf[:, c0:c0 + 256], in1=Ct[:, c0:c0 + 256], op=ALU.mult),
                          r=[("qf", ts), "Ct"], w=["t1b"])
                    S.add("dve", lambda e, c0=c0: e.tensor_tensor(out=dst[:, c0:c0 + 256], in0=t1[:, 0:256], in1=t1[:, 256:512], op=ALU.add),
                          r=["t1a", "t1b"], w=[dst_res])

        def evac_qf(ts, bank):
            S.add("act", lambda e, ts=ts, bank=bank: e.copy(out=qf[:, ts * 512:(ts + 1) * 512], in_=psb[bank][:, :]),
                  r=[PS(bank)], w=[("qf", ts)])

        ci = [0]
        for g in range(3):
            proj_chunk(win1_d[g], hT, wbufs, ci[0], evac_qf, (0, 1))
            ci[0] += 1
            rotary_chunk(kT2[:, g, :], ("kT2", g))
        cat_n = [0]

        def store_cat(chunk, buf_i):
            dma("sp", cat_d[chunk], cst[buf_i], [("cst", buf_i)], [("catd", chunk)], ("cstst", buf_i))

        NQ = 12 * NT

        def geom(q):
            j, n = q // NT, q % NT
            kb0 = max(n - 1, 0)
            kb1 = min(n + 1, NT - 1)
            nk = (kb1 - kb0 + 1) * 128
            moff = 0 if n > 0 else 128
            return j, n, j // 4, kb0, nk, moff

        def st_cols(q):
            i4 = q % 4
            base = 24 + 6 * i4
            return stat[:, base:base + 2], stat[:, base + 2:base + 4], stat[:, base + 4:base + 6]

        def ph0(q):
            j, n, g, kb0, nk, moff = geom(q)
            ib = q % 2
            i4 = q % 4
            qb = qTb[j % 2]
            qres = ("qTb", j % 2)
            sb0 = 3 if ib == 0 else 0
            mx, sm, es_ = st_cols(q)
            for hh in range(2):
                def qk(e, hh=hh):
                    e.matmul(psb[sb0 + hh][:, 0:nk], qb[hh * 64:(hh + 1) * 64, n * 128:(n + 1) * 128],
                             kT2[hh * 64:(hh + 1) * 64, g, kb0 * 128:kb0 * 128 + nk], start=True, stop=False)
                    return e.matmul(psb[sb0 + hh][:, 0:nk], identb, mask[:, 0, moff:moff + nk], start=False, stop=True)
                S.add("pe", qk, r=[qres, ("kT2", g), "_mask", "_identb"], w=[PS(sb0 + hh)])
            for hh in range(2):
                S.add("dve", lambda e, hh=hh: e.tensor_reduce(out=mx[:, hh:hh + 1], in_=psb[sb0 + hh][:, 0:nk], axis=AX.X, op=ALU.max),
                      r=[PS(sb0 + hh)], w=[("amx", i4, hh)])
            S.add("dve", lambda e: e.scalar_tensor_tensor(out=mx, in0=mx, scalar=-0.125, in1=nsink[:, 2 * j:2 * j + 2], op0=ALU.mult, op1=ALU.min),
                  r=[("amx", i4, 0), ("amx", i4, 1), "_nsink"], w=[("anm", i4)])
            S.add("dve", lambda e: e.tensor_tensor(out=es_, in0=mx, in1=nsink[:, 2 * j:2 * j + 2], op=ALU.subtract),
                  r=[("anm", i4), "_nsink"], w=[("aes", i4)])
            for hh in range(2):
                S.add("act", lambda e, hh=hh: e.activation(
                    out=pm_[ib][:, hh, 0:nk], in_=psb[sb0 + hh][:, 0:nk], func=AF.Exp, bias=mx[:, hh:hh + 1], scale=0.125,
                    accum_out=sm[:, hh:hh + 1]),
                    r=[PS(sb0 + hh), ("anm", i4)], w=[("apm", ib, hh), ("asm", i4, hh)])
            S.add("act", lambda e: e.activation(out=es_, in_=es_, func=AF.Exp), r=[("aes", i4)], w=[("aes", i4)])

        def ph1(q):
            j, n, g, kb0, nk, moff = geom(q)
            ib = q % 2
            i4 = q % 4
            nkb = nk // 128
            mx, sm, es_ = st_cols(q)
            S.add("dve", lambda e: e.tensor_tensor(out=sm, in0=sm, in1=es_, op=ALU.add),
                  r=[("aes", i4), ("asm", i4, 0), ("asm", i4, 1)], w=[("ard", i4)])
            S.add("dve", lambda e: e.reciprocal(out=sm, in_=sm), r=[("ard", i4)], w=[("ard", i4)])
            pv = psbf(7)[:, 0:768].rearrange("p (a b) -> p a b", a=6)

            def tr(e):
                last = None
                for hh in range(2):
                    for kb in range(nkb):
                        last = e.transpose(out=pv[:, hh * 3 + kb, :], in_=pm_[ib][:, hh, kb * 128:(kb + 1) * 128], identity=identb)
                return last
            S.add("pe", tr, r=[("apm", ib, 0), ("apm", ib, 1), "_identb"], w=[(PS(7), 0)])
            if nkb == 3:
                S.add("act", lambda e: e.copy(out=pTs[ib], in_=pv), r=[(PS(7), 0)], w=[("apT", ib)])
            else:
                for hh in range(2):
                    S.add("act", lambda e, hh=hh: e.copy(out=pTs[ib][:, hh * 3:hh * 3 + nkb, :], in_=pv[:, hh * 3:hh * 3 + nkb, :]),
                          r=[(PS(7), 0)], w=[("apT", ib)])

        def ph2(q):
            j, n, g, kb0, nk, moff = geom(q)
            ib = q % 2
            i4 = q % 4
            nkb = nk // 128
            mx, sm, es_ = st_cols(q)
            po = psb[5][:, 0:128]

            def pvm(e):
                last = None
                for hh in range(2):
                    for kb in range(nkb):
                        last = e.matmul(po[:, hh * 64:(hh + 1) * 64], pTs[ib][:, hh * 3 + kb, :], v_tm[:, kb0 + kb, g * 64:(g + 1) * 64],
                                        start=(kb == 0), stop=(kb == nkb - 1))
                return last
            S.add("pe", pvm, r=[("apT", ib)] + [("v_tm", kb0 + kb) for kb in range(nkb)], w=[PS(5)])
            for hh in range(2):
                S.add("act", lambda e, hh=hh: e.mul(
                    out=otm[ib][:, hh * 64:(hh + 1) * 64], in_=po[:, hh * 64:(hh + 1) * 64], mul=sm[:, hh:hh + 1]),
                    r=[PS(5), ("ard", i4)], w=[("otm", ib)])

        def ph3(q):
            j, n, g, kb0, nk, moff = geom(q)
            ib = q % 2
            bi = j % 2
            ft = psbf(6)[:, 0:128]
            S.add("pe", lambda e: e.transpose(out=ft, in_=otm[ib], identity=identb), r=[("otm", ib), "_identb"], w=[PS(6)])
            S.add("dve", lambda e: e.tensor_copy(out=cst[bi][:, n * 128:(n + 1) * 128], in_=ft), r=[PS(6)], w=[("cst", bi)])
            if n == NT - 1:
                store_cat(j, bi)

        for s_ in range(NQ + 3):
            if s_ < NQ and s_ % NT == 0:
                j = s_ // NT
                proj_chunk(win1_d[3 + j], hT, wbufs, ci[0], evac_qf, (0, 1))
                ci[0] += 1
                rotary_chunk(qTb[j % 2], ("qTb", j % 2))
            if s_ < NQ:
                ph0(s_)
            if 0 <= s_ - 1 < NQ:
                ph1(s_ - 1)
            if 0 <= s_ - 2 < NQ:
                ph2(s_ - 2)
            if 0 <= s_ - 3 < NQ:
                ph3(s_ - 3)
        cat_n[0] = 12
        for h in range(4):
            qb = qx[h % 2]

            def evac(ts, bank, qb=qb, h=h):
                S.add("act", lambda e, ts=ts, bank=bank: e.copy(out=qb[:, ts * 512:(ts + 1) * 512], in_=psb[bank][:, :]),
                      r=[PS(bank)], w=[("qx", h % 2)])
            proj_chunk(win1_d[15 + h], hT, wbufs, ci[0], evac, (0, 1))
            ci[0] += 1
            bi = cat_n[0] % 2
            cat_n[0] += 1
            xattn_head(h, qb, ("qx", h % 2), kT, V, cst[bi], ("cst", bi), 5, 6)
            store_cat(12 + h, bi)
        S.barrier()
        A.reset(m0)

    def stage_final():
        m0 = A.mark()
        xts = [A.alloc([D], F32) for _ in range(2)]
        outs = [A.alloc([D], F32) for _ in range(2)]
        junk = A.alloc([D], BF)
        load_gain(5)
        for t in range(NT):
            b = t % 2
            dma("sp", xts[b], xres_d[t * 128:(t + 1) * 128, :], [], [("xt", b)], ("xt", b))
            norm_tile(xts[b], ("xt", b), outs[b], ("of", b), junk, 2 * b)
            dma("sp", y_d[t * 128:(t + 1) * 128, :], outs[b], [("of", b)], ["y"], ("yst", b))
        A.reset(m0)

    stages = [
        ("mem", stage_mem),
        ("mix0", stage_mixer0),
        ("out0", lambda: stage_outproj(0, x_d)),
        ("ex0", lambda: stage_moe(0)),
        ("mix1", stage_mixer1),
        ("out1", lambda: stage_outproj(1, xres_d)),
        ("ex1", lambda: stage_moe(1)),
        ("final", stage_final),
    ]
    for name, fn in stages:
        if only is not None and name not in only:
            continue
        fn()
        if stop_after == name:
            break
    if stop_after is not None and stop_after != "final":
        S.barrier()
    finals = [k for k in S.dma_cnt if isinstance(k, tuple) and k[0] in ("yst", "xost", "cstst", "sc")]
    S.emit(es, final_dma_keys=finals)
    es.close()
    return nc


def _consts():
    identb = np.eye(128, dtype=np.float32).astype(ml_dtypes.bfloat16)
    identf = np.eye(128, dtype=np.float32)
    rot = np.zeros((128, 128), np.float32)
    for b in (0, 64):
        for r in range(8):
            rot[b + r + 8, b + r] = -1.0
            rot[b + r, b + r + 8] = 1.0
    inv_freq = (500000.0 ** (-np.arange(0, 16, 2, dtype=np.float32) / 16)).astype(np.float32)
    invf = np.zeros((128, 2), np.float32)
    for p in range(128):
        r = p % 64
        if r < 16:
            invf[p, 0] = np.float32(inv_freq[r % 8] / (2.0 * np.pi))
            invf[p, 1] = 1.0
    qq = np.arange(128)[:, None]
    kk = np.arange(128)[None, :]
    m = np.concatenate([(kk >= qq), np.ones((128, 128), bool), (kk <= qq)], axis=1)
    m = np.where(m, 0.0, -30000.0).astype(np.float32)
    mask = np.concatenate([m, m], axis=1).astype(ml_dtypes.bfloat16)
    pedge = np.zeros((4, 16), np.float32)
    t = np.arange(S_LEN)
    for g, w in enumerate((2, 4, 8, 16)):
        lo = np.maximum(t - w // 2, 0)
        hi = np.minimum(t + w // 2 - 1, S_LEN - 1)
        cnt = (hi - lo + 1).astype(np.float32)
        pedge[g, 0:8] = 1.0 / cnt[0:8]
        pedge[g, 8:16] = 1.0 / cnt[S_LEN - 8:]
    pedge = np.broadcast_to(pedge.reshape(1, 64), (128, 64)).copy()
    return identb, identf, rot, invf, mask, pedge


def _pieces(w, pc):
    R, C = w.shape
    n = R // 128
    return np.ascontiguousarray(w.reshape(n, 128, C // pc, pc).transpose(2, 1, 0, 3)).reshape(C // pc, 128, n * pc)


def _host_layout(inp, slim=False):
    f = lambda a: np.ascontiguousarray(np.asarray(a, dtype=np.float32))
    shared = {}
    shared["gains"] = np.stack([f(inp["norm_mix_g"])[0], f(inp["norm_ffn_g"])[0], f(inp["norm_mix_g"])[1],
                                f(inp["norm_ffn_g"])[1], f(inp["mem_norm_g"]), f(inp["final_g"])], axis=0)
    wkv = f(inp["mem_w_kv"])
    shared["wkv"] = np.ascontiguousarray(wkv.reshape(2, 4, 4, 128, 1024).transpose(0, 1, 3, 2, 4)).reshape(2, 4, 128, 4096)
    shared["win0"] = _pieces(f(inp["pool_w_in"])[0], 128)
    gw = f(inp["pool_group_w"])[0]
    shared["wgrp"] = np.ascontiguousarray(gw.reshape(4, 3, 128, 384).transpose(0, 2, 1, 3)).reshape(4, 128, 1152)
    shared["pscale"] = np.ascontiguousarray(f(inp["pool_scale"])[0].reshape(12, 128).T)
    shared["wout"] = np.stack([f(inp["pool_w_out"])[0].reshape(16, 128, 2048), f(inp["attn_w_out"])[0].reshape(16, 128, 2048)], axis=0)
    w1 = f(inp["attn_w_in"])[0]
    kcols = []
    for g in range(3):
        kc = w1[:, 1536 + g * 64:1536 + (g + 1) * 64]
        kcols.append(np.concatenate([kc, kc], axis=1))
    w1c = np.concatenate(kcols + [w1[:, 0:1536], w1[:, 1920:2432]], axis=1)
    shared["win1"] = _pieces(w1c, 128)
    wv = w1[:, 1728:1920]
    shared["wv1"] = np.ascontiguousarray(wv.reshape(16, 128, 192).transpose(1, 0, 2)).reshape(128, 16 * 192)
    shared["nsink"] = np.ascontiguousarray(np.broadcast_to(-f(inp["attn_sink"])[0][None, :], (128, 24)))
    wr = f(inp["router_w"])
    shared["wr"] = np.ascontiguousarray(wr.reshape(2, 16, 128, 16).transpose(0, 2, 1, 3)).reshape(2, 128, 256)
    wg = inp["exp_w_gate"]
    wu = inp["exp_w_up"]
    wd = inp["exp_w_down"]
    if slim:
        wg, wu, wd = wg[:slim[0], :slim[1]], wu[:slim[0], :slim[1]], wd[:slim[0], :slim[1]]
    wg, wu, wd = f(wg), f(wu), f(wd)
    nl_, ne_ = wg.shape[0], wg.shape[1]
    shared["wg"] = np.ascontiguousarray(wg.reshape(nl_, ne_, 16, 128, 2, 512).transpose(0, 1, 4, 3, 2, 5)).reshape(nl_, ne_, 2, 128, 8192)
    shared["wu"] = np.ascontiguousarray(wu.reshape(nl_, ne_, 16, 128, 2, 512).transpose(0, 1, 4, 3, 2, 5)).reshape(nl_, ne_, 2, 128, 8192)
    shared["wd"] = np.ascontiguousarray(wd.reshape(nl_, ne_, 8, 128, 4, 512).transpose(0, 1, 4, 3, 2, 5)).reshape(nl_, ne_, 4, 128, 4096)
    identb, identf, rot, invf, mask, pedge = _consts()
    shared["c_identb"] = identb
    shared["c_identf"] = identf
    shared["c_rot"] = rot
    shared["c_invf"] = invf
    shared["c_mask"] = mask
    shared["c_pedge"] = pedge
    return shared


_NC_CACHE = {}


def kernel(x, mem, positions, norm_mix_g, norm_ffn_g, mem_norm_g, final_g, mem_w_kv,
           pool_w_in, pool_group_w, pool_scale, pool_w_out,
           attn_w_in, attn_sink, attn_w_out,
           router_w, exp_w_gate, exp_w_up, exp_w_down):
    inp = dict(norm_mix_g=norm_mix_g, norm_ffn_g=norm_ffn_g, mem_norm_g=mem_norm_g, final_g=final_g,
               mem_w_kv=mem_w_kv, pool_w_in=pool_w_in, pool_group_w=pool_group_w, pool_scale=pool_scale,
               pool_w_out=pool_w_out, attn_w_in=attn_w_in, attn_sink=attn_sink, attn_w_out=attn_w_out,
               router_w=router_w, exp_w_gate=exp_w_gate, exp_w_up=exp_w_up, exp_w_down=exp_w_down)
    shared = _host_layout(inp)
    x = np.asarray(x, dtype=np.float32)
    mem = np.asarray(mem, dtype=np.float32)
    positions = np.asarray(positions, dtype=np.int32)
    if "nc" not in _NC_CACHE:
        _NC_CACHE["nc"] = build()
    nc = _NC_CACHE["nc"]
    n = 8
    in_maps = []
    for b in range(n):
        m = dict(shared)
        m["x"] = np.ascontiguousarray(x[b])
        m["mem"] = np.ascontiguousarray(mem[b])
        m["pos"] = np.ascontiguousarray(positions[b].reshape(1, S_LEN))
        in_maps.append(m)
    res = run_bass_kernel_spmd(nc, in_maps, core_ids=list(range(n)))
    return np.stack([r["y"] for r in res.results], axis=0)
```

```python
import numpy as np
from contextlib import ExitStack
import ml_dtypes
import concourse.bass as bass
import concourse.mybir as mybir
from concourse.bass_utils import run_bass_kernel_spmd

dt = mybir.dt
F32 = dt.float32
BF = dt.bfloat16
I32 = dt.int32
U32 = dt.uint32
AF = mybir.ActivationFunctionType
ALU = mybir.AluOpType
AX = mybir.AxisListType

D = 2048
S_LEN = 2048
NT = 16
NDC = 16
MEM = 256
NE = 16
CAP = 256
DFF = 1024
EPS = 1e-6
ENGS = ("sp", "act", "pool", "dve", "pe")


STATS = {}


class Op:
    __slots__ = ("eng", "fn", "deps", "dma", "needed", "sem", "val", "idx")


class Sched:
    def __init__(self, nc):
        self.nc = nc
        self.ops = {e: [] for e in ENGS}
        self.last_w = {}
        self.readers = {}
        self.dma_cnt = {}
        self.dma_last = {}
        self.dma_prev = {}
        self.n = 0

    def _new(self, eng, fn, dma):
        op = Op()
        op.eng = eng
        op.fn = fn
        op.dma = dma
        op.needed = False
        op.sem = None
        op.val = 0
        op.idx = self.n
        op.deps = []
        self.n += 1
        return op

    def add(self, eng, fn, r=(), w=(), dma=None):
        op = self._new(eng, fn, dma)
        deps = {}
        for x in r:
            lw = self.last_w.get(x)
            if lw is not None:
                deps[lw.idx] = lw
        for x in w:
            lw = self.last_w.get(x)
            if lw is not None:
                deps[lw.idx] = lw
            for rd in self.readers.get(x, ()):
                deps[rd.idx] = rd
        best = {}
        out = []
        for d in deps.values():
            if d.dma is not None:
                out.append(d)
            else:
                if d.eng == "pe" and eng == "pe" and dma is None:
                    continue
                b = best.get(d.eng)
                if b is None or d.idx > b.idx:
                    best[d.eng] = d
        out.extend(best.values())
        if dma is not None:
            pv_ = self.dma_prev.get(dma)
            if pv_ is not None and all(pv_ is not d for d in out):
                out.append(pv_)
            self.dma_prev[dma] = op
        op.deps = out
        for d in out:
            d.needed = True
        for x in r:
            if not (isinstance(x, str) and x.startswith("_")):
                self.readers.setdefault(x, []).append(op)
        for x in w:
            self.last_w[x] = op
            self.readers[x] = []
        if dma is not None:
            c = self.dma_cnt.get(dma, 0) + 1
            self.dma_cnt[dma] = c
            op.val = 16 * c
            self.dma_last[dma] = op
        self.ops[eng].append(op)
        return op

    def barrier(self):
        lasts = []
        for e in ENGS:
            for op in reversed(self.ops[e]):
                if op.dma is None and op.fn is not None:
                    lasts.append(op)
                    break
        dl = list(self.dma_last.values())
        for e in ENGS:
            op = self._new(e, None, None)
            op.deps = [x for x in lasts] + dl
            for d in op.deps:
                d.needed = True
            self.ops[e].append(op)
        self.last_w = {}
        self.readers = {}
        self.dma_last = {}

    def emit(self, es, final_dma_keys=()):
        nc = self.nc
        esem = {e: es.enter_context(nc.semaphore("s_" + e)) for e in ENGS}
        dsem = {}
        for i, k in enumerate(self.dma_cnt):
            dsem[k] = es.enter_context(nc.semaphore("d%d" % i))
        for e in ENGS:
            c = 0
            for op in self.ops[e]:
                if op.dma is not None:
                    op.sem = dsem[op.dma]
                else:
                    op.sem = esem[e]
                    if op.needed:
                        c += 1
                        op.val = c
            STATS[e] = (c, len(self.ops[e]))
        STATS["dma_max"] = max(16 * v for v in self.dma_cnt.values())
        STATS["n_dma_keys"] = len(self.dma_cnt)
        ops = self.ops
        dma_cnt = self.dma_cnt

        def run(e, eng):
            waited = {}
            for op in ops[e]:
                for d in op.deps:
                    key = id(d.sem)
                    if waited.get(key, 0) < d.val:
                        eng.wait_ge(d.sem, d.val)
                        waited[key] = d.val
                if op.fn is None:
                    continue
                ins = op.fn(eng)
                if op.dma is not None:
                    ins.then_inc(op.sem, 16)
                elif op.needed:
                    ins.then_inc(op.sem, 1)
            if e == "sp":
                for k in final_dma_keys:
                    eng.wait_ge(dsem[k], 16 * dma_cnt[k])

        with nc.Block() as block:
            @block.sync
            def _(eng):
                run("sp", eng)

            @block.scalar
            def _(eng):
                run("act", eng)

            @block.gpsimd
            def _(eng):
                run("pool", eng)

            @block.vector
            def _(eng):
                run("dve", eng)

            @block.tensor
            def _(eng):
                run("pe", eng)


ARENA_BYTES = 206 * 1024


class Arena:
    def __init__(self, ap):
        self.ap = ap
        self.off = 0

    def mark(self):
        return self.off

    def reset(self, m):
        self.off = m

    def alloc(self, shape, dtype, parts=128):
        esz = {F32: 4, BF: 2, I32: 4, U32: 4}[dtype]
        n = 1
        for s in shape:
            n *= s
        nbytes = (n * esz + 31) // 32 * 32
        assert self.off + nbytes <= ARENA_BYTES, ("SBUF overflow", self.off, nbytes)
        v = self.ap[0:parts, self.off:self.off + n * esz].bitcast(dtype)
        self.off += nbytes
        if len(shape) == 2:
            v = v.rearrange("p (a b) -> p a b", a=shape[0])
        elif len(shape) == 3:
            v = v.rearrange("p (a b c) -> p a b c", a=shape[0], b=shape[1])
        return v


def build(stop_after=None, debug=False, slim=False, dump=None, only=None):
    nc = bass.Bass("TRN2", target_bir_lowering=False)

    def din(name, shape, dtype=F32):
        return nc.dram_tensor(name, list(shape), dtype, kind="ExternalInput").ap()

    x_d = din("x", [S_LEN, D])
    mem_d = din("mem", [MEM, D])
    pos_d = din("pos", [1, S_LEN], I32)
    gains_d = din("gains", [6, D])
    wkv_d = din("wkv", [2, 4, 128, 4 * 1024])
    win0_d = din("win0", [16, 128, 16 * 128])
    wgrp_d = din("wgrp", [4, 128, 3 * 384])
    pscale_d = din("pscale", [128, 12])
    wout_d = din("wout", [2, 16, 128, 2048])
    win1_d = din("win1", [19, 128, 16 * 128])
    wv1_d = din("wv1", [128, 16 * 192])
    sink_d = din("nsink", [128, 24])
    wr_d = din("wr", [2, 128, 16 * 16])
    nl_, ne_ = slim if slim else (2, NE)
    wg_d = din("wg", [nl_, ne_, 2, 128, 16 * 512])
    wu_d = din("wu", [nl_, ne_, 2, 128, 16 * 512])
    wd_d = din("wd", [nl_, ne_, 4, 128, 8 * 512])
    cidb_d = din("c_identb", [128, 128], BF)
    cidf_d = din("c_identf", [128, 128])
    crot_d = din("c_rot", [128, 128])
    cinvf_d = din("c_invf", [128, 2])
    cmask_d = din("c_mask", [128, 2 * 384], BF)
    cpedge_d = din("c_pedge", [128, 4 * 16])

    y_d = nc.dram_tensor("y", [S_LEN, D], F32, kind="ExternalOutput").ap()
    kind_dbg = "ExternalOutput" if debug else "Internal"
    xres_d = nc.dram_tensor("xres", [S_LEN, D], F32, kind=kind_dbg).ap()
    hrow_d = nc.dram_tensor("hrow", [S_LEN, D], BF, kind="Internal").ap()
    cat_d = nc.dram_tensor("catT", [16, 128, S_LEN], BF, kind=kind_dbg).ap()
    if debug:
        dbg_d = nc.dram_tensor("dbg", [128, 4096], F32, kind="ExternalOutput").ap()

    es = ExitStack()
    arena_t = es.enter_context(nc.sbuf_tensor("arena", [128, ARENA_BYTES], dt.uint8))
    A = Arena(arena_t)
    psb = [es.enter_context(nc.psum_tensor("ps%d" % i, [128, 512], F32)) for i in range(8)]
    S = Sched(nc)
    uid = [0]

    def PS(i):
        return ("ps", i)

    def psf(i, n=512):
        return psb[i][:, 0:n]

    def psbf(i):
        return psb[i][:, :].bitcast(BF)

    def dma(q, out, in_, r, w, key):
        return S.add(q, lambda e: e.dma_start(out=out, in_=in_), r=r, w=w, dma=key)

    def dump_buf(ap, ncols, col_off, res):
        st = A.alloc([ncols], F32)
        S.add("dve", lambda e: e.tensor_copy(out=st, in_=ap), r=res, w=["dumpst"])
        dma("sp", dbg_d[:, col_off:col_off + ncols], st, ["dumpst"], ["dbgd"], "dumpd")

    identb = A.alloc([128], BF)
    identf = A.alloc([128], F32)
    mem_nT = A.alloc([16, MEM], BF)
    gbc = A.alloc([D], F32)
    nsink = A.alloc([24], F32)
    pscale = A.alloc([12], F32)
    idxT = A.alloc([2, 16], U32)
    gT = A.alloc([2, 16], F32)
    stat = A.alloc([64], F32)
    dma("sp", identb, cidb_d, [], ["_identb"], "c0")
    dma("sp", identf, cidf_d, [], ["_identf"], "c1")
    dma("sp", nsink, sink_d, [], ["_nsink"], "c2")
    dma("sp", pscale, pscale_d, [], ["_pscale"], "c3")
    PMARK = A.mark()

    def load_gain(i):
        dma("sp", gbc, gains_d[i:i + 1, :].partition_broadcast(128), [], ["gbc"], "gbc")

    def norm_tile(xt, xt_res, h_out, h_res, junk, sidx):
        ss = stat[:, sidx:sidx + 1]
        rs = stat[:, sidx + 1:sidx + 2]
        S.add("act", lambda e: e.activation(out=junk, in_=xt, func=AF.Square, accum_out=ss),
              r=[xt_res], w=["junk", ("stat", sidx)])
        S.add("act", lambda e: e.activation(out=ss, in_=ss, func=AF.Sqrt, scale=1.0 / D, bias=EPS),
              r=[("stat", sidx)], w=[("stat", sidx)])
        S.add("dve", lambda e: e.reciprocal(out=rs, in_=ss), r=[("stat", sidx)], w=[("stat", sidx + 1)])
        S.add("dve", lambda e: e.scalar_tensor_tensor(out=h_out, in0=xt, scalar=rs, in1=gbc,
                                                      op0=ALU.mult, op1=ALU.mult),
              r=[xt_res, ("stat", sidx + 1), "gbc"], w=[h_res])

    def transpose_tile(h_tm, h_res, dst3, dst_res, pbanks, ncols=128, evac="act"):
        for half in range(2):
            bank = pbanks[half]
            pv = psbf(bank)[:, 0:8 * 128].rearrange("p (a b) -> p a b", a=8)

            def tr(e, half=half, pv=pv):
                last = None
                for k in range(8):
                    dc = half * 8 + k
                    last = e.transpose(out=pv[:, k, 0:ncols], in_=h_tm[0:ncols, dc * 128:(dc + 1) * 128],
                                       identity=identb[0:ncols, 0:ncols])
                return last
            S.add("pe", tr, r=[h_res, "_identb"], w=[PS(bank)])
            dsl = dst3[:, half * 8:(half + 1) * 8, :]
            if evac == "act":
                S.add("act", lambda e, dsl=dsl, pv=pv: e.copy(out=dsl, in_=pv[:, :, 0:ncols]),
                      r=[PS(bank)], w=[(dst_res, half)])
            else:
                S.add("dve", lambda e, dsl=dsl, pv=pv: e.tensor_copy(out=dsl, in_=pv[:, :, 0:ncols]),
                      r=[PS(bank)], w=[(dst_res, half)])

    def stage_mem():
        m0 = A.mark()
        xt = A.alloc([D], F32)
        hm = A.alloc([D], BF)
        junk = A.alloc([D], BF)
        load_gain(4)
        for mt in range(2):
            dma("sp", xt, mem_d[mt * 128:(mt + 1) * 128, :], [], ["xt"], "xt")
            norm_tile(xt, "xt", hm, "hm", junk, 0)
            transpose_tile(hm, "hm", mem_nT[:, :, mt * 128:(mt + 1) * 128], ("memT", mt), (0, 1))
        S.barrier()
        A.reset(m0)

    def stage_norm_hT(src_d, gain_idx, hT):
        m0 = A.mark()
        xts = [A.alloc([D], F32) for _ in range(2)]
        hms = [A.alloc([D], BF) for _ in range(2)]
        junk = A.alloc([D], BF)
        load_gain(gain_idx)
        for t in range(NT):
            b = t % 2
            dma("sp", xts[b], src_d[t * 128:(t + 1) * 128, :], [], [("xt", b)], ("xt", b))
            norm_tile(xts[b], ("xt", b), hms[b], ("hm", b), junk, 2 * b)
            transpose_tile(hms[b], ("hm", b), hT[:, :, t * 128:(t + 1) * 128], ("hT", t),
                           (2 * b, 2 * b + 1), evac=("act" if b == 0 else "dve"))
        S.barrier()
        A.reset(m0)

    def mem_kv(layer, kT, V):
        m0 = A.mark()
        wk = [A.alloc([4, 1024], BF) for _ in range(4)]
        for pc in range(4):
            dma("pool", wk[pc], wkv_d[layer, pc].rearrange("p (a b) -> p a b", a=4), [], [("wkv", pc)], ("wkv", pc))
        wres = [("wkv", pc) for pc in range(4)] + [(("memT", m), h) for m in range(2) for h in range(2)]
        for h in range(4):
            def mmk(e, h=h):
                last = None
                o = psb[4 + h // 2][:, (h % 2) * 256:(h % 2) * 256 + 256]
                for dc in range(16):
                    last = e.matmul(o, wk[dc // 4][:, dc % 4, h * 128:(h + 1) * 128], mem_nT[:, dc, :],
                                    start=(dc == 0), stop=(dc == 15))
                return last
            S.add("pe", mmk, r=wres, w=[PS(4 + h // 2)])
        for mt in range(2):
            def mmv(e, mt=mt):
                last = None
                for dc in range(16):
                    last = e.matmul(psb[6 + mt][:, :], mem_nT[:, dc, mt * 128:(mt + 1) * 128],
                                    wk[dc // 4][:, dc % 4, 512:1024], start=(dc == 0), stop=(dc == 15))
                return last
            S.add("pe", mmv, r=wres, w=[PS(6 + mt)])
        for h in range(4):
            if dump in ("hT2a", "hT2b"):
                break
            S.add("act", lambda e, h=h: e.copy(out=kT[:, h, :], in_=psb[4 + h // 2][:, (h % 2) * 256:(h % 2) * 256 + 256]),
                  r=[PS(4 + h // 2)], w=[("kT", h)])
        for mt in range(2):
            if dump in ("hT2a", "hT2b"):
                break
            S.add("dve", lambda e, mt=mt: e.tensor_copy(out=V[:, mt, :], in_=psb[6 + mt][:, :]),
                  r=[PS(6 + mt)], w=[("V", mt)])
        S.barrier()
        A.reset(m0)

    def proj_chunk(wsrc, hT, wbufs, ci, evac, banks):
        b = ci % len(wbufs)
        wb = wbufs[b]
        dma("pool", wb, wsrc.rearrange("p (a b) -> p a b", a=16), [], [("wb", b)], ("wb", b))
        for ts in range(4):
            bank = banks[ts % len(banks)]

            def mm(e, ts=ts, bank=bank, wb=wb):
                last = None
                for dc in range(16):
                    last = e.matmul(psb[bank][:, :], wb[:, dc, :], hT[:, dc, ts * 512:(ts + 1) * 512],
                                    start=(dc == 0), stop=(dc == 15))
                return last
            S.add("pe", mm, r=[("wb", b)] + [(("hT", t), h) for t in range(ts * 4, ts * 4 + 4) for h in range(2)],
                  w=[PS(bank)])
            evac(ts, bank)

    def xattn_head(h, qT, q_res, kT, V, cst, cst_res, banks):
        sA, sB, bT, bO = banks
        scale = 128.0 ** -0.5
        m0 = A.mark()
        pbuf = [A.alloc([MEM], BF) for _ in range(2)]
        pTb = [A.alloc([2, 128], BF) for _ in range(2)]

        def pa(t):
            b = t % 2
            bank = sA if b == 0 else sB
            sps = psb[bank][:, 0:256]
            mx = stat[:, 8 + b:9 + b]
            sm = stat[:, 10 + b:11 + b]
            S.add("pe", lambda e: e.matmul(sps, qT[:, t * 128:(t + 1) * 128], kT[:, h, :], start=True, stop=True),
                  r=[q_res, ("kT", h)], w=[PS(bank)])
            S.add("dve", lambda e: e.tensor_reduce(out=mx, in_=sps, axis=AX.X, op=ALU.max), r=[PS(bank)], w=[("xs", b)])
            S.add("dve", lambda e: e.tensor_scalar(out=mx, in0=mx, scalar1=-scale, scalar2=None, op0=ALU.mult),
                  r=[("xs", b)], w=[("xs", b)])
            S.add("act", lambda e: e.activation(out=pbuf[b], in_=sps, func=AF.Exp, bias=mx, scale=scale, accum_out=sm),
                  r=[PS(bank), ("xs", b)], w=[("xp", b), ("xsm", b)])

        def pb(t):
            b = t % 2
            sm = stat[:, 10 + b:11 + b]
            S.add("dve", lambda e: e.reciprocal(out=sm, in_=sm), r=[("xsm", b)], w=[("xsm", b)])
            S.add("dve", lambda e: e.tensor_scalar(out=pbuf[b], in0=pbuf[b], scalar1=sm, scalar2=None, op0=ALU.mult),
                  r=[("xp", b), ("xsm", b)], w=[("xp", b)])
            pv = psbf(bT)[:, 0:256].rearrange("p (a c) -> p a c", a=2)

            def tr(e):
                last = None
                for mt in range(2):
                    last = e.transpose(out=pv[:, mt, :], in_=pbuf[b][:, mt * 128:(mt + 1) * 128], identity=identb)
                return last
            S.add("pe", tr, r=[("xp", b), "_identb"], w=[PS(bT)])
            S.add("act", lambda e: e.copy(out=pTb[b], in_=pv), r=[PS(bT)], w=[("xpT", b)])

        def pc(t):
            b = t % 2
            ops_ = psb[bO][:, 0:128]

            def pvmm(e):
                last = None
                for mt in range(2):
                    last = e.matmul(ops_, V[:, mt, h * 128:(h + 1) * 128], pTb[b][:, mt, :], start=(mt == 0), stop=(mt == 1))
                return last
            S.add("pe", pvmm, r=[("xpT", b), ("V", 0), ("V", 1)], w=[PS(bO)])
            S.add("dve", lambda e: e.tensor_copy(out=cst[:, t * 128:(t + 1) * 128], in_=ops_), r=[PS(bO)], w=[cst_res])

        for s_ in range(NT + 2):
            if s_ < NT:
                pa(s_)
            if 0 <= s_ - 1 < NT:
                pb(s_ - 1)
            if 0 <= s_ - 2 < NT:
                pc(s_ - 2)
        A.reset(m0)

    def stage_mixer0():
        m0 = A.mark()
        hT = A.alloc([16, S_LEN], BF)
        kT = A.alloc([4, MEM], BF)
        V = A.alloc([2, 512], BF)
        stage_norm_hT(x_d, 0, hT)
        if dump == "hT":
            rr = [(("hT", t), h) for t in range(NT) for h in range(2)]
            dump_buf(hT[:, 0, :], 2048, 0, rr)
            dump_buf(hT[:, 15, :], 2048, 2048, rr)
            S.barrier()
            return
        mem_kv(0, kT, V)
        if dump in ("hT2", "hT2a", "hT2b"):
            rr = []
            dump_buf(hT[:, 0, :], 2048, 0, rr)
            dump_buf(hT[:, 15, :], 2048, 2048, rr)
            S.barrier()
            return
        L = S_LEN + 16
        Us = [A.alloc([L], F32) for _ in range(2)]
        B1 = A.alloc([L], F32)
        B2 = A.alloc([L], F32)
        pooled = A.alloc([3, S_LEN], BF)
        wbufs = [A.alloc([16, 128], BF) for _ in range(3)]
        wgrp = A.alloc([4, 3 * 384], BF)
        pedge = A.alloc([4, 16], F32)
        tmp8 = A.alloc([16], F32)
        cst = [A.alloc([S_LEN], BF) for _ in range(2)]
        qx = [A.alloc([S_LEN], BF) for _ in range(2)]
        dma("sp", pedge, cpedge_d.rearrange("p (a b) -> p a b", a=4), [], ["_pedge"], "c4")
        for g in range(4):
            dma("pool", wgrp[:, g, :], wgrp_d[g], [], ["_wgrp"], "wgrp")
        for U in Us:
            S.add("dve", lambda e, U=U: e.memset(U[:, 0:8], 0.0), w=["Upad"])
            S.add("dve", lambda e, U=U: e.memset(U[:, 8 + S_LEN:L], 0.0), w=["Upad"])
        S.add("dve", lambda e: e.memset(B1[:, L - 8:L], 0.0), w=["Bpad"])
        S.add("dve", lambda e: e.memset(B2[:, L - 8:L], 0.0), w=["Bpad"])
        cat_n = [0]

        def store_cat(chunk, buf_i):
            dma("sp", cat_d[chunk], cst[buf_i], [("cst", buf_i)], [("catd", chunk)], ("cstst", buf_i))

        def group_linear(g):
            for oc in range(3):
                bi = cat_n[0] % 2
                cat_n[0] += 1
                for ts in range(4):
                    bank = 4 + (ts % 2)

                    def mm(e, oc=oc, ts=ts, bank=bank, g=g):
                        last = None
                        for c3 in range(3):
                            last = e.matmul(psb[bank][:, :], wgrp[:, g, c3 * 384 + oc * 128:c3 * 384 + (oc + 1) * 128],
                                            pooled[:, c3, ts * 512:(ts + 1) * 512], start=(c3 == 0), stop=(c3 == 2))
                        return last
                    S.add("pe", mm, r=["_wgrp"] + [("pooled", c3) for c3 in range(3)], w=[PS(bank)])
                    col = g * 3 + oc
                    S.add("act", lambda e, bi=bi, ts=ts, bank=bank, col=col: e.mul(
                        out=cst[bi][:, ts * 512:(ts + 1) * 512], in_=psb[bank][:, :], mul=pscale[:, col:col + 1]),
                        r=[PS(bank), "_pscale"], w=[("cst", bi)])
                store_cat(g * 3 + oc, bi)

        for ec in range(16):
            g = ec // 3
            if ec < 12:
                ui = ec % 2
                U = Us[ui]

                def evac(ts, bank, U=U, ui=ui):
                    S.add("act", lambda e, ts=ts, bank=bank: e.copy(out=U[:, 8 + ts * 512:8 + (ts + 1) * 512], in_=psb[bank][:, :]),
                          r=[PS(bank)], w=[("U", ui, ts)])
                proj_chunk(win0_d[ec], hT, wbufs, ec, evac, (0, 1, 2, 3))
                if ec % 3 == 0 and ec > 0:
                    group_linear(ec // 3 - 1)
                k = g + 1
                half = 1 << g
                w_ = 2 * half
                src = U
                src_r = [("U", ui, ts) for ts in range(4)] + ["Upad"]
                cur = None
                for step in range(k):
                    sh = 1 << step
                    dst = B1 if (step % 2 == 0) else B2
                    dres = "B1" if (step % 2 == 0) else "B2"
                    n = L - 8 if step > 0 else L - 1
                    n = L - sh - (0 if step == 0 else 0)
                    nn = L - sh
                    eng = "dve"
                    S.add(eng, lambda e, dst=dst, src=src, sh=sh, nn=nn: e.tensor_tensor(out=dst[:, 0:nn], in0=src[:, 0:nn], in1=src[:, sh:sh + nn], op=ALU.add),
                          r=src_r + ["Bpad"], w=[dres])
                    src = dst
                    src_r = [dres]
                off = 8 - half
                cc = ec % 3
                S.add("dve", lambda e, src=src, off=off, w_=w_, cc=cc, U=U: e.scalar_tensor_tensor(
                    out=pooled[:, cc, :], in0=src[:, off:off + S_LEN], scalar=1.0 / w_, in1=U[:, 8:8 + S_LEN],
                    op0=ALU.mult, op1=ALU.subtract), r=src_r + [("U", ui, ts) for ts in range(4)], w=[("pooled", cc)])
                for (c0, pe0) in ((0, 0), (S_LEN - 8, 8)):
                    S.add("dve", lambda e, src=src, off=off, c0=c0, pe0=pe0, g=g: e.tensor_tensor(
                        out=tmp8[:, 0:8], in0=src[:, off + c0:off + c0 + 8], in1=pedge[:, g, pe0:pe0 + 8], op=ALU.mult),
                        r=src_r + ["_pedge"], w=["tmp8"])
                    S.add("dve", lambda e, c0=c0, cc=cc, U=U: e.tensor_tensor(
                        out=pooled[:, cc, c0:c0 + 8], in0=tmp8[:, 0:8], in1=U[:, 8 + c0:16 + c0], op=ALU.subtract),
                        r=["tmp8"] + [("U", ui, ts) for ts in range(4)], w=[("pooled", cc)])
            else:
                h = ec - 12
                qb = qx[h % 2]

                def evac(ts, bank, qb=qb, h=h):
                    S.add("act", lambda e, ts=ts, bank=bank: e.copy(out=qb[:, ts * 512:(ts + 1) * 512], in_=psb[bank][:, :]),
                          r=[PS(bank)], w=[("qx", h % 2)])
                proj_chunk(win0_d[ec], hT, wbufs, ec, evac, (0, 1, 2, 3))
                if ec == 12:
                    group_linear(3)
                bi = cat_n[0] % 2
                cat_n[0] += 1
                xattn_head(h, qb, ("qx", h % 2), kT, V, cst[bi], ("cst", bi), (4, 5, 6, 7))
                store_cat(12 + h, bi)
        S.barrier()
        A.reset(m0)

    def stage_outproj(layer, src_d):
        m0 = A.mark()
        catT = A.alloc([16, S_LEN], BF)
        wo = A.alloc([16, D], BF)
        xts = [A.alloc([D], F32) for _ in range(2)]
        for ec in range(16):
            dma("sp", catT[:, ec, :], cat_d[ec], [("catd", ec)], [("catT", ec)], ("catl", ec % 4))
            dma("pool", wo[:, ec, :], wout_d[layer, ec], [], [("wo", ec)], ("wol", ec % 4))
        for t in range(NT):
            b = t % 2
            dma("sp", xts[b], src_d[t * 128:(t + 1) * 128, :], [], [("xo", b)], ("xo", b))
            for ds in range(4):
                bank = 4 * b + ds

                def mm(e, t=t, ds=ds, bank=bank):
                    last = None
                    for ec in range(16):
                        last = e.matmul(psb[bank][:, :], catT[:, ec, t * 128:(t + 1) * 128], wo[:, ec, ds * 512:(ds + 1) * 512],
                                        start=(ec == 0), stop=(ec == 15))
                    return last
                S.add("pe", mm, r=[("catT", ec) for ec in range(16)] + [("wo", ec) for ec in range(16)], w=[PS(bank)])
                S.add("dve", lambda e, b=b, ds=ds, bank=bank: e.tensor_tensor(
                    out=xts[b][:, ds * 512:(ds + 1) * 512], in0=psb[bank][:, :], in1=xts[b][:, ds * 512:(ds + 1) * 512], op=ALU.add),
                    r=[PS(bank), ("xo", b)], w=[("xo", b)])
            dma("sp", xres_d[t * 128:(t + 1) * 128, :], xts[b], [("xo", b)], [("xres", t)], ("xost", b))
        S.barrier()
        A.reset(m0)

    def stage_router(layer, pre=None):
        m0 = A.mark()
        if pre is not None:
            pre()
        xts = [A.alloc([D], F32) for _ in range(2)]
        hms = [A.alloc([D], BF) for _ in range(2)]
        junk = A.alloc([D], BF)
        hTt = [A.alloc([16, 128], BF) for _ in range(2)]
        wr = A.alloc([16, 16], BF)
        affT = A.alloc([S_LEN], F32)
        wkb = A.alloc([S_LEN], F32)
        vals = A.alloc([CAP], F32)
        idxu = A.alloc([CAP], U32)
        idxf = A.alloc([CAP], F32)
        lg = [A.alloc([16], F32) for _ in range(2)]
        load_gain(1 + 2 * layer)
        dma("pool", wr, wr_d[layer].rearrange("p (a b) -> p a b", a=16), [], ["_wr"], "wr")
        for t in range(NT):
            b = t % 2
            dma("sp", xts[b], xres_d[t * 128:(t + 1) * 128, :], [("xres", t)], [("xt", b)], ("xt", b))
            norm_tile(xts[b], ("xt", b), hms[b], ("hm", b), junk, 2 * b)
            dma("sp", hrow_d[t * 128:(t + 1) * 128, :], hms[b], [("hm", b)], ["hrow"], ("hst", b))
            transpose_tile(hms[b], ("hm", b), hTt[b], ("hTt", b), (2 * b, 2 * b + 1), evac=("act" if b == 0 else "dve"))
            bank = 4 + b

            def mm(e, b=b, bank=bank):
                last = None
                for dc in range(16):
                    last = e.matmul(psb[bank][:, 0:16], hTt[b][:, dc, :], wr[:, dc, :], start=(dc == 0), stop=(dc == 15))
                return last
            S.add("pe", mm, r=[(("hTt", b), 0), (("hTt", b), 1), "_wr"], w=[PS(bank)])
            mx = stat[:, 16 + b:17 + b]
            sm = stat[:, 18 + b:19 + b]
            S.add("dve", lambda e, bank=bank, mx=mx: e.tensor_reduce(out=mx, in_=psb[bank][:, 0:16], axis=AX.X, op=ALU.max),
                  r=[PS(bank)], w=[("rs", b)])
            S.add("dve", lambda e, mx=mx: e.tensor_scalar(out=mx, in0=mx, scalar1=-1.0, scalar2=None, op0=ALU.mult),
                  r=[("rs", b)], w=[("rs", b)])
            S.add("act", lambda e, b=b, bank=bank, mx=mx, sm=sm: e.activation(out=lg[b], in_=psb[bank][:, 0:16], func=AF.Exp, bias=mx, scale=1.0, accum_out=sm),
                  r=[PS(bank), ("rs", b)], w=[("lg", b), ("rsm", b)])
            S.add("dve", lambda e, sm=sm: e.reciprocal(out=sm, in_=sm), r=[("rsm", b)], w=[("rsm", b)])
            S.add("dve", lambda e, b=b, sm=sm: e.tensor_scalar(out=lg[b], in0=lg[b], scalar1=sm, scalar2=None, op0=ALU.mult),
                  r=[("lg", b), ("rsm", b)], w=[("lg", b)])
            tb = 6 + b
            S.add("pe", lambda e, b=b, tb=tb: e.transpose(out=psb[tb][0:16, 0:128], in_=lg[b], identity=identf),
                  r=[("lg", b), "_identf"], w=[PS(tb)])
            S.add("act", lambda e, t=t, tb=tb: e.copy(out=affT[0:16, t * 128:(t + 1) * 128], in_=psb[tb][0:16, 0:128]),
                  r=[PS(tb)], w=["affT"])
        cur = affT
        for it in range(CAP // 8):
            v8 = vals[0:16, it * 8:it * 8 + 8]
            i8 = idxu[0:16, it * 8:it * 8 + 8]
            S.add("dve", lambda e, v8=v8, cur=cur: e.max(out=v8, in_=cur[0:16, :]), r=["affT", "wkb"], w=[("v8", it)])
            S.add("dve", lambda e, v8=v8, i8=i8, cur=cur: e.max_index(out=i8, in_max=v8, in_values=cur[0:16, :]),
                  r=["affT", "wkb", ("v8", it)], w=[("i8", it)])
            if it < CAP // 8 - 1:
                S.add("dve", lambda e, v8=v8, cur=cur: e.match_replace(out=wkb[0:16, :], in_to_replace=v8, in_values=cur[0:16, :], imm_value=-1.0),
                      r=["affT", ("v8", it)], w=["wkb"])
                cur = wkb
        S.add("dve", lambda e: e.tensor_copy(out=idxf[0:16, :], in_=idxu[0:16, :]), r=[("i8", it) for it in range(CAP // 8)], w=["idxf"])
        for ct in range(2):
            S.add("pe", lambda e, ct=ct: e.transpose(out=psb[0][:, ct * 16:(ct + 1) * 16], in_=idxf[0:16, ct * 128:(ct + 1) * 128], identity=identf[0:16, 0:16]),
                  r=["idxf", "_identf"], w=[(PS(0), ct)])
            S.add("pe", lambda e, ct=ct: e.transpose(out=psb[1][:, ct * 16:(ct + 1) * 16], in_=vals[0:16, ct * 128:(ct + 1) * 128], identity=identf[0:16, 0:16]),
                  r=[("v8", it) for it in range(CAP // 8)] + ["_identf"], w=[(PS(1), ct)])
        S.add("dve", lambda e: e.tensor_copy(out=idxT.rearrange("p a b -> p (a b)"), in_=psb[0][:, 0:32]), r=[(PS(0), 0), (PS(0), 1)], w=["idxT"])
        S.add("dve", lambda e: e.tensor_copy(out=gT.rearrange("p a b -> p (a b)"), in_=psb[1][:, 0:32]), r=[(PS(1), 0), (PS(1), 1)], w=["gT"])
        if dump == "rt":
            dump_buf(idxT.rearrange("p a b -> p (a b)"), 32, 0, ["idxT"])
            dump_buf(gT.rearrange("p a b -> p (a b)"), 32, 32, ["gT"])
        S.barrier()
        A.reset(m0)

    def stage_moe(layer):
        m0 = A.mark()
        NBIG, NSM = 4, 5
        big = [A.alloc([16, 512], BF) for _ in range(NBIG)]
        sml = [A.alloc([8, 512], BF) for _ in range(NSM)]
        nb = [0]
        ns = [0]
        slots_of = {}

        def load_big(e_):
            slots = slots_of.setdefault(e_, {})
            for fs in range(2):
                for nm, src in (("g", wg_d), ("u", wu_d)):
                    bi = nb[0] % NBIG
                    nb[0] += 1
                    dma("pool", big[bi], src[layer, e_, fs].rearrange("p (a b) -> p a b", a=16), [], [("big", bi)], ("big", bi))
                    slots[(nm, fs)] = bi

        def load_sml(e_):
            slots = slots_of.setdefault(e_, {})
            for ds in range(4):
                si = ns[0] % NSM
                ns[0] += 1
                dma("pool", sml[si], wd_d[layer, e_, ds].rearrange("p (a b) -> p a b", a=8), [], [("sml", si)], ("sml", si))
                slots[("d", ds)] = si

        stage_router(layer, pre=lambda: (load_big(0), load_sml(0)))
        xg = [[A.alloc([D], BF) for _ in range(2)] for _ in range(2)]
        xgT = [A.alloc([16, CAP], BF) for _ in range(2)]
        act_tm = A.alloc([2, DFF], BF)
        actT = A.alloc([8, CAP], BF)
        sa = [A.alloc([512], F32) for _ in range(2)]
        yo = [A.alloc([D], F32) for _ in range(2)]

        def gather(e_):
            pb = e_ % 2
            for ct in range(2):
                S.add("pool", lambda e, pb=pb, ct=ct, e_=e_: e.indirect_dma_start(
                    out=xg[pb][ct], out_offset=None, in_=hrow_d,
                    in_offset=bass.IndirectOffsetOnAxis(ap=idxT[:, ct, e_:e_ + 1], axis=0)),
                    r=["hrow", "idxT"], w=[("xg", pb, ct)], dma=("xg", pb, ct))

        gather(0)
        for e_ in range(NE):
            pb = e_ % 2
            slots = slots_of[e_]
            if e_ + 1 < NE:
                gather(e_ + 1)
            for ct in range(2):
                transpose_tile(xg[pb][ct], ("xg", pb, ct), xgT[pb][:, :, ct * 128:(ct + 1) * 128], ("xgT", pb, ct),
                               (6, 7), evac=("act" if ct == 0 else "dve"))
            xg_r = [(("xgT", pb, ct), h) for ct in range(2) for h in range(2)]
            for fs in range(2):
                bg = slots[("g", fs)]
                bu = slots[("u", fs)]
                for ct in range(2):
                    pa = ct
                    pu = 2 + ct

                    def mm(e, bw, bank, ct=ct, pb=pb):
                        last = None
                        for dc in range(16):
                            last = e.matmul(psb[bank][:, :], xgT[pb][:, dc, ct * 128:(ct + 1) * 128], big[bw][:, dc, :],
                                            start=(dc == 0), stop=(dc == 15))
                        return last
                    S.add("pe", lambda e, bg=bg, pa=pa, mm=mm: mm(e, bg, pa), r=xg_r + [("big", bg)], w=[PS(pa)])
                    S.add("pe", lambda e, bu=bu, pu=pu, mm=mm: mm(e, bu, pu), r=xg_r + [("big", bu)], w=[PS(pu)])
                    S.add("act", lambda e, ct=ct, pa=pa: e.activation(out=sa[ct], in_=psb[pa][:, :], func=AF.Silu),
                          r=[PS(pa)], w=[("sa", ct)])
                    S.add("dve", lambda e, ct=ct, pu=pu, fs=fs: e.tensor_tensor(out=act_tm[:, ct, fs * 512:(fs + 1) * 512], in0=sa[ct], in1=psb[pu][:, :], op=ALU.mult),
                          r=[("sa", ct), PS(pu)], w=[("act_tm", ct, fs)])
            if e_ + 1 < NE:
                load_big(e_ + 1)
            for ct in range(2):
                pv = psbf(6 + ct)[:, :].rearrange("p (a b) -> p a b", a=8)

                def tr(e, ct=ct, pv=pv):
                    last = None
                    for fc in range(8):
                        last = e.transpose(out=pv[:, fc, :], in_=act_tm[:, ct, fc * 128:(fc + 1) * 128], identity=identb)
                    return last
                S.add("pe", tr, r=[("act_tm", ct, 0), ("act_tm", ct, 1), "_identb"], w=[PS(6 + ct)])
                if ct == 0:
                    S.add("act", lambda e, ct=ct, pv=pv: e.copy(out=actT[:, :, ct * 128:(ct + 1) * 128], in_=pv), r=[PS(6 + ct)], w=[("actT", ct)])
                else:
                    S.add("dve", lambda e, ct=ct, pv=pv: e.tensor_copy(out=actT[:, :, ct * 128:(ct + 1) * 128], in_=pv), r=[PS(6 + ct)], w=[("actT", ct)])
            k = 0
            for ds in range(4):
                si = slots[("d", ds)]
                for ct in range(2):
                    bank = 4 + (k % 2)
                    k += 1

                    def mmd(e, ct=ct, si=si, bank=bank):
                        last = None
                        for fc in range(8):
                            last = e.matmul(psb[bank][:, :], actT[:, fc, ct * 128:(ct + 1) * 128], sml[si][:, fc, :],
                                            start=(fc == 0), stop=(fc == 7))
                        return last
                    S.add("pe", mmd, r=[("actT", 0), ("actT", 1), ("sml", si)], w=[PS(bank)])
                    if ct == 0:
                        S.add("act", lambda e, ct=ct, ds=ds, bank=bank, e_=e_: e.mul(
                            out=yo[ct][:, ds * 512:(ds + 1) * 512], in_=psb[bank][:, :], mul=gT[:, ct, e_:e_ + 1]),
                            r=[PS(bank), "gT"], w=[("yo", ct)])
                    else:
                        S.add("dve", lambda e, ct=ct, ds=ds, bank=bank, e_=e_: e.tensor_scalar(
                            out=yo[ct][:, ds * 512:(ds + 1) * 512], in0=psb[bank][:, :], scalar1=gT[:, ct, e_:e_ + 1], scalar2=None, op0=ALU.mult),
                            r=[PS(bank), "gT"], w=[("yo", ct)])
            if e_ + 1 < NE:
                load_sml(e_ + 1)
            for ct in range(2):
                S.add("pool", lambda e, ct=ct, e_=e_: e.indirect_dma_start(
                    out=xres_d, out_offset=bass.IndirectOffsetOnAxis(ap=idxT[:, ct, e_:e_ + 1], axis=0),
                    in_=yo[ct], in_offset=None, compute_op=ALU.add),
                    r=[("yo", ct), "idxT"], w=["xres_all"], dma=("sc", ct))
        S.barrier()
        A.reset(m0)

    def stage_mixer1():
        m0 = A.mark()
        hT = A.alloc([16, S_LEN], BF)
        kT = A.alloc([4, MEM], BF)
        V = A.alloc([2, 512], BF)
        Ct = A.alloc([S_LEN], F32)
        St = A.alloc([S_LEN], F32)
        invf = A.alloc([2], F32)
        rotT = A.alloc([128], F32)
        stage_norm_hT(xres_d, 2, hT)
        m1 = A.mark()
        posi = A.alloc([S_LEN], I32)
        posf = A.alloc([S_LEN], F32)
        dma("sp", posi, pos_d.partition_broadcast(128), [], ["posi"], "posi")
        dma("sp", invf, cinvf_d, [], ["_invf"], "c5")
        dma("sp", rotT, crot_d, [], ["_rotT"], "c6")
        S.add("dve", lambda e: e.tensor_copy(out=posf, in_=posi), r=["posi"], w=["posf"])
        ki = A.alloc([S_LEN], I32)
        mk = A.alloc([S_LEN], F32)
        SC = 6.283179

        def reduce_(T, res):
            S.add("dve", lambda e: e.tensor_copy(out=ki, in_=T), r=[res], w=["ki"])
            S.add("dve", lambda e: e.tensor_copy(out=mk, in_=ki), r=["ki"], w=["mk"])
            S.add("dve", lambda e: e.tensor_tensor(out=T, in0=T, in1=mk, op=ALU.subtract), r=[res, "mk"], w=[res])
            S.add("dve", lambda e: e.tensor_scalar(out=mk, in0=T, scalar1=0.5, scalar2=None, op0=ALU.is_gt), r=[res], w=["mk"])
            S.add("dve", lambda e: e.tensor_tensor(out=T, in0=T, in1=mk, op=ALU.subtract), r=[res, "mk"], w=[res])
            S.add("dve", lambda e: e.tensor_scalar(out=mk, in0=T, scalar1=-0.5, scalar2=None, op0=ALU.is_lt), r=[res], w=["mk"])
            S.add("dve", lambda e: e.tensor_tensor(out=T, in0=T, in1=mk, op=ALU.add), r=[res, "mk"], w=[res])
            S.add("act", lambda e: e.activation(out=T, in_=T, func=AF.Sin, scale=SC), r=[res], w=[res])
        S.add("dve", lambda e: e.tensor_scalar(out=St, in0=posf, scalar1=invf[:, 0:1], scalar2=None, op0=ALU.mult),
              r=["posf", "_invf"], w=["St"])
        S.add("dve", lambda e: e.tensor_scalar(out=Ct, in0=posf, scalar1=invf[:, 0:1], scalar2=0.25, op0=ALU.mult, op1=ALU.add),
              r=["posf", "_invf"], w=["Ct"])
        reduce_(St, "St")
        reduce_(Ct, "Ct")
        S.add("dve", lambda e: e.tensor_scalar(out=St, in0=St, scalar1=invf[:, 1:2], scalar2=None, op0=ALU.mult), r=["St", "_invf"], w=["St"])
        S.add("dve", lambda e: e.tensor_scalar(out=Ct, in0=Ct, scalar1=-1.0, scalar2=invf[:, 1:2], op0=ALU.add, op1=ALU.mult), r=["Ct", "_invf"], w=["Ct"])
        S.add("dve", lambda e: e.tensor_scalar(out=Ct, in0=Ct, scalar1=1.0, scalar2=None, op0=ALU.add), r=["Ct"], w=["Ct"])
        S.barrier()
        A.reset(m1)
        mem_kv(1, kT, V)
        wbufs = [A.alloc([16, 128], BF) for _ in range(3)]
        wv = A.alloc([16, 192], BF)
        v_tm = A.alloc([NT, 192], BF)
        kT2 = A.alloc([3, S_LEN], BF)
        qf = A.alloc([S_LEN], F32)
        t1 = A.alloc([512], F32)
        qTb = [A.alloc([S_LEN], BF) for _ in range(2)]
        mask = A.alloc([2, 384], BF)
        pb_ = [A.alloc([2, 384], BF) for _ in range(2)]
        pm_ = [A.alloc([2, 384], BF) for _ in range(2)]
        pTs = [A.alloc([6, 128], BF) for _ in range(2)]
        otm = [A.alloc([128], BF) for _ in range(2)]
        cst = [A.alloc([S_LEN], BF) for _ in range(2)]
        qx = [A.alloc([S_LEN], BF) for _ in range(2)]
        dma("sp", mask, cmask_d.rearrange("p (a b) -> p a b", a=2), [], ["_mask"], "c7")
        dma("pool", wv, wv1_d.rearrange("p (a b) -> p a b", a=16), [], ["_wv"], "wv")
        hT_all = [(("hT", t), h) for t in range(NT) for h in range(2)]
        for t in range(NT):
            bank = t % 2

            def mm(e, t=t, bank=bank):
                last = None
                for dc in range(16):
                    last = e.matmul(psb[bank][:, 0:192], hT[:, dc, t * 128:(t + 1) * 128], wv[:, dc, :], start=(dc == 0), stop=(dc == 15))
                return last
            S.add("pe", mm, r=["_wv", (("hT", t), 0), (("hT", t), 1)], w=[PS(bank)])
            S.add("act", lambda e, t=t, bank=bank: e.copy(out=v_tm[:, t, :], in_=psb[bank][:, 0:192]), r=[PS(bank)], w=[("v_tm", t)])

        def rotary_sub(dst, dst_res, ts, hh):
            c0 = ts * 512 + hh * 256
            S.add("pe", lambda e: e.matmul(psb[2][:, 0:256], rotT, qf[:, c0:c0 + 256], start=True, stop=True),
                  r=[("qf", ts), "_rotT"], w=[PS(2)])
            S.add("dve", lambda e: e.tensor_tensor(out=t1[:, 0:256], in0=psb[2][:, 0:256], in1=St[:, c0:c0 + 256], op=ALU.mult),
                  r=[PS(2), "St"], w=["t1a"])
            S.add("pool", lambda e: e.tensor_tensor(out=t1[:, 256:512], in0=qf[:, c0:c0 + 256], in1=Ct[:, c0:c0 + 256], op=ALU.mult),
                  r=[("qf", ts), "Ct"], w=["t1b"])
            S.add("dve", lambda e: e.tensor_tensor(out=dst[:, c0:c0 + 256], in0=t1[:, 0:256], in1=t1[:, 256:512], op=ALU.add),
                  r=["t1a", "t1b"], w=[dst_res])

        def rotary_chunk(dst, dst_res):
            for ts in range(4):
                for hh in range(2):
                    rotary_sub(dst, dst_res, ts, hh)

        nxt = {}

        def next_chunk_item(j1, k):
            if k == 0:
                b = ci[0] % len(wbufs)
                ci[0] += 1
                nxt["wb"] = (wbufs[b], b)
                dma("pool", wbufs[b], win1_d[3 + j1].rearrange("p (a b) -> p a b", a=16), [], [("wb", b)], ("wb", b))
            elif 2 <= k <= 5:
                ts = k - 2
                wb, b = nxt["wb"]

                def mm(e):
                    last = None
                    for dc in range(16):
                        last = e.matmul(psb[2][:, :], wb[:, dc, :], hT[:, dc, ts * 512:(ts + 1) * 512],
                                        start=(dc == 0), stop=(dc == 15))
                    return last
                S.add("pe", mm, r=[("wb", b)], w=[PS(2)])
                evac_qf(ts, 2)
            elif 6 <= k <= 13:
                rotary_sub(qTb[j1 % 2], ("qTb", j1 % 2), (k - 6) // 2, (k - 6) % 2)

        def evac_qf(ts, bank):
            S.add("act", lambda e, ts=ts, bank=bank: e.copy(out=qf[:, ts * 512:(ts + 1) * 512], in_=psb[bank][:, :]),
                  r=[PS(bank)], w=[("qf", ts)])

        ci = [0]
        for g in range(3):
            proj_chunk(win1_d[g], hT, wbufs, ci[0], evac_qf, (0, 1))
            ci[0] += 1
            rotary_chunk(kT2[:, g, :], ("kT2", g))
        cat_n = [0]

        def store_cat(chunk, buf_i):
            dma("sp", cat_d[chunk], cst[buf_i], [("cst", buf_i)], [("catd", chunk)], ("cstst", buf_i))

        NQ = 12 * NT

        def geom(q):
            j, n = q // NT, q % NT
            kb0 = max(n - 1, 0)
            kb1 = min(n + 1, NT - 1)
            nk = (kb1 - kb0 + 1) * 128
            moff = 0 if n > 0 else 128
            return j, n, j // 4, kb0, nk, moff

        def st_cols(q):
            i4 = q % 4
            base = 24 + 6 * i4
            return stat[:, base:base + 2], stat[:, base + 2:base + 4], stat[:, base + 4:base + 6]

        def ph0(q):
            j, n, g, kb0, nk, moff = geom(q)
            ib = q % 2
            i4 = q % 4
            qb = qTb[j % 2]
            qres = ("qTb", j % 2)
            sb0 = 3 if ib == 0 else 0
            mx, sm, es_ = st_cols(q)
            for hh in range(2):
                def qk(e, hh=hh):
                    e.matmul(psb[sb0 + hh][:, 0:nk], qb[hh * 64:(hh + 1) * 64, n * 128:(n + 1) * 128],
                             kT2[hh * 64:(hh + 1) * 64, g, kb0 * 128:kb0 * 128 + nk], start=True, stop=False)
                    return e.matmul(psb[sb0 + hh][:, 0:nk], identb, mask[:, 0, moff:moff + nk], start=False, stop=True)
                S.add("pe", qk, r=[qres, ("kT2", g), "_mask", "_identb"], w=[PS(sb0 + hh)])
            for hh in range(2):
                S.add("dve", lambda e, hh=hh: e.tensor_reduce(out=mx[:, hh:hh + 1], in_=psb[sb0 + hh][:, 0:nk], axis=AX.X, op=ALU.max),
                      r=[PS(sb0 + hh)], w=[("amx", i4, hh)])
            S.add("dve", lambda e: e.scalar_tensor_tensor(out=mx, in0=mx, scalar=-0.125, in1=nsink[:, 2 * j:2 * j + 2], op0=ALU.mult, op1=ALU.min),
                  r=[("amx", i4, 0), ("amx", i4, 1), "_nsink"], w=[("anm", i4)])
            S.add("dve", lambda e: e.tensor_tensor(out=es_, in0=mx, in1=nsink[:, 2 * j:2 * j + 2], op=ALU.subtract),
                  r=[("anm", i4), "_nsink"], w=[("aes", i4)])
            for hh in range(2):
                S.add("act", lambda e, hh=hh: e.activation(
                    out=pm_[ib][:, hh, 0:nk], in_=psb[sb0 + hh][:, 0:nk], func=AF.Exp, bias=mx[:, hh:hh + 1], scale=0.125,
                    accum_out=sm[:, hh:hh + 1]),
                    r=[PS(sb0 + hh), ("anm", i4)], w=[("apm", ib, hh), ("asm", i4, hh)])
            S.add("act", lambda e: e.activation(out=es_, in_=es_, func=AF.Exp), r=[("aes", i4)], w=[("aes", i4)])

        def ph1(q):
            j, n, g, kb0, nk, moff = geom(q)
            ib = q % 2
            i4 = q % 4
            nkb = nk // 128
            mx, sm, es_ = st_cols(q)
            S.add("dve", lambda e: e.tensor_tensor(out=sm, in0=sm, in1=es_, op=ALU.add),
                  r=[("aes", i4), ("asm", i4, 0), ("asm", i4, 1)], w=[("ard", i4)])
            S.add("dve", lambda e: e.reciprocal(out=sm, in_=sm), r=[("ard", i4)], w=[("ard", i4)])
            pv = psbf(7)[:, 0:768].rearrange("p (a b) -> p a b", a=6)

            def tr(e):
                last = None
                for hh in range(2):
                    for kb in range(nkb):
                        last = e.transpose(out=pv[:, hh * 3 + kb, :], in_=pm_[ib][:, hh, kb * 128:(kb + 1) * 128], identity=identb)
                return last
            S.add("pe", tr, r=[("apm", ib, 0), ("apm", ib, 1), "_identb"], w=[(PS(7), 0)])
            if nkb == 3:
                S.add("act", lambda e: e.copy(out=pTs[ib], in_=pv), r=[(PS(7), 0)], w=[("apT", ib)])
            else:
                for hh in range(2):
                    S.add("act", lambda e, hh=hh: e.copy(out=pTs[ib][:, hh * 3:hh * 3 + nkb, :], in_=pv[:, hh * 3:hh * 3 + nkb, :]),
                          r=[(PS(7), 0)], w=[("apT", ib)])

        def ph2(q):
            j, n, g, kb0, nk, moff = geom(q)
            ib = q % 2
            i4 = q % 4
            nkb = nk // 128
            mx, sm, es_ = st_cols(q)
            po = psb[5][:, 0:128]

            def pvm(e):
                last = None
                for hh in range(2):
                    for kb in range(nkb):
                        last = e.matmul(po[:, hh * 64:(hh + 1) * 64], pTs[ib][:, hh * 3 + kb, :], v_tm[:, kb0 + kb, g * 64:(g + 1) * 64],
                                        start=(kb == 0), stop=(kb == nkb - 1))
                return last
            S.add("pe", pvm, r=[("apT", ib)] + [("v_tm", kb0 + kb) for kb in range(nkb)], w=[PS(5)])
            for hh in range(2):
                S.add("act", lambda e, hh=hh: e.mul(
                    out=otm[ib][:, hh * 64:(hh + 1) * 64], in_=po[:, hh * 64:(hh + 1) * 64], mul=sm[:, hh:hh + 1]),
                    r=[PS(5), ("ard", i4)], w=[("otm", ib)])

        def ph3(q):
            j, n, g, kb0, nk, moff = geom(q)
            ib = q % 2
            bi = j % 2
            ft = psbf(6)[:, 0:128]
            S.add("pe", lambda e: e.transpose(out=ft, in_=otm[ib], identity=identb), r=[("otm", ib), "_identb"], w=[PS(6)])
            S.add("dve", lambda e: e.tensor_copy(out=cst[bi][:, n * 128:(n + 1) * 128], in_=ft), r=[PS(6)], w=[("cst", bi)])
            if n == NT - 1:
                store_cat(j, bi)

        proj_chunk(win1_d[3], hT, wbufs, ci[0], evac_qf, (0, 1))
        ci[0] += 1
        rotary_chunk(qTb[0], ("qTb", 0))
        for s_ in range(NQ + 3):
            if s_ < NQ:
                ph0(s_)
                j1 = s_ // NT + 1
                if j1 < 12:
                    next_chunk_item(j1, s_ % NT)
            if 0 <= s_ - 1 < NQ:
                ph1(s_ - 1)
            if 0 <= s_ - 2 < NQ:
                ph2(s_ - 2)
            if 0 <= s_ - 3 < NQ:
                ph3(s_ - 3)
        cat_n[0] = 12
        for h in range(4):
            qb = qx[h % 2]

            def evac(ts, bank, qb=qb, h=h):
                S.add("act", lambda e, ts=ts, bank=bank: e.copy(out=qb[:, ts * 512:(ts + 1) * 512], in_=psb[bank][:, :]),
                      r=[PS(bank)], w=[("qx", h % 2)])
            proj_chunk(win1_d[15 + h], hT, wbufs, ci[0], evac, (0, 1))
            ci[0] += 1
            bi = cat_n[0] % 2
            cat_n[0] += 1
            xattn_head(h, qb, ("qx", h % 2), kT, V, cst[bi], ("cst", bi), (3, 4, 7, 5))
            store_cat(12 + h, bi)
        S.barrier()
        A.reset(m0)

    def stage_final():
        m0 = A.mark()
        xts = [A.alloc([D], F32) for _ in range(2)]
        outs = [A.alloc([D], F32) for _ in range(2)]
        junk = A.alloc([D], BF)
        load_gain(5)
        for t in range(NT):
            b = t % 2
            dma("sp", xts[b], xres_d[t * 128:(t + 1) * 128, :], [], [("xt", b)], ("xt", b))
            norm_tile(xts[b], ("xt", b), outs[b], ("of", b), junk, 2 * b)
            dma("sp", y_d[t * 128:(t + 1) * 128, :], outs[b], [("of", b)], ["y"], ("yst", b))
        A.reset(m0)

    stages = [
        ("mem", stage_mem),
        ("mix0", stage_mixer0),
        ("out0", lambda: stage_outproj(0, x_d)),
        ("ex0", lambda: stage_moe(0)),
        ("mix1", stage_mixer1),
        ("out1", lambda: stage_outproj(1, xres_d)),
        ("ex1", lambda: stage_moe(1)),
        ("final", stage_final),
    ]
    for name, fn in stages:
        if only is not None and name not in only:
            continue
        fn()
        if stop_after == name:
            break
    if stop_after is not None and stop_after != "final":
        S.barrier()
    finals = [k for k in S.dma_cnt if isinstance(k, tuple) and k[0] in ("yst", "xost", "cstst", "sc")]
    S.emit(es, final_dma_keys=finals)
    es.close()
    return nc


def _consts():
    identb = np.eye(128, dtype=np.float32).astype(ml_dtypes.bfloat16)
    identf = np.eye(128, dtype=np.float32)
    rot = np.zeros((128, 128), np.float32)
    for b in (0, 64):
        for r in range(8):
            rot[b + r + 8, b + r] = -1.0
            rot[b + r, b + r + 8] = 1.0
    inv_freq = (500000.0 ** (-np.arange(0, 16, 2, dtype=np.float32) / 16)).astype(np.float32)
    invf = np.zeros((128, 2), np.float32)
    for p in range(128):
        r = p % 64
        if r < 16:
            invf[p, 0] = np.float32(inv_freq[r % 8] / (2.0 * np.pi))
            invf[p, 1] = 1.0
    qq = np.arange(128)[:, None]
    kk = np.arange(128)[None, :]
    m = np.concatenate([(kk >= qq), np.ones((128, 128), bool), (kk <= qq)], axis=1)
    m = np.where(m, 0.0, -30000.0).astype(np.float32)
    mask = np.concatenate([m, m], axis=1).astype(ml_dtypes.bfloat16)
    pedge = np.zeros((4, 16), np.float32)
    t = np.arange(S_LEN)
    for g, w in enumerate((2, 4, 8, 16)):
        lo = np.maximum(t - w // 2, 0)
        hi = np.minimum(t + w // 2 - 1, S_LEN - 1)
        cnt = (hi - lo + 1).astype(np.float32)
        pedge[g, 0:8] = 1.0 / cnt[0:8]
        pedge[g, 8:16] = 1.0 / cnt[S_LEN - 8:]
    pedge = np.broadcast_to(pedge.reshape(1, 64), (128, 64)).copy()
    return identb, identf, rot, invf, mask, pedge


def _pieces(w, pc):
    R, C = w.shape
    n = R // 128
    return np.ascontiguousarray(w.reshape(n, 128, C // pc, pc).transpose(2, 1, 0, 3)).reshape(C // pc, 128, n * pc)


def _host_layout(inp, slim=False):
    f = lambda a: np.ascontiguousarray(np.asarray(a, dtype=np.float32))
    shared = {}
    shared["gains"] = np.stack([f(inp["norm_mix_g"])[0], f(inp["norm_ffn_g"])[0], f(inp["norm_mix_g"])[1],
                                f(inp["norm_ffn_g"])[1], f(inp["mem_norm_g"]), f(inp["final_g"])], axis=0)
    wkv = f(inp["mem_w_kv"])
    shared["wkv"] = np.ascontiguousarray(wkv.reshape(2, 4, 4, 128, 1024).transpose(0, 1, 3, 2, 4)).reshape(2, 4, 128, 4096)
    shared["win0"] = _pieces(f(inp["pool_w_in"])[0], 128)
    gw = f(inp["pool_group_w"])[0]
    shared["wgrp"] = np.ascontiguousarray(gw.reshape(4, 3, 128, 384).transpose(0, 2, 1, 3)).reshape(4, 128, 1152)
    shared["pscale"] = np.ascontiguousarray(f(inp["pool_scale"])[0].reshape(12, 128).T)
    shared["wout"] = np.stack([f(inp["pool_w_out"])[0].reshape(16, 128, 2048), f(inp["attn_w_out"])[0].reshape(16, 128, 2048)], axis=0)
    w1 = f(inp["attn_w_in"])[0]
    kcols = []
    for g in range(3):
        kc = w1[:, 1536 + g * 64:1536 + (g + 1) * 64]
        kcols.append(np.concatenate([kc, kc], axis=1))
    w1c = np.concatenate(kcols + [w1[:, 0:1536], w1[:, 1920:2432]], axis=1)
    shared["win1"] = _pieces(w1c, 128)
    wv = w1[:, 1728:1920]
    shared["wv1"] = np.ascontiguousarray(wv.reshape(16, 128, 192).transpose(1, 0, 2)).reshape(128, 16 * 192)
    shared["nsink"] = np.ascontiguousarray(np.broadcast_to(-f(inp["attn_sink"])[0][None, :], (128, 24)))
    wr = f(inp["router_w"])
    shared["wr"] = np.ascontiguousarray(wr.reshape(2, 16, 128, 16).transpose(0, 2, 1, 3)).reshape(2, 128, 256)
    wg = inp["exp_w_gate"]
    wu = inp["exp_w_up"]
    wd = inp["exp_w_down"]
    if slim:
        wg, wu, wd = wg[:slim[0], :slim[1]], wu[:slim[0], :slim[1]], wd[:slim[0], :slim[1]]
    wg, wu, wd = f(wg), f(wu), f(wd)
    nl_, ne_ = wg.shape[0], wg.shape[1]
    shared["wg"] = np.ascontiguousarray(wg.reshape(nl_, ne_, 16, 128, 2, 512).transpose(0, 1, 4, 3, 2, 5)).reshape(nl_, ne_, 2, 128, 8192)
    shared["wu"] = np.ascontiguousarray(wu.reshape(nl_, ne_, 16, 128, 2, 512).transpose(0, 1, 4, 3, 2, 5)).reshape(nl_, ne_, 2, 128, 8192)
    shared["wd"] = np.ascontiguousarray(wd.reshape(nl_, ne_, 8, 128, 4, 512).transpose(0, 1, 4, 3, 2, 5)).reshape(nl_, ne_, 4, 128, 4096)
    identb, identf, rot, invf, mask, pedge = _consts()
    shared["c_identb"] = identb
    shared["c_identf"] = identf
    shared["c_rot"] = rot
    shared["c_invf"] = invf
    shared["c_mask"] = mask
    shared["c_pedge"] = pedge
    return shared


_NC_CACHE = {}


def kernel(x, mem, positions, norm_mix_g, norm_ffn_g, mem_norm_g, final_g, mem_w_kv,
           pool_w_in, pool_group_w, pool_scale, pool_w_out,
           attn_w_in, attn_sink, attn_w_out,
           router_w, exp_w_gate, exp_w_up, exp_w_down):
    inp = dict(norm_mix_g=norm_mix_g, norm_ffn_g=norm_ffn_g, mem_norm_g=mem_norm_g, final_g=final_g,
               mem_w_kv=mem_w_kv, pool_w_in=pool_w_in, pool_group_w=pool_group_w, pool_scale=pool_scale,
               pool_w_out=pool_w_out, attn_w_in=attn_w_in, attn_sink=attn_sink, attn_w_out=attn_w_out,
               router_w=router_w, exp_w_gate=exp_w_gate, exp_w_up=exp_w_up, exp_w_down=exp_w_down)
    shared = _host_layout(inp)
    x = np.asarray(x, dtype=np.float32)
    mem = np.asarray(mem, dtype=np.float32)
    positions = np.asarray(positions, dtype=np.int32)
    if "nc" not in _NC_CACHE:
        _NC_CACHE["nc"] = build()
    nc = _NC_CACHE["nc"]
    n = 8
    in_maps = []
    for b in range(n):
        m = dict(shared)
        m["x"] = np.ascontiguousarray(x[b])
        m["mem"] = np.ascontiguousarray(mem[b])
        m["pos"] = np.ascontiguousarray(positions[b].reshape(1, S_LEN))
        in_maps.append(m)
    res = run_bass_kernel_spmd(nc, in_maps, core_ids=list(range(n)))
    return np.stack([r["y"] for r in res.results], axis=0)
```

```python
import numpy as np
from contextlib import ExitStack
import ml_dtypes
import concourse.bass as bass
import concourse.mybir as mybir
from concourse.bass_utils import run_bass_kernel_spmd

dt = mybir.dt
F32 = dt.float32
BF = dt.bfloat16
I32 = dt.int32
U32 = dt.uint32
AF = mybir.ActivationFunctionType
ALU = mybir.AluOpType
AX = mybir.AxisListType

D = 2048
S_LEN = 2048
NT = 16
NDC = 16
MEM = 256
NE = 16
CAP = 256
DFF = 1024
EPS = 1e-6
ENGS = ("sp", "act", "pool", "dve", "pe")


STATS = {}


class Op:
    __slots__ = ("eng", "fn", "deps", "dma", "needed", "sem", "val", "idx")


class Sched:
    def __init__(self, nc):
        self.nc = nc
        self.ops = {e: [] for e in ENGS}
        self.last_w = {}
        self.readers = {}
        self.dma_cnt = {}
        self.dma_last = {}
        self.dma_prev = {}
        self.n = 0

    def _new(self, eng, fn, dma):
        op = Op()
        op.eng = eng
        op.fn = fn
        op.dma = dma
        op.needed = False
        op.sem = None
        op.val = 0
        op.idx = self.n
        op.deps = []
        self.n += 1
        return op

    def add(self, eng, fn, r=(), w=(), dma=None):
        op = self._new(eng, fn, dma)
        deps = {}
        for x in r:
            lw = self.last_w.get(x)
            if lw is not None:
                deps[lw.idx] = lw
        for x in w:
            lw = self.last_w.get(x)
            if lw is not None:
                deps[lw.idx] = lw
            for rd in self.readers.get(x, ()):
                deps[rd.idx] = rd
        best = {}
        out = []
        for d in deps.values():
            if d.dma is not None:
                out.append(d)
            else:
                if d.eng == "pe" and eng == "pe" and dma is None:
                    continue
                b = best.get(d.eng)
                if b is None or d.idx > b.idx:
                    best[d.eng] = d
        out.extend(best.values())
        if dma is not None:
            pv_ = self.dma_prev.get(dma)
            if pv_ is not None and all(pv_ is not d for d in out):
                out.append(pv_)
            self.dma_prev[dma] = op
        op.deps = out
        for d in out:
            d.needed = True
        for x in r:
            if not (isinstance(x, str) and x.startswith("_")):
                self.readers.setdefault(x, []).append(op)
        for x in w:
            self.last_w[x] = op
            self.readers[x] = []
        if dma is not None:
            c = self.dma_cnt.get(dma, 0) + 1
            self.dma_cnt[dma] = c
            op.val = 16 * c
            self.dma_last[dma] = op
        self.ops[eng].append(op)
        return op

    def barrier(self):
        lasts = []
        for e in ENGS:
            for op in reversed(self.ops[e]):
                if op.dma is None and op.fn is not None:
                    lasts.append(op)
                    break
        dl = list(self.dma_last.values())
        for e in ENGS:
            op = self._new(e, None, None)
            op.deps = [x for x in lasts] + dl
            for d in op.deps:
                d.needed = True
            self.ops[e].append(op)
        self.last_w = {}
        self.readers = {}
        self.dma_last = {}

    def emit(self, es, final_dma_keys=()):
        nc = self.nc
        esem = {e: es.enter_context(nc.semaphore("s_" + e)) for e in ENGS}
        dsem = {}
        for i, k in enumerate(self.dma_cnt):
            dsem[k] = es.enter_context(nc.semaphore("d%d" % i))
        for e in ENGS:
            c = 0
            for op in self.ops[e]:
                if op.dma is not None:
                    op.sem = dsem[op.dma]
                else:
                    op.sem = esem[e]
                    if op.needed:
                        c += 1
                        op.val = c
            STATS[e] = (c, len(self.ops[e]))
        STATS["dma_max"] = max(16 * v for v in self.dma_cnt.values())
        STATS["n_dma_keys"] = len(self.dma_cnt)
        ops = self.ops
        dma_cnt = self.dma_cnt

        def run(e, eng):
            waited = {}
            for op in ops[e]:
                for d in op.deps:
                    key = id(d.sem)
                    if waited.get(key, 0) < d.val:
                        eng.wait_ge(d.sem, d.val)
                        waited[key] = d.val
                if op.fn is None:
                    continue
                ins = op.fn(eng)
                if op.dma is not None:
                    ins.then_inc(op.sem, 16)
                elif op.needed:
                    ins.then_inc(op.sem, 1)
            if e == "sp":
                for k in final_dma_keys:
                    eng.wait_ge(dsem[k], 16 * dma_cnt[k])

        with nc.Block() as block:
            @block.sync
            def _(eng):
                run("sp", eng)

            @block.scalar
            def _(eng):
                run("act", eng)

            @block.gpsimd
            def _(eng):
                run("pool", eng)

            @block.vector
            def _(eng):
                run("dve", eng)

            @block.tensor
            def _(eng):
                run("pe", eng)


ARENA_BYTES = 206 * 1024


class Arena:
    def __init__(self, ap):
        self.ap = ap
        self.off = 0

    def mark(self):
        return self.off

    def reset(self, m):
        self.off = m

    def alloc(self, shape, dtype, parts=128):
        esz = {F32: 4, BF: 2, I32: 4, U32: 4}[dtype]
        n = 1
        for s in shape:
            n *= s
        nbytes = (n * esz + 31) // 32 * 32
        assert self.off + nbytes <= ARENA_BYTES, ("SBUF overflow", self.off, nbytes)
        v = self.ap[0:parts, self.off:self.off + n * esz].bitcast(dtype)
        self.off += nbytes
        if len(shape) == 2:
            v = v.rearrange("p (a b) -> p a b", a=shape[0])
        elif len(shape) == 3:
            v = v.rearrange("p (a b c) -> p a b c", a=shape[0], b=shape[1])
        return v


def build(stop_after=None, debug=False, slim=False, dump=None, only=None):
    nc = bass.Bass("TRN2", target_bir_lowering=False)

    def din(name, shape, dtype=F32):
        return nc.dram_tensor(name, list(shape), dtype, kind="ExternalInput").ap()

    x_d = din("x", [S_LEN, D])
    mem_d = din("mem", [MEM, D])
    pos_d = din("pos", [1, S_LEN], I32)
    gains_d = din("gains", [6, D])
    wkv_d = din("wkv", [2, 4, 128, 4 * 1024])
    win0_d = din("win0", [16, 128, 16 * 128])
    wgrp_d = din("wgrp", [4, 128, 3 * 384])
    pscale_d = din("pscale", [128, 12])
    wout_d = din("wout", [2, 16, 128, 2048])
    win1_d = din("win1", [19, 128, 16 * 128])
    wv1_d = din("wv1", [128, 16 * 192])
    sink_d = din("nsink", [128, 24])
    wr_d = din("wr", [2, 128, 16 * 16])
    nl_, ne_ = slim if slim else (2, NE)
    wg_d = din("wg", [nl_, ne_, 2, 128, 16 * 512])
    wu_d = din("wu", [nl_, ne_, 2, 128, 16 * 512])
    wd_d = din("wd", [nl_, ne_, 4, 128, 8 * 512])
    cidb_d = din("c_identb", [128, 128], BF)
    cidf_d = din("c_identf", [128, 128])
    crot_d = din("c_rot", [128, 128])
    cinvf_d = din("c_invf", [128, 2])
    cmask_d = din("c_mask", [128, 2 * 384], BF)
    cpedge_d = din("c_pedge", [128, 4 * 16])

    y_d = nc.dram_tensor("y", [S_LEN, D], F32, kind="ExternalOutput").ap()
    kind_dbg = "ExternalOutput" if debug else "Internal"
    xres_d = nc.dram_tensor("xres", [S_LEN, D], F32, kind=kind_dbg).ap()
    hrow_d = nc.dram_tensor("hrow", [S_LEN, D], BF, kind="Internal").ap()
    cat_d = nc.dram_tensor("catT", [16, 128, S_LEN], BF, kind=kind_dbg).ap()
    if debug:
        dbg_d = nc.dram_tensor("dbg", [128, 4096], F32, kind="ExternalOutput").ap()

    es = ExitStack()
    arena_t = es.enter_context(nc.sbuf_tensor("arena", [128, ARENA_BYTES], dt.uint8))
    A = Arena(arena_t)
    psb = [es.enter_context(nc.psum_tensor("ps%d" % i, [128, 512], F32)) for i in range(8)]
    S = Sched(nc)
    uid = [0]

    def PS(i):
        return ("ps", i)

    def psf(i, n=512):
        return psb[i][:, 0:n]

    def psbf(i):
        return psb[i][:, :].bitcast(BF)

    def dma(q, out, in_, r, w, key):
        return S.add(q, lambda e: e.dma_start(out=out, in_=in_), r=r, w=w, dma=key)

    def dump_buf(ap, ncols, col_off, res):
        st = A.alloc([ncols], F32)
        S.add("dve", lambda e: e.tensor_copy(out=st, in_=ap), r=res, w=["dumpst"])
        dma("sp", dbg_d[:, col_off:col_off + ncols], st, ["dumpst"], ["dbgd"], "dumpd")

    identb = A.alloc([128], BF)
    identf = A.alloc([128], F32)
    mem_nT = A.alloc([16, MEM], BF)
    gbc = A.alloc([D], F32)
    nsink = A.alloc([24], F32)
    pscale = A.alloc([12], F32)
    idxT = A.alloc([2, 16], U32)
    gT = A.alloc([2, 16], F32)
    stat = A.alloc([64], F32)
    dma("sp", identb, cidb_d, [], ["_identb"], "c0")
    dma("sp", identf, cidf_d, [], ["_identf"], "c1")
    dma("sp", nsink, sink_d, [], ["_nsink"], "c2")
    dma("sp", pscale, pscale_d, [], ["_pscale"], "c3")
    PMARK = A.mark()

    def load_gain(i):
        dma("sp", gbc, gains_d[i:i + 1, :].partition_broadcast(128), [], ["gbc"], "gbc")

    def norm_tile(xt, xt_res, h_out, h_res, junk, sidx):
        ss = stat[:, sidx:sidx + 1]
        rs = stat[:, sidx + 1:sidx + 2]
        S.add("act", lambda e: e.activation(out=junk, in_=xt, func=AF.Square, accum_out=ss),
              r=[xt_res], w=["junk", ("stat", sidx)])
        S.add("act", lambda e: e.activation(out=ss, in_=ss, func=AF.Sqrt, scale=1.0 / D, bias=EPS),
              r=[("stat", sidx)], w=[("stat", sidx)])
        S.add("dve", lambda e: e.reciprocal(out=rs, in_=ss), r=[("stat", sidx)], w=[("stat", sidx + 1)])
        S.add("dve", lambda e: e.scalar_tensor_tensor(out=h_out, in0=xt, scalar=rs, in1=gbc,
                                                      op0=ALU.mult, op1=ALU.mult),
              r=[xt_res, ("stat", sidx + 1), "gbc"], w=[h_res])

    def transpose_tile(h_tm, h_res, dst3, dst_res, pbanks, ncols=128, evac="act"):
        for half in range(2):
            bank = pbanks[half]
            pv = psbf(bank)[:, 0:8 * 128].rearrange("p (a b) -> p a b", a=8)

            def tr(e, half=half, pv=pv):
                last = None
                for k in range(8):
                    dc = half * 8 + k
                    last = e.transpose(out=pv[:, k, 0:ncols], in_=h_tm[0:ncols, dc * 128:(dc + 1) * 128],
                                       identity=identb[0:ncols, 0:ncols])
                return last
            S.add("pe", tr, r=[h_res, "_identb"], w=[PS(bank)])
            dsl = dst3[:, half * 8:(half + 1) * 8, :]
            if evac == "act":
                S.add("act", lambda e, dsl=dsl, pv=pv: e.copy(out=dsl, in_=pv[:, :, 0:ncols]),
                      r=[PS(bank)], w=[(dst_res, half)])
            else:
                S.add("dve", lambda e, dsl=dsl, pv=pv: e.tensor_copy(out=dsl, in_=pv[:, :, 0:ncols]),
                      r=[PS(bank)], w=[(dst_res, half)])

    def stage_mem():
        m0 = A.mark()
        xt = A.alloc([D], F32)
        hm = A.alloc([D], BF)
        junk = A.alloc([D], BF)
        load_gain(4)
        for mt in range(2):
            dma("sp", xt, mem_d[mt * 128:(mt + 1) * 128, :], [], ["xt"], "xt")
            norm_tile(xt, "xt", hm, "hm", junk, 0)
            transpose_tile(hm, "hm", mem_nT[:, :, mt * 128:(mt + 1) * 128], ("memT", mt), (0, 1))
        S.barrier()
        A.reset(m0)

    def stage_norm_hT(src_d, gain_idx, hT):
        m0 = A.mark()
        NB = 4
        xts = [A.alloc([D], F32) for _ in range(NB)]
        hms = [A.alloc([D], BF) for _ in range(NB)]
        junk = A.alloc([D], BF)
        load_gain(gain_idx)

        def pa(t):
            b = t % NB
            dma("sp", xts[b], src_d[t * 128:(t + 1) * 128, :], [], [("xt", b)], ("xt", b))
            norm_tile(xts[b], ("xt", b), hms[b], ("hm", b), junk, 2 * b)

        def pb(t):
            b = t % NB
            transpose_tile(hms[b], ("hm", b), hT[:, :, t * 128:(t + 1) * 128], ("hT", t),
                           (2 * (t % 2), 2 * (t % 2) + 1), evac=("act" if t % 2 == 0 else "dve"))
        pa(0)
        pa(1)
        for t in range(NT):
            if t + 2 < NT:
                pa(t + 2)
            pb(t)
        S.barrier()
        A.reset(m0)

    def mem_kv(layer, kT, V):
        m0 = A.mark()
        wk = [A.alloc([4, 1024], BF) for _ in range(4)]
        for pc in range(4):
            dma("pool", wk[pc], wkv_d[layer, pc].rearrange("p (a b) -> p a b", a=4), [], [("wkv", pc)], ("wkv", pc))
        wres = [("wkv", pc) for pc in range(4)] + [(("memT", m), h) for m in range(2) for h in range(2)]
        for h in range(4):
            def mmk(e, h=h):
                last = None
                o = psb[4 + h // 2][:, (h % 2) * 256:(h % 2) * 256 + 256]
                for dc in range(16):
                    last = e.matmul(o, wk[dc // 4][:, dc % 4, h * 128:(h + 1) * 128], mem_nT[:, dc, :],
                                    start=(dc == 0), stop=(dc == 15))
                return last
            S.add("pe", mmk, r=wres, w=[PS(4 + h // 2)])
        for mt in range(2):
            def mmv(e, mt=mt):
                last = None
                for dc in range(16):
                    last = e.matmul(psb[6 + mt][:, :], mem_nT[:, dc, mt * 128:(mt + 1) * 128],
                                    wk[dc // 4][:, dc % 4, 512:1024], start=(dc == 0), stop=(dc == 15))
                return last
            S.add("pe", mmv, r=wres, w=[PS(6 + mt)])
        for h in range(4):
            if dump in ("hT2a", "hT2b"):
                break
            S.add("act", lambda e, h=h: e.copy(out=kT[:, h, :], in_=psb[4 + h // 2][:, (h % 2) * 256:(h % 2) * 256 + 256]),
                  r=[PS(4 + h // 2)], w=[("kT", h)])
        for mt in range(2):
            if dump in ("hT2a", "hT2b"):
                break
            S.add("dve", lambda e, mt=mt: e.tensor_copy(out=V[:, mt, :], in_=psb[6 + mt][:, :]),
                  r=[PS(6 + mt)], w=[("V", mt)])
        S.barrier()
        A.reset(m0)

    def proj_chunk(wsrc, hT, wbufs, ci, evac, banks):
        b = ci % len(wbufs)
        wb = wbufs[b]
        dma("pool", wb, wsrc.rearrange("p (a b) -> p a b", a=16), [], [("wb", b)], ("wb", b))
        for ts in range(4):
            bank = banks[ts % len(banks)]

            def mm(e, ts=ts, bank=bank, wb=wb):
                last = None
                for dc in range(16):
                    last = e.matmul(psb[bank][:, :], wb[:, dc, :], hT[:, dc, ts * 512:(ts + 1) * 512],
                                    start=(dc == 0), stop=(dc == 15))
                return last
            S.add("pe", mm, r=[("wb", b)] + [(("hT", t), h) for t in range(ts * 4, ts * 4 + 4) for h in range(2)],
                  w=[PS(bank)])
            evac(ts, bank)

    def xattn_head(h, qT, q_res, kT, V, cst, cst_res, banks):
        sA, sB, bT, bO = banks
        scale = 128.0 ** -0.5
        m0 = A.mark()
        pbuf = [A.alloc([MEM], BF) for _ in range(2)]
        pTb = [A.alloc([2, 128], BF) for _ in range(2)]

        def pa(t):
            b = t % 2
            bank = sA if b == 0 else sB
            sps = psb[bank][:, 0:256]
            mx = stat[:, 8 + b:9 + b]
            sm = stat[:, 10 + b:11 + b]
            S.add("pe", lambda e: e.matmul(sps, qT[:, t * 128:(t + 1) * 128], kT[:, h, :], start=True, stop=True),
                  r=[q_res, ("kT", h)], w=[PS(bank)])
            S.add("dve", lambda e: e.tensor_reduce(out=mx, in_=sps, axis=AX.X, op=ALU.max), r=[PS(bank)], w=[("xs", b)])
            S.add("dve", lambda e: e.tensor_scalar(out=mx, in0=mx, scalar1=-scale, scalar2=None, op0=ALU.mult),
                  r=[("xs", b)], w=[("xs", b)])
            S.add("act", lambda e: e.activation(out=pbuf[b], in_=sps, func=AF.Exp, bias=mx, scale=scale, accum_out=sm),
                  r=[PS(bank), ("xs", b)], w=[("xp", b), ("xsm", b)])

        def pb(t):
            b = t % 2
            sm = stat[:, 10 + b:11 + b]
            S.add("dve", lambda e: e.reciprocal(out=sm, in_=sm), r=[("xsm", b)], w=[("xsm", b)])
            S.add("dve", lambda e: e.tensor_scalar(out=pbuf[b], in0=pbuf[b], scalar1=sm, scalar2=None, op0=ALU.mult),
                  r=[("xp", b), ("xsm", b)], w=[("xp", b)])
            pv = psbf(bT)[:, 0:256].rearrange("p (a c) -> p a c", a=2)

            def tr(e):
                last = None
                for mt in range(2):
                    last = e.transpose(out=pv[:, mt, :], in_=pbuf[b][:, mt * 128:(mt + 1) * 128], identity=identb)
                return last
            S.add("pe", tr, r=[("xp", b), "_identb"], w=[PS(bT)])
            S.add("act", lambda e: e.copy(out=pTb[b], in_=pv), r=[PS(bT)], w=[("xpT", b)])

        def pc(t):
            b = t % 2
            ops_ = psb[bO][:, 0:128]

            def pvmm(e):
                last = None
                for mt in range(2):
                    last = e.matmul(ops_, V[:, mt, h * 128:(h + 1) * 128], pTb[b][:, mt, :], start=(mt == 0), stop=(mt == 1))
                return last
            S.add("pe", pvmm, r=[("xpT", b), ("V", 0), ("V", 1)], w=[PS(bO)])
            S.add("dve", lambda e: e.tensor_copy(out=cst[:, t * 128:(t + 1) * 128], in_=ops_), r=[PS(bO)], w=[cst_res])

        for s_ in range(NT + 2):
            if s_ < NT:
                pa(s_)
            if 0 <= s_ - 1 < NT:
                pb(s_ - 1)
            if 0 <= s_ - 2 < NT:
                pc(s_ - 2)
        A.reset(m0)

    def stage_mixer0():
        m0 = A.mark()
        hT = A.alloc([16, S_LEN], BF)
        kT = A.alloc([4, MEM], BF)
        V = A.alloc([2, 512], BF)
        stage_norm_hT(x_d, 0, hT)
        if dump == "hT":
            rr = [(("hT", t), h) for t in range(NT) for h in range(2)]
            dump_buf(hT[:, 0, :], 2048, 0, rr)
            dump_buf(hT[:, 15, :], 2048, 2048, rr)
            S.barrier()
            return
        mem_kv(0, kT, V)
        if dump in ("hT2", "hT2a", "hT2b"):
            rr = []
            dump_buf(hT[:, 0, :], 2048, 0, rr)
            dump_buf(hT[:, 15, :], 2048, 2048, rr)
            S.barrier()
            return
        L = S_LEN + 16
        Us = [A.alloc([L], F32) for _ in range(2)]
        B1 = A.alloc([L], F32)
        B2 = A.alloc([L], F32)
        pooled = A.alloc([3, S_LEN], BF)
        wbufs = [A.alloc([16, 128], BF) for _ in range(3)]
        wgrp = A.alloc([4, 3 * 384], BF)
        pedge = A.alloc([4, 16], F32)
        tmp8 = A.alloc([16], F32)
        cst = [A.alloc([S_LEN], BF) for _ in range(2)]
        qx = [A.alloc([S_LEN], BF) for _ in range(2)]
        dma("sp", pedge, cpedge_d.rearrange("p (a b) -> p a b", a=4), [], ["_pedge"], "c4")
        for g in range(4):
            dma("pool", wgrp[:, g, :], wgrp_d[g], [], ["_wgrp"], "wgrp")
        for U in Us:
            S.add("dve", lambda e, U=U: e.memset(U[:, 0:8], 0.0), w=["Upad"])
            S.add("dve", lambda e, U=U: e.memset(U[:, 8 + S_LEN:L], 0.0), w=["Upad"])
        S.add("dve", lambda e: e.memset(B1[:, L - 8:L], 0.0), w=["Bpad"])
        S.add("dve", lambda e: e.memset(B2[:, L - 8:L], 0.0), w=["Bpad"])
        cat_n = [0]

        def store_cat(chunk, buf_i):
            dma("sp", cat_d[chunk], cst[buf_i], [("cst", buf_i)], [("catd", chunk)], ("cstst", buf_i))

        def group_linear(g):
            for oc in range(3):
                bi = cat_n[0] % 2
                cat_n[0] += 1
                for ts in range(4):
                    bank = 4 + (ts % 2)

                    def mm(e, oc=oc, ts=ts, bank=bank, g=g):
                        last = None
                        for c3 in range(3):
                            last = e.matmul(psb[bank][:, :], wgrp[:, g, c3 * 384 + oc * 128:c3 * 384 + (oc + 1) * 128],
                                            pooled[:, c3, ts * 512:(ts + 1) * 512], start=(c3 == 0), stop=(c3 == 2))
                        return last
                    S.add("pe", mm, r=["_wgrp"] + [("pooled", c3) for c3 in range(3)], w=[PS(bank)])
                    col = g * 3 + oc
                    S.add("act", lambda e, bi=bi, ts=ts, bank=bank, col=col: e.mul(
                        out=cst[bi][:, ts * 512:(ts + 1) * 512], in_=psb[bank][:, :], mul=pscale[:, col:col + 1]),
                        r=[PS(bank), "_pscale"], w=[("cst", bi)])
                store_cat(g * 3 + oc, bi)

        for ec in range(16):
            g = ec // 3
            if ec < 12:
                ui = ec % 2
                U = Us[ui]

                def evac(ts, bank, U=U, ui=ui):
                    S.add("act", lambda e, ts=ts, bank=bank: e.copy(out=U[:, 8 + ts * 512:8 + (ts + 1) * 512], in_=psb[bank][:, :]),
                          r=[PS(bank)], w=[("U", ui, ts)])
                proj_chunk(win0_d[ec], hT, wbufs, ec, evac, (0, 1, 2, 3))
                if ec % 3 == 0 and ec > 0:
                    group_linear(ec // 3 - 1)
                k = g + 1
                half = 1 << g
                w_ = 2 * half
                src = U
                src_r = [("U", ui, ts) for ts in range(4)] + ["Upad"]
                cur = None
                for step in range(k):
                    sh = 1 << step
                    dst = B1 if (step % 2 == 0) else B2
                    dres = "B1" if (step % 2 == 0) else "B2"
                    n = L - 8 if step > 0 else L - 1
                    n = L - sh - (0 if step == 0 else 0)
                    nn = L - sh
                    eng = "dve"
                    S.add(eng, lambda e, dst=dst, src=src, sh=sh, nn=nn: e.tensor_tensor(out=dst[:, 0:nn], in0=src[:, 0:nn], in1=src[:, sh:sh + nn], op=ALU.add),
                          r=src_r + ["Bpad"], w=[dres])
                    src = dst
                    src_r = [dres]
                off = 8 - half
                cc = ec % 3
                S.add("dve", lambda e, src=src, off=off, w_=w_, cc=cc, U=U: e.scalar_tensor_tensor(
                    out=pooled[:, cc, :], in0=src[:, off:off + S_LEN], scalar=1.0 / w_, in1=U[:, 8:8 + S_LEN],
                    op0=ALU.mult, op1=ALU.subtract), r=src_r + [("U", ui, ts) for ts in range(4)], w=[("pooled", cc)])
                for (c0, pe0) in ((0, 0), (S_LEN - 8, 8)):
                    S.add("dve", lambda e, src=src, off=off, c0=c0, pe0=pe0, g=g: e.tensor_tensor(
                        out=tmp8[:, 0:8], in0=src[:, off + c0:off + c0 + 8], in1=pedge[:, g, pe0:pe0 + 8], op=ALU.mult),
                        r=src_r + ["_pedge"], w=["tmp8"])
                    S.add("dve", lambda e, c0=c0, cc=cc, U=U: e.tensor_tensor(
                        out=pooled[:, cc, c0:c0 + 8], in0=tmp8[:, 0:8], in1=U[:, 8 + c0:16 + c0], op=ALU.subtract),
                        r=["tmp8"] + [("U", ui, ts) for ts in range(4)], w=[("pooled", cc)])
            else:
                h = ec - 12
                qb = qx[h % 2]

                def evac(ts, bank, qb=qb, h=h):
                    S.add("act", lambda e, ts=ts, bank=bank: e.copy(out=qb[:, ts * 512:(ts + 1) * 512], in_=psb[bank][:, :]),
                          r=[PS(bank)], w=[("qx", h % 2)])
                proj_chunk(win0_d[ec], hT, wbufs, ec, evac, (0, 1, 2, 3))
                if ec == 12:
                    group_linear(3)
                bi = cat_n[0] % 2
                cat_n[0] += 1
                xattn_head(h, qb, ("qx", h % 2), kT, V, cst[bi], ("cst", bi), (4, 5, 6, 7))
                store_cat(12 + h, bi)
        S.barrier()
        A.reset(m0)

    def stage_outproj(layer, src_d):
        m0 = A.mark()
        catT = A.alloc([16, S_LEN], BF)
        wo = A.alloc([16, D], BF)
        xts = [A.alloc([D], F32) for _ in range(2)]
        for ec in range(16):
            dma("sp", catT[:, ec, :], cat_d[ec], [("catd", ec)], [("catT", ec)], ("catl", ec % 4))
            dma("pool", wo[:, ec, :], wout_d[layer, ec], [], [("wo", ec)], ("wol", ec % 4))
        for t in range(NT):
            b = t % 2
            dma("sp", xts[b], src_d[t * 128:(t + 1) * 128, :], [], [("xo", b)], ("xo", b))
            for ds in range(4):
                bank = 4 * b + ds

                def mm(e, t=t, ds=ds, bank=bank):
                    last = None
                    for ec in range(16):
                        last = e.matmul(psb[bank][:, :], catT[:, ec, t * 128:(t + 1) * 128], wo[:, ec, ds * 512:(ds + 1) * 512],
                                        start=(ec == 0), stop=(ec == 15))
                    return last
                S.add("pe", mm, r=[("catT", ec) for ec in range(16)] + [("wo", ec) for ec in range(16)], w=[PS(bank)])
                S.add("dve", lambda e, b=b, ds=ds, bank=bank: e.tensor_tensor(
                    out=xts[b][:, ds * 512:(ds + 1) * 512], in0=psb[bank][:, :], in1=xts[b][:, ds * 512:(ds + 1) * 512], op=ALU.add),
                    r=[PS(bank), ("xo", b)], w=[("xo", b)])
            dma("sp", xres_d[t * 128:(t + 1) * 128, :], xts[b], [("xo", b)], [("xres", t)], ("xost", b))
        S.barrier()
        A.reset(m0)

    def stage_router(layer, pre=None):
        m0 = A.mark()
        if pre is not None:
            pre()
        NB = 4
        xts = [A.alloc([D], F32) for _ in range(NB)]
        hms = [A.alloc([D], BF) for _ in range(NB)]
        junk = A.alloc([D], BF)
        hTt = [A.alloc([16, 128], BF) for _ in range(2)]
        wr = A.alloc([16, 16], BF)
        affT = A.alloc([S_LEN], F32)
        wkb = A.alloc([S_LEN], F32)
        vals = A.alloc([CAP], F32)
        idxu = A.alloc([CAP], U32)
        idxf = A.alloc([CAP], F32)
        lg = [A.alloc([16], F32) for _ in range(2)]
        load_gain(1 + 2 * layer)
        dma("pool", wr, wr_d[layer].rearrange("p (a b) -> p a b", a=16), [], ["_wr"], "wr")

        def ra(t):
            b = t % NB
            dma("sp", xts[b], xres_d[t * 128:(t + 1) * 128, :], [("xres", t)], [("xt", b)], ("xt", b))
            norm_tile(xts[b], ("xt", b), hms[b], ("hm", b), junk, 2 * b)
            dma("sp", hrow_d[t * 128:(t + 1) * 128, :], hms[b], [("hm", b)], ["hrow"], ("hst", b))

        def rb(t):
            b = t % NB
            c = t % 2
            transpose_tile(hms[b], ("hm", b), hTt[c], ("hTt", c), (2 * c, 2 * c + 1), evac=("act" if c == 0 else "dve"))

        def rc(t):
            c = t % 2
            bank = 4 + c
            mx = stat[:, 16 + c:17 + c]
            sm = stat[:, 18 + c:19 + c]

            def mm(e):
                last = None
                for dc in range(16):
                    last = e.matmul(psb[bank][:, 0:16], hTt[c][:, dc, :], wr[:, dc, :], start=(dc == 0), stop=(dc == 15))
                return last
            S.add("pe", mm, r=[(("hTt", c), 0), (("hTt", c), 1), "_wr"], w=[PS(bank)])
            S.add("dve", lambda e: e.tensor_reduce(out=mx, in_=psb[bank][:, 0:16], axis=AX.X, op=ALU.max),
                  r=[PS(bank)], w=[("rs", c)])
            S.add("dve", lambda e: e.tensor_scalar(out=mx, in0=mx, scalar1=-1.0, scalar2=None, op0=ALU.mult),
                  r=[("rs", c)], w=[("rs", c)])
            S.add("act", lambda e: e.activation(out=lg[c], in_=psb[bank][:, 0:16], func=AF.Exp, bias=mx, scale=1.0, accum_out=sm),
                  r=[PS(bank), ("rs", c)], w=[("lg", c), ("rsm", c)])

        def rd(t):
            c = t % 2
            sm = stat[:, 18 + c:19 + c]
            tb = 6 + c
            S.add("dve", lambda e: e.reciprocal(out=sm, in_=sm), r=[("rsm", c)], w=[("rsm", c)])
            S.add("dve", lambda e: e.tensor_scalar(out=lg[c], in0=lg[c], scalar1=sm, scalar2=None, op0=ALU.mult),
                  r=[("lg", c), ("rsm", c)], w=[("lg", c)])
            S.add("pe", lambda e: e.transpose(out=psb[tb][0:16, 0:128], in_=lg[c], identity=identf),
                  r=[("lg", c), "_identf"], w=[PS(tb)])
            S.add("act", lambda e: e.copy(out=affT[0:16, t * 128:(t + 1) * 128], in_=psb[tb][0:16, 0:128]),
                  r=[PS(tb)], w=["affT"])

        for s_ in range(NT + 4):
            if s_ < NT:
                ra(s_)
            if 0 <= s_ - 2 < NT:
                rb(s_ - 2)
            if 0 <= s_ - 3 < NT:
                rc(s_ - 3)
            if 0 <= s_ - 4 < NT:
                rd(s_ - 4)
        cur = affT
        for it in range(CAP // 8):
            v8 = vals[0:16, it * 8:it * 8 + 8]
            i8 = idxu[0:16, it * 8:it * 8 + 8]
            S.add("dve", lambda e, v8=v8, cur=cur: e.max(out=v8, in_=cur[0:16, :]), r=["affT", "wkb"], w=[("v8", it)])
            S.add("dve", lambda e, v8=v8, i8=i8, cur=cur: e.max_index(out=i8, in_max=v8, in_values=cur[0:16, :]),
                  r=["affT", "wkb", ("v8", it)], w=[("i8", it)])
            if it < CAP // 8 - 1:
                S.add("dve", lambda e, v8=v8, cur=cur: e.match_replace(out=wkb[0:16, :], in_to_replace=v8, in_values=cur[0:16, :], imm_value=-1.0),
                      r=["affT", ("v8", it)], w=["wkb"])
                cur = wkb
        S.add("dve", lambda e: e.tensor_copy(out=idxf[0:16, :], in_=idxu[0:16, :]), r=[("i8", it) for it in range(CAP // 8)], w=["idxf"])
        for ct in range(2):
            S.add("pe", lambda e, ct=ct: e.transpose(out=psb[0][:, ct * 16:(ct + 1) * 16], in_=idxf[0:16, ct * 128:(ct + 1) * 128], identity=identf[0:16, 0:16]),
                  r=["idxf", "_identf"], w=[(PS(0), ct)])
            S.add("pe", lambda e, ct=ct: e.transpose(out=psb[1][:, ct * 16:(ct + 1) * 16], in_=vals[0:16, ct * 128:(ct + 1) * 128], identity=identf[0:16, 0:16]),
                  r=[("v8", it) for it in range(CAP // 8)] + ["_identf"], w=[(PS(1), ct)])
        S.add("dve", lambda e: e.tensor_copy(out=idxT.rearrange("p a b -> p (a b)"), in_=psb[0][:, 0:32]), r=[(PS(0), 0), (PS(0), 1)], w=["idxT"])
        S.add("dve", lambda e: e.tensor_copy(out=gT.rearrange("p a b -> p (a b)"), in_=psb[1][:, 0:32]), r=[(PS(1), 0), (PS(1), 1)], w=["gT"])
        if dump == "rt":
            dump_buf(idxT.rearrange("p a b -> p (a b)"), 32, 0, ["idxT"])
            dump_buf(gT.rearrange("p a b -> p (a b)"), 32, 32, ["gT"])
        S.barrier()
        A.reset(m0)

    def stage_moe(layer):
        m0 = A.mark()
        NBIG, NSM = 4, 5
        big = [A.alloc([16, 512], BF) for _ in range(NBIG)]
        sml = [A.alloc([8, 512], BF) for _ in range(NSM)]
        nb = [0]
        ns = [0]
        slots_of = {}

        def load_big(e_):
            slots = slots_of.setdefault(e_, {})
            for fs in range(2):
                for nm, src in (("g", wg_d), ("u", wu_d)):
                    bi = nb[0] % NBIG
                    nb[0] += 1
                    dma("pool", big[bi], src[layer, e_, fs].rearrange("p (a b) -> p a b", a=16), [], [("big", bi)], ("big", bi))
                    slots[(nm, fs)] = bi

        def load_sml(e_):
            slots = slots_of.setdefault(e_, {})
            for ds in range(4):
                si = ns[0] % NSM
                ns[0] += 1
                dma("pool", sml[si], wd_d[layer, e_, ds].rearrange("p (a b) -> p a b", a=8), [], [("sml", si)], ("sml", si))
                slots[("d", ds)] = si

        stage_router(layer, pre=lambda: (load_big(0), load_sml(0)))
        xg = [[A.alloc([D], BF) for _ in range(2)] for _ in range(2)]
        xgT = [A.alloc([16, CAP], BF) for _ in range(2)]
        act_tm = A.alloc([2, DFF], BF)
        actT = A.alloc([8, CAP], BF)
        sa = [A.alloc([512], F32) for _ in range(2)]
        yo = [A.alloc([D], F32) for _ in range(2)]

        def gather(e_):
            pb = e_ % 2
            for ct in range(2):
                S.add("pool", lambda e, pb=pb, ct=ct, e_=e_: e.indirect_dma_start(
                    out=xg[pb][ct], out_offset=None, in_=hrow_d,
                    in_offset=bass.IndirectOffsetOnAxis(ap=idxT[:, ct, e_:e_ + 1], axis=0)),
                    r=["hrow", "idxT"], w=[("xg", pb, ct)], dma=("xg", pb, ct))

        gather(0)
        for e_ in range(NE):
            pb = e_ % 2
            slots = slots_of[e_]
            if e_ + 1 < NE:
                gather(e_ + 1)
            for ct in range(2):
                transpose_tile(xg[pb][ct], ("xg", pb, ct), xgT[pb][:, :, ct * 128:(ct + 1) * 128], ("xgT", pb, ct),
                               (6, 7), evac=("act" if ct == 0 else "dve"))
            xg_r = [(("xgT", pb, ct), h) for ct in range(2) for h in range(2)]
            for fs in range(2):
                bg = slots[("g", fs)]
                bu = slots[("u", fs)]
                for ct in range(2):
                    pa = ct
                    pu = 2 + ct

                    def mm(e, bw, bank, ct=ct, pb=pb):
                        last = None
                        for dc in range(16):
                            last = e.matmul(psb[bank][:, :], xgT[pb][:, dc, ct * 128:(ct + 1) * 128], big[bw][:, dc, :],
                                            start=(dc == 0), stop=(dc == 15))
                        return last
                    S.add("pe", lambda e, bg=bg, pa=pa, mm=mm: mm(e, bg, pa), r=xg_r + [("big", bg)], w=[PS(pa)])
                    S.add("pe", lambda e, bu=bu, pu=pu, mm=mm: mm(e, bu, pu), r=xg_r + [("big", bu)], w=[PS(pu)])
                    S.add("act", lambda e, ct=ct, pa=pa: e.activation(out=sa[ct], in_=psb[pa][:, :], func=AF.Silu),
                          r=[PS(pa)], w=[("sa", ct)])
                    S.add("dve", lambda e, ct=ct, pu=pu, fs=fs: e.tensor_tensor(out=act_tm[:, ct, fs * 512:(fs + 1) * 512], in0=sa[ct], in1=psb[pu][:, :], op=ALU.mult),
                          r=[("sa", ct), PS(pu)], w=[("act_tm", ct, fs)])
            if e_ + 1 < NE:
                load_big(e_ + 1)
            for ct in range(2):
                pv = psbf(6 + ct)[:, :].rearrange("p (a b) -> p a b", a=8)

                def tr(e, ct=ct, pv=pv):
                    last = None
                    for fc in range(8):
                        last = e.transpose(out=pv[:, fc, :], in_=act_tm[:, ct, fc * 128:(fc + 1) * 128], identity=identb)
                    return last
                S.add("pe", tr, r=[("act_tm", ct, 0), ("act_tm", ct, 1), "_identb"], w=[PS(6 + ct)])
                if ct == 0:
                    S.add("act", lambda e, ct=ct, pv=pv: e.copy(out=actT[:, :, ct * 128:(ct + 1) * 128], in_=pv), r=[PS(6 + ct)], w=[("actT", ct)])
                else:
                    S.add("dve", lambda e, ct=ct, pv=pv: e.tensor_copy(out=actT[:, :, ct * 128:(ct + 1) * 128], in_=pv), r=[PS(6 + ct)], w=[("actT", ct)])
            k = 0
            for ds in range(4):
                si = slots[("d", ds)]
                for ct in range(2):
                    bank = 4 + (k % 2)
                    k += 1

                    def mmd(e, ct=ct, si=si, bank=bank):
                        last = None
                        for fc in range(8):
                            last = e.matmul(psb[bank][:, :], actT[:, fc, ct * 128:(ct + 1) * 128], sml[si][:, fc, :],
                                            start=(fc == 0), stop=(fc == 7))
                        return last
                    S.add("pe", mmd, r=[("actT", 0), ("actT", 1), ("sml", si)], w=[PS(bank)])
                    if ct == 0:
                        S.add("act", lambda e, ct=ct, ds=ds, bank=bank, e_=e_: e.mul(
                            out=yo[ct][:, ds * 512:(ds + 1) * 512], in_=psb[bank][:, :], mul=gT[:, ct, e_:e_ + 1]),
                            r=[PS(bank), "gT"], w=[("yo", ct)])
                    else:
                        S.add("dve", lambda e, ct=ct, ds=ds, bank=bank, e_=e_: e.tensor_scalar(
                            out=yo[ct][:, ds * 512:(ds + 1) * 512], in0=psb[bank][:, :], scalar1=gT[:, ct, e_:e_ + 1], scalar2=None, op0=ALU.mult),
                            r=[PS(bank), "gT"], w=[("yo", ct)])
            if e_ + 1 < NE:
                load_sml(e_ + 1)
            for ct in range(2):
                S.add("pool", lambda e, ct=ct, e_=e_: e.indirect_dma_start(
                    out=xres_d, out_offset=bass.IndirectOffsetOnAxis(ap=idxT[:, ct, e_:e_ + 1], axis=0),
                    in_=yo[ct], in_offset=None, compute_op=ALU.add),
                    r=[("yo", ct), "idxT"], w=["xres_all"], dma=("sc", ct))
        S.barrier()
        A.reset(m0)

    def stage_mixer1():
        m0 = A.mark()
        hT = A.alloc([16, S_LEN], BF)
        kT = A.alloc([4, MEM], BF)
        V = A.alloc([2, 512], BF)
        Ct = A.alloc([S_LEN], F32)
        St = A.alloc([S_LEN], F32)
        invf = A.alloc([2], F32)
        rotT = A.alloc([128], F32)
        stage_norm_hT(xres_d, 2, hT)
        m1 = A.mark()
        posi = A.alloc([S_LEN], I32)
        posf = A.alloc([S_LEN], F32)
        dma("sp", posi, pos_d.partition_broadcast(128), [], ["posi"], "posi")
        dma("sp", invf, cinvf_d, [], ["_invf"], "c5")
        dma("sp", rotT, crot_d, [], ["_rotT"], "c6")
        S.add("dve", lambda e: e.tensor_copy(out=posf, in_=posi), r=["posi"], w=["posf"])
        ki = A.alloc([S_LEN], I32)
        mk = A.alloc([S_LEN], F32)
        SC = 6.283179

        def reduce_(T, res):
            S.add("dve", lambda e: e.tensor_copy(out=ki, in_=T), r=[res], w=["ki"])
            S.add("dve", lambda e: e.tensor_copy(out=mk, in_=ki), r=["ki"], w=["mk"])
            S.add("dve", lambda e: e.tensor_tensor(out=T, in0=T, in1=mk, op=ALU.subtract), r=[res, "mk"], w=[res])
            S.add("dve", lambda e: e.tensor_scalar(out=mk, in0=T, scalar1=0.5, scalar2=None, op0=ALU.is_gt), r=[res], w=["mk"])
            S.add("dve", lambda e: e.tensor_tensor(out=T, in0=T, in1=mk, op=ALU.subtract), r=[res, "mk"], w=[res])
            S.add("dve", lambda e: e.tensor_scalar(out=mk, in0=T, scalar1=-0.5, scalar2=None, op0=ALU.is_lt), r=[res], w=["mk"])
            S.add("dve", lambda e: e.tensor_tensor(out=T, in0=T, in1=mk, op=ALU.add), r=[res, "mk"], w=[res])
            S.add("act", lambda e: e.activation(out=T, in_=T, func=AF.Sin, scale=SC), r=[res], w=[res])
        S.add("dve", lambda e: e.tensor_scalar(out=St, in0=posf, scalar1=invf[:, 0:1], scalar2=None, op0=ALU.mult),
              r=["posf", "_invf"], w=["St"])
        S.add("dve", lambda e: e.tensor_scalar(out=Ct, in0=posf, scalar1=invf[:, 0:1], scalar2=0.25, op0=ALU.mult, op1=ALU.add),
              r=["posf", "_invf"], w=["Ct"])
        reduce_(St, "St")
        reduce_(Ct, "Ct")
        S.add("dve", lambda e: e.tensor_scalar(out=St, in0=St, scalar1=invf[:, 1:2], scalar2=None, op0=ALU.mult), r=["St", "_invf"], w=["St"])
        S.add("dve", lambda e: e.tensor_scalar(out=Ct, in0=Ct, scalar1=-1.0, scalar2=invf[:, 1:2], op0=ALU.add, op1=ALU.mult), r=["Ct", "_invf"], w=["Ct"])
        S.add("dve", lambda e: e.tensor_scalar(out=Ct, in0=Ct, scalar1=1.0, scalar2=None, op0=ALU.add), r=["Ct"], w=["Ct"])
        S.barrier()
        A.reset(m1)
        mem_kv(1, kT, V)
        wbufs = [A.alloc([16, 128], BF) for _ in range(3)]
        wv = A.alloc([16, 192], BF)
        v_tm = A.alloc([NT, 192], BF)
        kT2 = A.alloc([3, S_LEN], BF)
        qf = A.alloc([S_LEN], F32)
        t1 = A.alloc([512], F32)
        qTb = [A.alloc([S_LEN], BF) for _ in range(2)]
        mask = A.alloc([2, 384], BF)
        pb_ = [A.alloc([2, 384], BF) for _ in range(2)]
        pm_ = [A.alloc([2, 384], BF) for _ in range(2)]
        pTs = [A.alloc([6, 128], BF) for _ in range(2)]
        otm = [A.alloc([128], BF) for _ in range(2)]
        cst = [A.alloc([S_LEN], BF) for _ in range(2)]
        qx = [A.alloc([S_LEN], BF) for _ in range(2)]
        dma("sp", mask, cmask_d.rearrange("p (a b) -> p a b", a=2), [], ["_mask"], "c7")
        dma("pool", wv, wv1_d.rearrange("p (a b) -> p a b", a=16), [], ["_wv"], "wv")
        hT_all = [(("hT", t), h) for t in range(NT) for h in range(2)]
        for t in range(NT):
            bank = t % 2

            def mm(e, t=t, bank=bank):
                last = None
                for dc in range(16):
                    last = e.matmul(psb[bank][:, 0:192], hT[:, dc, t * 128:(t + 1) * 128], wv[:, dc, :], start=(dc == 0), stop=(dc == 15))
                return last
            S.add("pe", mm, r=["_wv", (("hT", t), 0), (("hT", t), 1)], w=[PS(bank)])
            S.add("act", lambda e, t=t, bank=bank: e.copy(out=v_tm[:, t, :], in_=psb[bank][:, 0:192]), r=[PS(bank)], w=[("v_tm", t)])

        def rotary_sub(dst, dst_res, ts, hh):
            c0 = ts * 512 + hh * 256
            S.add("pe", lambda e: e.matmul(psb[2][:, 0:256], rotT, qf[:, c0:c0 + 256], start=True, stop=True),
                  r=[("qf", ts), "_rotT"], w=[PS(2)])
            S.add("dve", lambda e: e.tensor_tensor(out=t1[:, 0:256], in0=psb[2][:, 0:256], in1=St[:, c0:c0 + 256], op=ALU.mult),
                  r=[PS(2), "St"], w=["t1a"])
            S.add("pool", lambda e: e.tensor_tensor(out=t1[:, 256:512], in0=qf[:, c0:c0 + 256], in1=Ct[:, c0:c0 + 256], op=ALU.mult),
                  r=[("qf", ts), "Ct"], w=["t1b"])
            S.add("dve", lambda e: e.tensor_tensor(out=dst[:, c0:c0 + 256], in0=t1[:, 0:256], in1=t1[:, 256:512], op=ALU.add),
                  r=["t1a", "t1b"], w=[dst_res])

        def rotary_chunk(dst, dst_res):
            for ts in range(4):
                for hh in range(2):
                    rotary_sub(dst, dst_res, ts, hh)

        nxt = {}

        def next_chunk_item(j1, k):
            if k == 0:
                b = ci[0] % len(wbufs)
                ci[0] += 1
                nxt["wb"] = (wbufs[b], b)
                dma("pool", wbufs[b], win1_d[3 + j1].rearrange("p (a b) -> p a b", a=16), [], [("wb", b)], ("wb", b))
            elif 2 <= k <= 5:
                ts = k - 2
                wb, b = nxt["wb"]

                def mm(e):
                    last = None
                    for dc in range(16):
                        last = e.matmul(psb[2][:, :], wb[:, dc, :], hT[:, dc, ts * 512:(ts + 1) * 512],
                                        start=(dc == 0), stop=(dc == 15))
                    return last
                S.add("pe", mm, r=[("wb", b)], w=[PS(2)])
                evac_qf(ts, 2)
            elif 6 <= k <= 13:
                rotary_sub(qTb[j1 % 2], ("qTb", j1 % 2), (k - 6) // 2, (k - 6) % 2)

        def evac_qf(ts, bank):
            S.add("act", lambda e, ts=ts, bank=bank: e.copy(out=qf[:, ts * 512:(ts + 1) * 512], in_=psb[bank][:, :]),
                  r=[PS(bank)], w=[("qf", ts)])

        ci = [0]
        for g in range(3):
            proj_chunk(win1_d[g], hT, wbufs, ci[0], evac_qf, (0, 1))
            ci[0] += 1
            rotary_chunk(kT2[:, g, :], ("kT2", g))
        cat_n = [0]

        def store_cat(chunk, buf_i):
            dma("sp", cat_d[chunk], cst[buf_i], [("cst", buf_i)], [("catd", chunk)], ("cstst", buf_i))

        NQ = 12 * NT

        def geom(q):
            j, n = q // NT, q % NT
            kb0 = max(n - 1, 0)
            kb1 = min(n + 1, NT - 1)
            nk = (kb1 - kb0 + 1) * 128
            moff = 0 if n > 0 else 128
            return j, n, j // 4, kb0, nk, moff

        def st_cols(q):
            i4 = q % 4
            base = 24 + 6 * i4
            return stat[:, base:base + 2], stat[:, base + 2:base + 4], stat[:, base + 4:base + 6]

        def ph0(q):
            j, n, g, kb0, nk, moff = geom(q)
            ib = q % 2
            i4 = q % 4
            qb = qTb[j % 2]
            qres = ("qTb", j % 2)
            sb0 = 3 if ib == 0 else 0
            mx, sm, es_ = st_cols(q)
            for hh in range(2):
                def qk(e, hh=hh):
                    e.matmul(psb[sb0 + hh][:, 0:nk], qb[hh * 64:(hh + 1) * 64, n * 128:(n + 1) * 128],
                             kT2[hh * 64:(hh + 1) * 64, g, kb0 * 128:kb0 * 128 + nk], start=True, stop=False)
                    return e.matmul(psb[sb0 + hh][:, 0:nk], identb, mask[:, 0, moff:moff + nk], start=False, stop=True)
                S.add("pe", qk, r=[qres, ("kT2", g), "_mask", "_identb"], w=[PS(sb0 + hh)])
            for hh in range(2):
                S.add("dve", lambda e, hh=hh: e.tensor_reduce(out=mx[:, hh:hh + 1], in_=psb[sb0 + hh][:, 0:nk], axis=AX.X, op=ALU.max),
                      r=[PS(sb0 + hh)], w=[("amx", i4, hh)])
            S.add("dve", lambda e: e.scalar_tensor_tensor(out=mx, in0=mx, scalar=-0.125, in1=nsink[:, 2 * j:2 * j + 2], op0=ALU.mult, op1=ALU.min),
                  r=[("amx", i4, 0), ("amx", i4, 1), "_nsink"], w=[("anm", i4)])
            S.add("dve", lambda e: e.tensor_tensor(out=es_, in0=mx, in1=nsink[:, 2 * j:2 * j + 2], op=ALU.subtract),
                  r=[("anm", i4), "_nsink"], w=[("aes", i4)])
            for hh in range(2):
                S.add("act", lambda e, hh=hh: e.activation(
                    out=pm_[ib][:, hh, 0:nk], in_=psb[sb0 + hh][:, 0:nk], func=AF.Exp, bias=mx[:, hh:hh + 1], scale=0.125,
                    accum_out=sm[:, hh:hh + 1]),
                    r=[PS(sb0 + hh), ("anm", i4)], w=[("apm", ib, hh), ("asm", i4, hh)])
            S.add("act", lambda e: e.activation(out=es_, in_=es_, func=AF.Exp), r=[("aes", i4)], w=[("aes", i4)])

        def ph1(q):
            j, n, g, kb0, nk, moff = geom(q)
            ib = q % 2
            i4 = q % 4
            nkb = nk // 128
            mx, sm, es_ = st_cols(q)
            S.add("dve", lambda e: e.tensor_tensor(out=sm, in0=sm, in1=es_, op=ALU.add),
                  r=[("aes", i4), ("asm", i4, 0), ("asm", i4, 1)], w=[("ard", i4)])
            S.add("dve", lambda e: e.reciprocal(out=sm, in_=sm), r=[("ard", i4)], w=[("ard", i4)])
            pv = psbf(7)[:, 0:768].rearrange("p (a b) -> p a b", a=6)

            def tr(e):
                last = None
                for hh in range(2):
                    for kb in range(nkb):
                        last = e.transpose(out=pv[:, hh * 3 + kb, :], in_=pm_[ib][:, hh, kb * 128:(kb + 1) * 128], identity=identb)
                return last
            S.add("pe", tr, r=[("apm", ib, 0), ("apm", ib, 1), "_identb"], w=[(PS(7), 0)])
            if nkb == 3:
                S.add("act", lambda e: e.copy(out=pTs[ib], in_=pv), r=[(PS(7), 0)], w=[("apT", ib)])
            else:
                for hh in range(2):
                    S.add("act", lambda e, hh=hh: e.copy(out=pTs[ib][:, hh * 3:hh * 3 + nkb, :], in_=pv[:, hh * 3:hh * 3 + nkb, :]),
                          r=[(PS(7), 0)], w=[("apT", ib)])

        def ph2(q):
            j, n, g, kb0, nk, moff = geom(q)
            ib = q % 2
            i4 = q % 4
            nkb = nk // 128
            mx, sm, es_ = st_cols(q)
            po = psb[5][:, 0:128]

            def pvm(e):
                last = None
                for hh in range(2):
                    for kb in range(nkb):
                        last = e.matmul(po[:, hh * 64:(hh + 1) * 64], pTs[ib][:, hh * 3 + kb, :], v_tm[:, kb0 + kb, g * 64:(g + 1) * 64],
                                        start=(kb == 0), stop=(kb == nkb - 1))
                return last
            S.add("pe", pvm, r=[("apT", ib)] + [("v_tm", kb0 + kb) for kb in range(nkb)], w=[PS(5)])
            for hh in range(2):
                S.add("act", lambda e, hh=hh: e.mul(
                    out=otm[ib][:, hh * 64:(hh + 1) * 64], in_=po[:, hh * 64:(hh + 1) * 64], mul=sm[:, hh:hh + 1]),
                    r=[PS(5), ("ard", i4)], w=[("otm", ib)])

        def ph3(q):
            j, n, g, kb0, nk, moff = geom(q)
            ib = q % 2
            bi = j % 2
            ft = psbf(6)[:, 0:128]
            S.add("pe", lambda e: e.transpose(out=ft, in_=otm[ib], identity=identb), r=[("otm", ib), "_identb"], w=[PS(6)])
            S.add("dve", lambda e: e.tensor_copy(out=cst[bi][:, n * 128:(n + 1) * 128], in_=ft), r=[PS(6)], w=[("cst", bi)])
            if n == NT - 1:
                store_cat(j, bi)

        proj_chunk(win1_d[3], hT, wbufs, ci[0], evac_qf, (0, 1))
        ci[0] += 1
        rotary_chunk(qTb[0], ("qTb", 0))
        for s_ in range(NQ + 3):
            if s_ < NQ:
                ph0(s_)
                j1 = s_ // NT + 1
                if j1 < 12:
                    next_chunk_item(j1, s_ % NT)
            if 0 <= s_ - 1 < NQ:
                ph1(s_ - 1)
            if 0 <= s_ - 2 < NQ:
                ph2(s_ - 2)
            if 0 <= s_ - 3 < NQ:
                ph3(s_ - 3)
        cat_n[0] = 12
        for h in range(4):
            qb = qx[h % 2]

            def evac(ts, bank, qb=qb, h=h):
                S.add("act", lambda e, ts=ts, bank=bank: e.copy(out=qb[:, ts * 512:(ts + 1) * 512], in_=psb[bank][:, :]),
                      r=[PS(bank)], w=[("qx", h % 2)])
            proj_chunk(win1_d[15 + h], hT, wbufs, ci[0], evac, (0, 1))
            ci[0] += 1
            bi = cat_n[0] % 2
            cat_n[0] += 1
            xattn_head(h, qb, ("qx", h % 2), kT, V, cst[bi], ("cst", bi), (3, 4, 7, 5))
            store_cat(12 + h, bi)
        S.barrier()
        A.reset(m0)

    def stage_final():
        m0 = A.mark()
        xts = [A.alloc([D], F32) for _ in range(2)]
        outs = [A.alloc([D], F32) for _ in range(2)]
        junk = A.alloc([D], BF)
        load_gain(5)
        for t in range(NT):
            b = t % 2
            dma("sp", xts[b], xres_d[t * 128:(t + 1) * 128, :], [], [("xt", b)], ("xt", b))
            norm_tile(xts[b], ("xt", b), outs[b], ("of", b), junk, 2 * b)
            dma("sp", y_d[t * 128:(t + 1) * 128, :], outs[b], [("of", b)], ["y"], ("yst", b))
        A.reset(m0)

    stages = [
        ("mem", stage_mem),
        ("mix0", stage_mixer0),
        ("out0", lambda: stage_outproj(0, x_d)),
        ("ex0", lambda: stage_moe(0)),
        ("mix1", stage_mixer1),
        ("out1", lambda: stage_outproj(1, xres_d)),
        ("ex1", lambda: stage_moe(1)),
        ("final", stage_final),
    ]
    for name, fn in stages:
        if only is not None and name not in only:
            continue
        fn()
        if stop_after == name:
            break
    if stop_after is not None and stop_after != "final":
        S.barrier()
    finals = [k for k in S.dma_cnt if isinstance(k, tuple) and k[0] in ("yst", "xost", "cstst", "sc")]
    S.emit(es, final_dma_keys=finals)
    es.close()
    return nc


def _consts():
    identb = np.eye(128, dtype=np.float32).astype(ml_dtypes.bfloat16)
    identf = np.eye(128, dtype=np.float32)
    rot = np.zeros((128, 128), np.float32)
    for b in (0, 64):
        for r in range(8):
            rot[b + r + 8, b + r] = -1.0
            rot[b + r, b + r + 8] = 1.0
    inv_freq = (500000.0 ** (-np.arange(0, 16, 2, dtype=np.float32) / 16)).astype(np.float32)
    invf = np.zeros((128, 2), np.float32)
    for p in range(128):
        r = p % 64
        if r < 16:
            invf[p, 0] = np.float32(inv_freq[r % 8] / (2.0 * np.pi))
            invf[p, 1] = 1.0
    qq = np.arange(128)[:, None]
    kk = np.arange(128)[None, :]
    m = np.concatenate([(kk >= qq), np.ones((128, 128), bool), (kk <= qq)], axis=1)
    m = np.where(m, 0.0, -30000.0).astype(np.float32)
    mask = np.concatenate([m, m], axis=1).astype(ml_dtypes.bfloat16)
    pedge = np.zeros((4, 16), np.float32)
    t = np.arange(S_LEN)
    for g, w in enumerate((2, 4, 8, 16)):
        lo = np.maximum(t - w // 2, 0)
        hi = np.minimum(t + w // 2 - 1, S_LEN - 1)
        cnt = (hi - lo + 1).astype(np.float32)
        pedge[g, 0:8] = 1.0 / cnt[0:8]
        pedge[g, 8:16] = 1.0 / cnt[S_LEN - 8:]
    pedge = np.broadcast_to(pedge.reshape(1, 64), (128, 64)).copy()
    return identb, identf, rot, invf, mask, pedge


def _pieces(w, pc):
    R, C = w.shape
    n = R // 128
    return np.ascontiguousarray(w.reshape(n, 128, C // pc, pc).transpose(2, 1, 0, 3)).reshape(C // pc, 128, n * pc)


def _host_layout(inp, slim=False):
    f = lambda a: np.ascontiguousarray(np.asarray(a, dtype=np.float32))
    shared = {}
    shared["gains"] = np.stack([f(inp["norm_mix_g"])[0], f(inp["norm_ffn_g"])[0], f(inp["norm_mix_g"])[1],
                                f(inp["norm_ffn_g"])[1], f(inp["mem_norm_g"]), f(inp["final_g"])], axis=0)
    wkv = f(inp["mem_w_kv"])
    shared["wkv"] = np.ascontiguousarray(wkv.reshape(2, 4, 4, 128, 1024).transpose(0, 1, 3, 2, 4)).reshape(2, 4, 128, 4096)
    shared["win0"] = _pieces(f(inp["pool_w_in"])[0], 128)
    gw = f(inp["pool_group_w"])[0]
    shared["wgrp"] = np.ascontiguousarray(gw.reshape(4, 3, 128, 384).transpose(0, 2, 1, 3)).reshape(4, 128, 1152)
    shared["pscale"] = np.ascontiguousarray(f(inp["pool_scale"])[0].reshape(12, 128).T)
    shared["wout"] = np.stack([f(inp["pool_w_out"])[0].reshape(16, 128, 2048), f(inp["attn_w_out"])[0].reshape(16, 128, 2048)], axis=0)
    w1 = f(inp["attn_w_in"])[0]
    kcols = []
    for g in range(3):
        kc = w1[:, 1536 + g * 64:1536 + (g + 1) * 64]
        kcols.append(np.concatenate([kc, kc], axis=1))
    w1c = np.concatenate(kcols + [w1[:, 0:1536], w1[:, 1920:2432]], axis=1)
    shared["win1"] = _pieces(w1c, 128)
    wv = w1[:, 1728:1920]
    shared["wv1"] = np.ascontiguousarray(wv.reshape(16, 128, 192).transpose(1, 0, 2)).reshape(128, 16 * 192)
    shared["nsink"] = np.ascontiguousarray(np.broadcast_to(-f(inp["attn_sink"])[0][None, :], (128, 24)))
    wr = f(inp["router_w"])
    shared["wr"] = np.ascontiguousarray(wr.reshape(2, 16, 128, 16).transpose(0, 2, 1, 3)).reshape(2, 128, 256)
    wg = inp["exp_w_gate"]
    wu = inp["exp_w_up"]
    wd = inp["exp_w_down"]
    if slim:
        wg, wu, wd = wg[:slim[0], :slim[1]], wu[:slim[0], :slim[1]], wd[:slim[0], :slim[1]]
    wg, wu, wd = f(wg), f(wu), f(wd)
    nl_, ne_ = wg.shape[0], wg.shape[1]
    shared["wg"] = np.ascontiguousarray(wg.reshape(nl_, ne_, 16, 128, 2, 512).transpose(0, 1, 4, 3, 2, 5)).reshape(nl_, ne_, 2, 128, 8192)
    shared["wu"] = np.ascontiguousarray(wu.reshape(nl_, ne_, 16, 128, 2, 512).transpose(0, 1, 4, 3, 2, 5)).reshape(nl_, ne_, 2, 128, 8192)
    shared["wd"] = np.ascontiguousarray(wd.reshape(nl_, ne_, 8, 128, 4, 512).transpose(0, 1, 4, 3, 2, 5)).reshape(nl_, ne_, 4, 128, 4096)
    identb, identf, rot, invf, mask, pedge = _consts()
    shared["c_identb"] = identb
    shared["c_identf"] = identf
    shared["c_rot"] = rot
    shared["c_invf"] = invf
    shared["c_mask"] = mask
    shared["c_pedge"] = pedge
    return shared


_NC_CACHE = {}


def kernel(x, mem, positions, norm_mix_g, norm_ffn_g, mem_norm_g, final_g, mem_w_kv,
           pool_w_in, pool_group_w, pool_scale, pool_w_out,
           attn_w_in, attn_sink, attn_w_out,
           router_w, exp_w_gate, exp_w_up, exp_w_down):
    inp = dict(norm_mix_g=norm_mix_g, norm_ffn_g=norm_ffn_g, mem_norm_g=mem_norm_g, final_g=final_g,
               mem_w_kv=mem_w_kv, pool_w_in=pool_w_in, pool_group_w=pool_group_w, pool_scale=pool_scale,
               pool_w_out=pool_w_out, attn_w_in=attn_w_in, attn_sink=attn_sink, attn_w_out=attn_w_out,
               router_w=router_w, exp_w_gate=exp_w_gate, exp_w_up=exp_w_up, exp_w_down=exp_w_down)
    shared = _host_layout(inp)
    x = np.asarray(x, dtype=np.float32)
    mem = np.asarray(mem, dtype=np.float32)
    positions = np.asarray(positions, dtype=np.int32)
    if "nc" not in _NC_CACHE:
        _NC_CACHE["nc"] = build()
    nc = _NC_CACHE["nc"]
    n = 8
    in_maps = []
    for b in range(n):
        m = dict(shared)
        m["x"] = np.ascontiguousarray(x[b])
        m["mem"] = np.ascontiguousarray(mem[b])
        m["pos"] = np.ascontiguousarray(positions[b].reshape(1, S_LEN))
        in_maps.append(m)
    res = run_bass_kernel_spmd(nc, in_maps, core_ids=list(range(n)))
    return np.stack([r["y"] for r in res.results], axis=0)
```

```python
import numpy as np
from contextlib import ExitStack
import ml_dtypes
import concourse.bass as bass
import concourse.mybir as mybir
from concourse.bass_utils import run_bass_kernel_spmd

dt = mybir.dt
F32 = dt.float32
BF = dt.bfloat16
I32 = dt.int32
U32 = dt.uint32
AF = mybir.ActivationFunctionType
ALU = mybir.AluOpType
AX = mybir.AxisListType

D = 2048
S_LEN = 2048
NT = 16
NDC = 16
MEM = 256
NE = 16
CAP = 256
DFF = 1024
EPS = 1e-6
ENGS = ("sp", "act", "pool", "dve", "pe")


STATS = {}


class Op:
    __slots__ = ("eng", "fn", "deps", "dma", "needed", "sem", "val", "idx")


class Sched:
    def __init__(self, nc):
        self.nc = nc
        self.ops = {e: [] for e in ENGS}
        self.last_w = {}
        self.readers = {}
        self.dma_cnt = {}
        self.dma_last = {}
        self.dma_prev = {}
        self.n = 0

    def _new(self, eng, fn, dma):
        op = Op()
        op.eng = eng
        op.fn = fn
        op.dma = dma
        op.needed = False
        op.sem = None
        op.val = 0
        op.idx = self.n
        op.deps = []
        self.n += 1
        return op

    def add(self, eng, fn, r=(), w=(), dma=None):
        op = self._new(eng, fn, dma)
        deps = {}
        for x in r:
            lw = self.last_w.get(x)
            if lw is not None:
                deps[lw.idx] = lw
        for x in w:
            lw = self.last_w.get(x)
            if lw is not None:
                deps[lw.idx] = lw
            for rd in self.readers.get(x, ()):
                deps[rd.idx] = rd
        best = {}
        out = []
        for d in deps.values():
            if d.dma is not None:
                out.append(d)
            else:
                if d.eng == "pe" and eng == "pe" and dma is None:
                    continue
                b = best.get(d.eng)
                if b is None or d.idx > b.idx:
                    best[d.eng] = d
        out.extend(best.values())
        if dma is not None:
            pv_ = self.dma_prev.get(dma)
            if pv_ is not None and all(pv_ is not d for d in out):
                out.append(pv_)
            self.dma_prev[dma] = op
        op.deps = out
        for d in out:
            d.needed = True
        for x in r:
            if not (isinstance(x, str) and x.startswith("_")):
                self.readers.setdefault(x, []).append(op)
        for x in w:
            self.last_w[x] = op
            self.readers[x] = []
        if dma is not None:
            c = self.dma_cnt.get(dma, 0) + 1
            self.dma_cnt[dma] = c
            op.val = 16 * c
            self.dma_last[dma] = op
        self.ops[eng].append(op)
        return op

    def barrier(self):
        lasts = []
        for e in ENGS:
            for op in reversed(self.ops[e]):
                if op.dma is None and op.fn is not None:
                    lasts.append(op)
                    break
        dl = list(self.dma_last.values())
        for e in ENGS:
            op = self._new(e, None, None)
            op.deps = [x for x in lasts] + dl
            for d in op.deps:
                d.needed = True
            self.ops[e].append(op)
        self.last_w = {}
        self.readers = {}
        self.dma_last = {}

    def emit(self, es, final_dma_keys=()):
        nc = self.nc
        esem = {e: es.enter_context(nc.semaphore("s_" + e)) for e in ENGS}
        dsem = {}
        for i, k in enumerate(self.dma_cnt):
            dsem[k] = es.enter_context(nc.semaphore("d%d" % i))
        for e in ENGS:
            c = 0
            for op in self.ops[e]:
                if op.dma is not None:
                    op.sem = dsem[op.dma]
                else:
                    op.sem = esem[e]
                    if op.needed:
                        c += 1
                        op.val = c
            STATS[e] = (c, len(self.ops[e]))
        STATS["dma_max"] = max(16 * v for v in self.dma_cnt.values())
        STATS["n_dma_keys"] = len(self.dma_cnt)
        ops = self.ops
        dma_cnt = self.dma_cnt

        def run(e, eng):
            waited = {}
            for op in ops[e]:
                for d in op.deps:
                    key = id(d.sem)
                    if waited.get(key, 0) < d.val:
                        eng.wait_ge(d.sem, d.val)
                        waited[key] = d.val
                if op.fn is None:
                    continue
                ins = op.fn(eng)
                if op.dma is not None:
                    ins.then_inc(op.sem, 16)
                elif op.needed:
                    ins.then_inc(op.sem, 1)
            if e == "sp":
                for k in final_dma_keys:
                    eng.wait_ge(dsem[k], 16 * dma_cnt[k])

        with nc.Block() as block:
            @block.sync
            def _(eng):
                run("sp", eng)

            @block.scalar
            def _(eng):
                run("act", eng)

            @block.gpsimd
            def _(eng):
                run("pool", eng)

            @block.vector
            def _(eng):
                run("dve", eng)

            @block.tensor
            def _(eng):
                run("pe", eng)


ARENA_BYTES = 206 * 1024


class Arena:
    def __init__(self, ap):
        self.ap = ap
        self.off = 0

    def mark(self):
        return self.off

    def reset(self, m):
        self.off = m

    def alloc(self, shape, dtype, parts=128):
        esz = {F32: 4, BF: 2, I32: 4, U32: 4}[dtype]
        n = 1
        for s in shape:
            n *= s
        nbytes = (n * esz + 31) // 32 * 32
        assert self.off + nbytes <= ARENA_BYTES, ("SBUF overflow", self.off, nbytes)
        v = self.ap[0:parts, self.off:self.off + n * esz].bitcast(dtype)
        self.off += nbytes
        if len(shape) == 2:
            v = v.rearrange("p (a b) -> p a b", a=shape[0])
        elif len(shape) == 3:
            v = v.rearrange("p (a b c) -> p a b c", a=shape[0], b=shape[1])
        return v


def build(stop_after=None, debug=False, slim=False, dump=None, only=None):
    nc = bass.Bass("TRN2", target_bir_lowering=False)

    def din(name, shape, dtype=F32):
        return nc.dram_tensor(name, list(shape), dtype, kind="ExternalInput").ap()

    x_d = din("x", [S_LEN, D])
    mem_d = din("mem", [MEM, D])
    pos_d = din("pos", [1, S_LEN], I32)
    gains_d = din("gains", [6, D])
    wkv_d = din("wkv", [2, 4, 128, 4 * 1024])
    win0_d = din("win0", [16, 128, 16 * 128])
    wgrp_d = din("wgrp", [4, 128, 3 * 384])
    pscale_d = din("pscale", [128, 12])
    wout_d = din("wout", [2, 16, 128, 2048])
    win1_d = din("win1", [19, 128, 16 * 128])
    wv1_d = din("wv1", [128, 16 * 192])
    sink_d = din("nsink", [128, 24])
    wr_d = din("wr", [2, 128, 16 * 16])
    nl_, ne_ = slim if slim else (2, NE)
    wg_d = din("wg", [nl_, ne_, 2, 128, 16 * 512])
    wu_d = din("wu", [nl_, ne_, 2, 128, 16 * 512])
    wd_d = din("wd", [nl_, ne_, 4, 128, 8 * 512])
    cidb_d = din("c_identb", [128, 128], BF)
    cidf_d = din("c_identf", [128, 128])
    crot_d = din("c_rot", [128, 128])
    cinvf_d = din("c_invf", [128, 2])
    cmask_d = din("c_mask", [128, 2 * 384], BF)
    cpedge_d = din("c_pedge", [128, 4 * 16])

    y_d = nc.dram_tensor("y", [S_LEN, D], F32, kind="ExternalOutput").ap()
    kind_dbg = "ExternalOutput" if debug else "Internal"
    xres_d = nc.dram_tensor("xres", [S_LEN, D], F32, kind=kind_dbg).ap()
    hrow_d = nc.dram_tensor("hrow", [S_LEN, D], BF, kind="Internal").ap()
    cat_d = nc.dram_tensor("catT", [16, 128, S_LEN], BF, kind=kind_dbg).ap()
    if debug:
        dbg_d = nc.dram_tensor("dbg", [128, 4096], F32, kind="ExternalOutput").ap()

    es = ExitStack()
    arena_t = es.enter_context(nc.sbuf_tensor("arena", [128, ARENA_BYTES], dt.uint8))
    A = Arena(arena_t)
    psb = [es.enter_context(nc.psum_tensor("ps%d" % i, [128, 512], F32)) for i in range(8)]
    S = Sched(nc)
    uid = [0]

    def PS(i):
        return ("ps", i)

    def psf(i, n=512):
        return psb[i][:, 0:n]

    def psbf(i):
        return psb[i][:, :].bitcast(BF)

    def dma(q, out, in_, r, w, key):
        return S.add(q, lambda e: e.dma_start(out=out, in_=in_), r=r, w=w, dma=key)

    def dump_buf(ap, ncols, col_off, res):
        st = A.alloc([ncols], F32)
        S.add("dve", lambda e: e.tensor_copy(out=st, in_=ap), r=res, w=["dumpst"])
        dma("sp", dbg_d[:, col_off:col_off + ncols], st, ["dumpst"], ["dbgd"], "dumpd")

    identb = A.alloc([128], BF)
    identf = A.alloc([128], F32)
    mem_nT = A.alloc([16, MEM], BF)
    gbc = A.alloc([D], F32)
    nsink = A.alloc([24], F32)
    pscale = A.alloc([12], F32)
    idxT = A.alloc([2, 16], U32)
    gT = A.alloc([2, 16], F32)
    stat = A.alloc([64], F32)
    dma("sp", identb, cidb_d, [], ["_identb"], "c0")
    dma("sp", identf, cidf_d, [], ["_identf"], "c1")
    dma("sp", nsink, sink_d, [], ["_nsink"], "c2")
    dma("sp", pscale, pscale_d, [], ["_pscale"], "c3")
    PMARK = A.mark()

    def load_gain(i):
        dma("sp", gbc, gains_d[i:i + 1, :].partition_broadcast(128), [], ["gbc"], "gbc")

    def norm_tile(xt, xt_res, h_out, h_res, junk, sidx):
        ss = stat[:, sidx:sidx + 1]
        rs = stat[:, sidx + 1:sidx + 2]
        S.add("act", lambda e: e.activation(out=junk, in_=xt, func=AF.Square, accum_out=ss),
              r=[xt_res], w=["junk", ("stat", sidx)])
        S.add("act", lambda e: e.activation(out=ss, in_=ss, func=AF.Sqrt, scale=1.0 / D, bias=EPS),
              r=[("stat", sidx)], w=[("stat", sidx)])
        S.add("dve", lambda e: e.reciprocal(out=rs, in_=ss), r=[("stat", sidx)], w=[("stat", sidx + 1)])
        S.add("dve", lambda e: e.scalar_tensor_tensor(out=h_out, in0=xt, scalar=rs, in1=gbc,
                                                      op0=ALU.mult, op1=ALU.mult),
              r=[xt_res, ("stat", sidx + 1), "gbc"], w=[h_res])

    def transpose_tile(h_tm, h_res, dst3, dst_res, pbanks, ncols=128, evac="act"):
        for half in range(2):
            bank = pbanks[half]
            pv = psbf(bank)[:, 0:8 * 128].rearrange("p (a b) -> p a b", a=8)

            def tr(e, half=half, pv=pv):
                last = None
                for k in range(8):
                    dc = half * 8 + k
                    last = e.transpose(out=pv[:, k, 0:ncols], in_=h_tm[0:ncols, dc * 128:(dc + 1) * 128],
                                       identity=identb[0:ncols, 0:ncols])
                return last
            S.add("pe", tr, r=[h_res, "_identb"], w=[PS(bank)])
            dsl = dst3[:, half * 8:(half + 1) * 8, :]
            if evac == "act":
                S.add("act", lambda e, dsl=dsl, pv=pv: e.copy(out=dsl, in_=pv[:, :, 0:ncols]),
                      r=[PS(bank)], w=[(dst_res, half)])
            else:
                S.add("dve", lambda e, dsl=dsl, pv=pv: e.tensor_copy(out=dsl, in_=pv[:, :, 0:ncols]),
                      r=[PS(bank)], w=[(dst_res, half)])

    def stage_mem():
        m0 = A.mark()
        xt = A.alloc([D], F32)
        hm = A.alloc([D], BF)
        junk = A.alloc([D], BF)
        load_gain(4)
        for mt in range(2):
            dma("sp", xt, mem_d[mt * 128:(mt + 1) * 128, :], [], ["xt"], "xt")
            norm_tile(xt, "xt", hm, "hm", junk, 0)
            transpose_tile(hm, "hm", mem_nT[:, :, mt * 128:(mt + 1) * 128], ("memT", mt), (0, 1))
        S.barrier()
        A.reset(m0)

    def stage_norm_hT(src_d, gain_idx, hT):
        m0 = A.mark()
        NB = 4
        xts = [A.alloc([D], F32) for _ in range(NB)]
        hms = [A.alloc([D], BF) for _ in range(NB)]
        junk = A.alloc([D], BF)
        load_gain(gain_idx)

        def pa(t):
            b = t % NB
            dma("sp", xts[b], src_d[t * 128:(t + 1) * 128, :], [], [("xt", b)], ("xt", b))
            norm_tile(xts[b], ("xt", b), hms[b], ("hm", b), junk, 2 * b)

        def pb(t):
            b = t % NB
            transpose_tile(hms[b], ("hm", b), hT[:, :, t * 128:(t + 1) * 128], ("hT", t),
                           (2 * (t % 2), 2 * (t % 2) + 1), evac=("act" if t % 2 == 0 else "dve"))
        pa(0)
        pa(1)
        for t in range(NT):
            if t + 2 < NT:
                pa(t + 2)
            pb(t)
        S.barrier()
        A.reset(m0)

    def mem_kv(layer, kT, V):
        m0 = A.mark()
        wk = [A.alloc([4, 1024], BF) for _ in range(4)]
        for pc in range(4):
            dma("pool", wk[pc], wkv_d[layer, pc].rearrange("p (a b) -> p a b", a=4), [], [("wkv", pc)], ("wkv", pc))
        wres = [("wkv", pc) for pc in range(4)] + [(("memT", m), h) for m in range(2) for h in range(2)]
        for h in range(4):
            def mmk(e, h=h):
                last = None
                o = psb[4 + h // 2][:, (h % 2) * 256:(h % 2) * 256 + 256]
                for dc in range(16):
                    last = e.matmul(o, wk[dc // 4][:, dc % 4, h * 128:(h + 1) * 128], mem_nT[:, dc, :],
                                    start=(dc == 0), stop=(dc == 15))
                return last
            S.add("pe", mmk, r=wres, w=[PS(4 + h // 2)])
        for mt in range(2):
            def mmv(e, mt=mt):
                last = None
                for dc in range(16):
                    last = e.matmul(psb[6 + mt][:, :], mem_nT[:, dc, mt * 128:(mt + 1) * 128],
                                    wk[dc // 4][:, dc % 4, 512:1024], start=(dc == 0), stop=(dc == 15))
                return last
            S.add("pe", mmv, r=wres, w=[PS(6 + mt)])
        for h in range(4):
            if dump in ("hT2a", "hT2b"):
                break
            S.add("act", lambda e, h=h: e.copy(out=kT[:, h, :], in_=psb[4 + h // 2][:, (h % 2) * 256:(h % 2) * 256 + 256]),
                  r=[PS(4 + h // 2)], w=[("kT", h)])
        for mt in range(2):
            if dump in ("hT2a", "hT2b"):
                break
            S.add("dve", lambda e, mt=mt: e.tensor_copy(out=V[:, mt, :], in_=psb[6 + mt][:, :]),
                  r=[PS(6 + mt)], w=[("V", mt)])
        S.barrier()
        A.reset(m0)

    def proj_chunk(wsrc, hT, wbufs, ci, evac, banks):
        b = ci % len(wbufs)
        wb = wbufs[b]
        dma("pool", wb, wsrc.rearrange("p (a b) -> p a b", a=16), [], [("wb", b)], ("wb", b))
        for ts in range(4):
            bank = banks[ts % len(banks)]

            def mm(e, ts=ts, bank=bank, wb=wb):
                last = None
                for dc in range(16):
                    last = e.matmul(psb[bank][:, :], wb[:, dc, :], hT[:, dc, ts * 512:(ts + 1) * 512],
                                    start=(dc == 0), stop=(dc == 15))
                return last
            S.add("pe", mm, r=[("wb", b)] + [(("hT", t), h) for t in range(ts * 4, ts * 4 + 4) for h in range(2)],
                  w=[PS(bank)])
            evac(ts, bank)

    def xattn_head(h, qT, q_res, kT, V, cst, cst_res, banks):
        sA, sB, bT, bO = banks
        scale = 128.0 ** -0.5
        m0 = A.mark()
        pbuf = [A.alloc([MEM], BF) for _ in range(2)]
        pTb = [A.alloc([2, 128], BF) for _ in range(2)]

        def pa(t):
            b = t % 2
            bank = sA if b == 0 else sB
            sps = psb[bank][:, 0:256]
            mx = stat[:, 8 + b:9 + b]
            sm = stat[:, 10 + b:11 + b]
            S.add("pe", lambda e: e.matmul(sps, qT[:, t * 128:(t + 1) * 128], kT[:, h, :], start=True, stop=True),
                  r=[q_res, ("kT", h)], w=[PS(bank)])
            S.add("dve", lambda e: e.tensor_reduce(out=mx, in_=sps, axis=AX.X, op=ALU.max), r=[PS(bank)], w=[("xs", b)])
            S.add("dve", lambda e: e.tensor_scalar(out=mx, in0=mx, scalar1=-scale, scalar2=None, op0=ALU.mult),
                  r=[("xs", b)], w=[("xs", b)])
            S.add("act", lambda e: e.activation(out=pbuf[b], in_=sps, func=AF.Exp, bias=mx, scale=scale, accum_out=sm),
                  r=[PS(bank), ("xs", b)], w=[("xp", b), ("xsm", b)])

        def pb(t):
            b = t % 2
            sm = stat[:, 10 + b:11 + b]
            S.add("dve", lambda e: e.reciprocal(out=sm, in_=sm), r=[("xsm", b)], w=[("xsm", b)])
            S.add("dve", lambda e: e.tensor_scalar(out=pbuf[b], in0=pbuf[b], scalar1=sm, scalar2=None, op0=ALU.mult),
                  r=[("xp", b), ("xsm", b)], w=[("xp", b)])
            pv = psbf(bT)[:, 0:256].rearrange("p (a c) -> p a c", a=2)

            def tr(e):
                last = None
                for mt in range(2):
                    last = e.transpose(out=pv[:, mt, :], in_=pbuf[b][:, mt * 128:(mt + 1) * 128], identity=identb)
                return last
            S.add("pe", tr, r=[("xp", b), "_identb"], w=[PS(bT)])
            S.add("act", lambda e: e.copy(out=pTb[b], in_=pv), r=[PS(bT)], w=[("xpT", b)])

        def pc(t):
            b = t % 2
            ops_ = psb[bO][:, 0:128]

            def pvmm(e):
                last = None
                for mt in range(2):
                    last = e.matmul(ops_, V[:, mt, h * 128:(h + 1) * 128], pTb[b][:, mt, :], start=(mt == 0), stop=(mt == 1))
                return last
            S.add("pe", pvmm, r=[("xpT", b), ("V", 0), ("V", 1)], w=[PS(bO)])
            S.add("dve", lambda e: e.tensor_copy(out=cst[:, t * 128:(t + 1) * 128], in_=ops_), r=[PS(bO)], w=[cst_res])

        for s_ in range(NT + 2):
            if s_ < NT:
                pa(s_)
            if 0 <= s_ - 1 < NT:
                pb(s_ - 1)
            if 0 <= s_ - 2 < NT:
                pc(s_ - 2)
        A.reset(m0)

    def stage_mixer0():
        m0 = A.mark()
        hT = A.alloc([16, S_LEN], BF)
        kT = A.alloc([4, MEM], BF)
        V = A.alloc([2, 512], BF)
        stage_norm_hT(x_d, 0, hT)
        if dump == "hT":
            rr = [(("hT", t), h) for t in range(NT) for h in range(2)]
            dump_buf(hT[:, 0, :], 2048, 0, rr)
            dump_buf(hT[:, 15, :], 2048, 2048, rr)
            S.barrier()
            return
        mem_kv(0, kT, V)
        if dump in ("hT2", "hT2a", "hT2b"):
            rr = []
            dump_buf(hT[:, 0, :], 2048, 0, rr)
            dump_buf(hT[:, 15, :], 2048, 2048, rr)
            S.barrier()
            return
        L = S_LEN + 16
        Us = [A.alloc([L], F32) for _ in range(2)]
        B1 = A.alloc([L], F32)
        B2 = A.alloc([L], F32)
        pooled = A.alloc([3, S_LEN], BF)
        wbufs = [A.alloc([16, 128], BF) for _ in range(3)]
        wgrp = A.alloc([4, 3 * 384], BF)
        pedge = A.alloc([4, 16], F32)
        tmp8 = A.alloc([16], F32)
        cst = [A.alloc([S_LEN], BF) for _ in range(2)]
        qx = [A.alloc([S_LEN], BF) for _ in range(2)]
        dma("sp", pedge, cpedge_d.rearrange("p (a b) -> p a b", a=4), [], ["_pedge"], "c4")
        for g in range(4):
            dma("pool", wgrp[:, g, :], wgrp_d[g], [], ["_wgrp"], "wgrp")
        for U in Us:
            S.add("dve", lambda e, U=U: e.memset(U[:, 0:8], 0.0), w=["Upad"])
            S.add("dve", lambda e, U=U: e.memset(U[:, 8 + S_LEN:L], 0.0), w=["Upad"])
        S.add("dve", lambda e: e.memset(B1[:, L - 8:L], 0.0), w=["Bpad"])
        S.add("dve", lambda e: e.memset(B2[:, L - 8:L], 0.0), w=["Bpad"])
        cat_n = [0]

        def store_cat(chunk, buf_i):
            dma("sp", cat_d[chunk], cst[buf_i], [("cst", buf_i)], [("catd", chunk)], ("cstst", buf_i))

        def group_linear(g):
            for oc in range(3):
                bi = cat_n[0] % 2
                cat_n[0] += 1
                for ts in range(4):
                    bank = 4 + (ts % 2)

                    def mm(e, oc=oc, ts=ts, bank=bank, g=g):
                        last = None
                        for c3 in range(3):
                            last = e.matmul(psb[bank][:, :], wgrp[:, g, c3 * 384 + oc * 128:c3 * 384 + (oc + 1) * 128],
                                            pooled[:, c3, ts * 512:(ts + 1) * 512], start=(c3 == 0), stop=(c3 == 2))
                        return last
                    S.add("pe", mm, r=["_wgrp"] + [("pooled", c3) for c3 in range(3)], w=[PS(bank)])
                    col = g * 3 + oc
                    S.add("act", lambda e, bi=bi, ts=ts, bank=bank, col=col: e.mul(
                        out=cst[bi][:, ts * 512:(ts + 1) * 512], in_=psb[bank][:, :], mul=pscale[:, col:col + 1]),
                        r=[PS(bank), "_pscale"], w=[("cst", bi)])
                store_cat(g * 3 + oc, bi)

        for ec in range(16):
            g = ec // 3
            if ec < 12:
                ui = ec % 2
                U = Us[ui]

                def evac(ts, bank, U=U, ui=ui):
                    S.add("act", lambda e, ts=ts, bank=bank: e.copy(out=U[:, 8 + ts * 512:8 + (ts + 1) * 512], in_=psb[bank][:, :]),
                          r=[PS(bank)], w=[("U", ui, ts)])
                proj_chunk(win0_d[ec], hT, wbufs, ec, evac, (0, 1, 2, 3))
                if ec % 3 == 0 and ec > 0:
                    group_linear(ec // 3 - 1)
                k = g + 1
                half = 1 << g
                w_ = 2 * half
                src = U
                src_r = [("U", ui, ts) for ts in range(4)] + ["Upad"]
                cur = None
                for step in range(k):
                    sh = 1 << step
                    dst = B1 if (step % 2 == 0) else B2
                    dres = "B1" if (step % 2 == 0) else "B2"
                    n = L - 8 if step > 0 else L - 1
                    n = L - sh - (0 if step == 0 else 0)
                    nn = L - sh
                    eng = "dve"
                    S.add(eng, lambda e, dst=dst, src=src, sh=sh, nn=nn: e.tensor_tensor(out=dst[:, 0:nn], in0=src[:, 0:nn], in1=src[:, sh:sh + nn], op=ALU.add),
                          r=src_r + ["Bpad"], w=[dres])
                    src = dst
                    src_r = [dres]
                off = 8 - half
                cc = ec % 3
                S.add("dve", lambda e, src=src, off=off, w_=w_, cc=cc, U=U: e.scalar_tensor_tensor(
                    out=pooled[:, cc, :], in0=src[:, off:off + S_LEN], scalar=1.0 / w_, in1=U[:, 8:8 + S_LEN],
                    op0=ALU.mult, op1=ALU.subtract), r=src_r + [("U", ui, ts) for ts in range(4)], w=[("pooled", cc)])
                for (c0, pe0) in ((0, 0), (S_LEN - 8, 8)):
                    S.add("dve", lambda e, src=src, off=off, c0=c0, pe0=pe0, g=g: e.tensor_tensor(
                        out=tmp8[:, 0:8], in0=src[:, off + c0:off + c0 + 8], in1=pedge[:, g, pe0:pe0 + 8], op=ALU.mult),
                        r=src_r + ["_pedge"], w=["tmp8"])
                    S.add("dve", lambda e, c0=c0, cc=cc, U=U: e.tensor_tensor(
                        out=pooled[:, cc, c0:c0 + 8], in0=tmp8[:, 0:8], in1=U[:, 8 + c0:16 + c0], op=ALU.subtract),
                        r=["tmp8"] + [("U", ui, ts) for ts in range(4)], w=[("pooled", cc)])
            else:
                h = ec - 12
                qb = qx[h % 2]

                def evac(ts, bank, qb=qb, h=h):
                    S.add("act", lambda e, ts=ts, bank=bank: e.copy(out=qb[:, ts * 512:(ts + 1) * 512], in_=psb[bank][:, :]),
                          r=[PS(bank)], w=[("qx", h % 2)])
                proj_chunk(win0_d[ec], hT, wbufs, ec, evac, (0, 1, 2, 3))
                if ec == 12:
                    group_linear(3)
                bi = cat_n[0] % 2
                cat_n[0] += 1
                xattn_head(h, qb, ("qx", h % 2), kT, V, cst[bi], ("cst", bi), (4, 5, 6, 7))
                store_cat(12 + h, bi)
        S.barrier()
        A.reset(m0)

    def stage_outproj(layer, src_d):
        m0 = A.mark()
        catT = A.alloc([16, S_LEN], BF)
        wo = A.alloc([16, D], BF)
        xts = [A.alloc([D], F32) for _ in range(2)]
        for ec in range(16):
            dma("sp", catT[:, ec, :], cat_d[ec], [("catd", ec)], [("catT", ec)], ("catl", ec % 4))
            dma("pool", wo[:, ec, :], wout_d[layer, ec], [], [("wo", ec)], ("wol", ec % 4))
        for t in range(NT):
            b = t % 2
            dma("sp", xts[b], src_d[t * 128:(t + 1) * 128, :], [], [("xo", b)], ("xo", b))
            for ds in range(4):
                bank = 4 * b + ds

                def mm(e, t=t, ds=ds, bank=bank):
                    last = None
                    for ec in range(16):
                        last = e.matmul(psb[bank][:, :], catT[:, ec, t * 128:(t + 1) * 128], wo[:, ec, ds * 512:(ds + 1) * 512],
                                        start=(ec == 0), stop=(ec == 15))
                    return last
                S.add("pe", mm, r=[("catT", ec) for ec in range(16)] + [("wo", ec) for ec in range(16)], w=[PS(bank)])
                S.add("dve", lambda e, b=b, ds=ds, bank=bank: e.tensor_tensor(
                    out=xts[b][:, ds * 512:(ds + 1) * 512], in0=psb[bank][:, :], in1=xts[b][:, ds * 512:(ds + 1) * 512], op=ALU.add),
                    r=[PS(bank), ("xo", b)], w=[("xo", b)])
            dma("sp", xres_d[t * 128:(t + 1) * 128, :], xts[b], [("xo", b)], [("xres", t)], ("xost", b))
        S.barrier()
        A.reset(m0)

    def stage_router(layer, pre=None):
        m0 = A.mark()
        if pre is not None:
            pre()
        NB = 4
        xts = [A.alloc([D], F32) for _ in range(NB)]
        hms = [A.alloc([D], BF) for _ in range(NB)]
        junk = A.alloc([D], BF)
        hTt = [A.alloc([16, 128], BF) for _ in range(2)]
        wr = A.alloc([16, 16], BF)
        affT = A.alloc([S_LEN], F32)
        wkb = A.alloc([S_LEN], F32)
        vals = A.alloc([CAP], F32)
        idxu = A.alloc([CAP], U32)
        idxf = A.alloc([CAP], F32)
        lg = [A.alloc([16], F32) for _ in range(2)]
        load_gain(1 + 2 * layer)
        dma("pool", wr, wr_d[layer].rearrange("p (a b) -> p a b", a=16), [], ["_wr"], "wr")

        def ra(t):
            b = t % NB
            dma("sp", xts[b], xres_d[t * 128:(t + 1) * 128, :], [("xres", t)], [("xt", b)], ("xt", b))
            norm_tile(xts[b], ("xt", b), hms[b], ("hm", b), junk, 2 * b)
            dma("sp", hrow_d[t * 128:(t + 1) * 128, :], hms[b], [("hm", b)], ["hrow"], ("hst", b))

        def rb(t):
            b = t % NB
            c = t % 2
            transpose_tile(hms[b], ("hm", b), hTt[c], ("hTt", c), (2 * c, 2 * c + 1), evac=("act" if c == 0 else "dve"))

        def rc(t):
            c = t % 2
            bank = 4 + c
            mx = stat[:, 16 + c:17 + c]
            sm = stat[:, 18 + c:19 + c]

            def mm(e):
                last = None
                for dc in range(16):
                    last = e.matmul(psb[bank][:, 0:16], hTt[c][:, dc, :], wr[:, dc, :], start=(dc == 0), stop=(dc == 15))
                return last
            S.add("pe", mm, r=[(("hTt", c), 0), (("hTt", c), 1), "_wr"], w=[PS(bank)])
            S.add("dve", lambda e: e.tensor_reduce(out=mx, in_=psb[bank][:, 0:16], axis=AX.X, op=ALU.max),
                  r=[PS(bank)], w=[("rs", c)])
            S.add("dve", lambda e: e.tensor_scalar(out=mx, in0=mx, scalar1=-1.0, scalar2=None, op0=ALU.mult),
                  r=[("rs", c)], w=[("rs", c)])
            S.add("act", lambda e: e.activation(out=lg[c], in_=psb[bank][:, 0:16], func=AF.Exp, bias=mx, scale=1.0, accum_out=sm),
                  r=[PS(bank), ("rs", c)], w=[("lg", c), ("rsm", c)])

        def rd(t):
            c = t % 2
            sm = stat[:, 18 + c:19 + c]
            tb = 6 + c
            S.add("dve", lambda e: e.reciprocal(out=sm, in_=sm), r=[("rsm", c)], w=[("rsm", c)])
            S.add("dve", lambda e: e.tensor_scalar(out=lg[c], in0=lg[c], scalar1=sm, scalar2=None, op0=ALU.mult),
                  r=[("lg", c), ("rsm", c)], w=[("lg", c)])
            S.add("pe", lambda e: e.transpose(out=psb[tb][0:16, 0:128], in_=lg[c], identity=identf),
                  r=[("lg", c), "_identf"], w=[PS(tb)])
            S.add("act", lambda e: e.copy(out=affT[0:16, t * 128:(t + 1) * 128], in_=psb[tb][0:16, 0:128]),
                  r=[PS(tb)], w=["affT"])

        for s_ in range(NT + 4):
            if s_ < NT:
                ra(s_)
            if 0 <= s_ - 2 < NT:
                rb(s_ - 2)
            if 0 <= s_ - 3 < NT:
                rc(s_ - 3)
            if 0 <= s_ - 4 < NT:
                rd(s_ - 4)
        cur = affT
        for it in range(CAP // 8):
            v8 = vals[0:16, it * 8:it * 8 + 8]
            i8 = idxu[0:16, it * 8:it * 8 + 8]
            S.add("dve", lambda e, v8=v8, cur=cur: e.max(out=v8, in_=cur[0:16, :]), r=["affT", "wkb"], w=[("v8", it)])
            S.add("dve", lambda e, v8=v8, i8=i8, cur=cur: e.max_index(out=i8, in_max=v8, in_values=cur[0:16, :]),
                  r=["affT", "wkb", ("v8", it)], w=[("i8", it)])
            if it < CAP // 8 - 1:
                S.add("dve", lambda e, v8=v8, cur=cur: e.match_replace(out=wkb[0:16, :], in_to_replace=v8, in_values=cur[0:16, :], imm_value=-1.0),
                      r=["affT", ("v8", it)], w=["wkb"])
                cur = wkb
        S.add("dve", lambda e: e.tensor_copy(out=idxf[0:16, :], in_=idxu[0:16, :]), r=[("i8", it) for it in range(CAP // 8)], w=["idxf"])
        for ct in range(2):
            S.add("pe", lambda e, ct=ct: e.transpose(out=psb[0][:, ct * 16:(ct + 1) * 16], in_=idxf[0:16, ct * 128:(ct + 1) * 128], identity=identf[0:16, 0:16]),
                  r=["idxf", "_identf"], w=[(PS(0), ct)])
            S.add("pe", lambda e, ct=ct: e.transpose(out=psb[1][:, ct * 16:(ct + 1) * 16], in_=vals[0:16, ct * 128:(ct + 1) * 128], identity=identf[0:16, 0:16]),
                  r=[("v8", it) for it in range(CAP // 8)] + ["_identf"], w=[(PS(1), ct)])
        S.add("dve", lambda e: e.tensor_copy(out=idxT.rearrange("p a b -> p (a b)"), in_=psb[0][:, 0:32]), r=[(PS(0), 0), (PS(0), 1)], w=["idxT"])
        S.add("dve", lambda e: e.tensor_copy(out=gT.rearrange("p a b -> p (a b)"), in_=psb[1][:, 0:32]), r=[(PS(1), 0), (PS(1), 1)], w=["gT"])
        if dump == "rt":
            dump_buf(idxT.rearrange("p a b -> p (a b)"), 32, 0, ["idxT"])
            dump_buf(gT.rearrange("p a b -> p (a b)"), 32, 32, ["gT"])
        S.barrier()
        A.reset(m0)

    def stage_moe(layer):
        m0 = A.mark()
        NBIG, NSM = 4, 5
        big = [A.alloc([16, 512], BF) for _ in range(NBIG)]
        sml = [A.alloc([8, 512], BF) for _ in range(NSM)]
        nb = [0]
        ns = [0]
        slots_of = {}

        def load_big(e_):
            slots = slots_of.setdefault(e_, {})
            for fs in range(2):
                for nm, src in (("g", wg_d), ("u", wu_d)):
                    bi = nb[0] % NBIG
                    nb[0] += 1
                    dma("pool", big[bi], src[layer, e_, fs].rearrange("p (a b) -> p a b", a=16), [], [("big", bi)], ("big", bi))
                    slots[(nm, fs)] = bi

        def load_sml(e_):
            slots = slots_of.setdefault(e_, {})
            for ds in range(4):
                si = ns[0] % NSM
                ns[0] += 1
                dma("pool", sml[si], wd_d[layer, e_, ds].rearrange("p (a b) -> p a b", a=8), [], [("sml", si)], ("sml", si))
                slots[("d", ds)] = si

        stage_router(layer, pre=lambda: (load_big(0), load_sml(0)))
        xg = [[A.alloc([D], BF) for _ in range(2)] for _ in range(2)]
        xgT = [A.alloc([16, CAP], BF) for _ in range(2)]
        act_tm = A.alloc([2, DFF], BF)
        actT = A.alloc([8, CAP], BF)
        sa = [A.alloc([512], F32) for _ in range(2)]
        yo = [A.alloc([D], F32) for _ in range(2)]

        def gather(e_):
            pb = e_ % 2
            for ct in range(2):
                S.add("pool", lambda e, pb=pb, ct=ct, e_=e_: e.indirect_dma_start(
                    out=xg[pb][ct], out_offset=None, in_=hrow_d,
                    in_offset=bass.IndirectOffsetOnAxis(ap=idxT[:, ct, e_:e_ + 1], axis=0)),
                    r=["hrow", "idxT"], w=[("xg", pb, ct)], dma=("xg", pb, ct))

        gather(0)
        for e_ in range(NE):
            pb = e_ % 2
            slots = slots_of[e_]
            if e_ + 1 < NE:
                gather(e_ + 1)
            for ct in range(2):
                transpose_tile(xg[pb][ct], ("xg", pb, ct), xgT[pb][:, :, ct * 128:(ct + 1) * 128], ("xgT", pb, ct),
                               (6, 7), evac=("act" if ct == 0 else "dve"))
            xg_r = [(("xgT", pb, ct), h) for ct in range(2) for h in range(2)]
            for fs in range(2):
                bg = slots[("g", fs)]
                bu = slots[("u", fs)]
                for ct in range(2):
                    pa = ct
                    pu = 2 + ct

                    def mm(e, bw, bank, ct=ct, pb=pb):
                        last = None
                        for dc in range(16):
                            last = e.matmul(psb[bank][:, :], xgT[pb][:, dc, ct * 128:(ct + 1) * 128], big[bw][:, dc, :],
                                            start=(dc == 0), stop=(dc == 15))
                        return last
                    S.add("pe", lambda e, bg=bg, pa=pa, mm=mm: mm(e, bg, pa), r=xg_r + [("big", bg)], w=[PS(pa)])
                    S.add("pe", lambda e, bu=bu, pu=pu, mm=mm: mm(e, bu, pu), r=xg_r + [("big", bu)], w=[PS(pu)])
                    S.add("act", lambda e, ct=ct, pa=pa: e.activation(out=sa[ct], in_=psb[pa][:, :], func=AF.Silu),
                          r=[PS(pa)], w=[("sa", ct)])
                    S.add("dve", lambda e, ct=ct, pu=pu, fs=fs: e.tensor_tensor(out=act_tm[:, ct, fs * 512:(fs + 1) * 512], in0=sa[ct], in1=psb[pu][:, :], op=ALU.mult),
                          r=[("sa", ct), PS(pu)], w=[("act_tm", ct, fs)])
            if e_ + 1 < NE:
                load_big(e_ + 1)
            for ct in range(2):
                pv = psbf(6 + ct)[:, :].rearrange("p (a b) -> p a b", a=8)

                def tr(e, ct=ct, pv=pv):
                    last = None
                    for fc in range(8):
                        last = e.transpose(out=pv[:, fc, :], in_=act_tm[:, ct, fc * 128:(fc + 1) * 128], identity=identb)
                    return last
                S.add("pe", tr, r=[("act_tm", ct, 0), ("act_tm", ct, 1), "_identb"], w=[PS(6 + ct)])
                if ct == 0:
                    S.add("act", lambda e, ct=ct, pv=pv: e.copy(out=actT[:, :, ct * 128:(ct + 1) * 128], in_=pv), r=[PS(6 + ct)], w=[("actT", ct)])
                else:
                    S.add("dve", lambda e, ct=ct, pv=pv: e.tensor_copy(out=actT[:, :, ct * 128:(ct + 1) * 128], in_=pv), r=[PS(6 + ct)], w=[("actT", ct)])
            k = 0
            for ds in range(4):
                si = slots[("d", ds)]
                for ct in range(2):
                    bank = 4 + (k % 2)
                    k += 1

                    def mmd(e, ct=ct, si=si, bank=bank):
                        last = None
                        for fc in range(8):
                            last = e.matmul(psb[bank][:, :], actT[:, fc, ct * 128:(ct + 1) * 128], sml[si][:, fc, :],
                                            start=(fc == 0), stop=(fc == 7))
                        return last
                    S.add("pe", mmd, r=[("actT", 0), ("actT", 1), ("sml", si)], w=[PS(bank)])
                    if ct == 0:
                        S.add("act", lambda e, ct=ct, ds=ds, bank=bank, e_=e_: e.mul(
                            out=yo[ct][:, ds * 512:(ds + 1) * 512], in_=psb[bank][:, :], mul=gT[:, ct, e_:e_ + 1]),
                            r=[PS(bank), "gT"], w=[("yo", ct)])
                    else:
                        S.add("dve", lambda e, ct=ct, ds=ds, bank=bank, e_=e_: e.tensor_scalar(
                            out=yo[ct][:, ds * 512:(ds + 1) * 512], in0=psb[bank][:, :], scalar1=gT[:, ct, e_:e_ + 1], scalar2=None, op0=ALU.mult),
                            r=[PS(bank), "gT"], w=[("yo", ct)])
            if e_ + 1 < NE:
                load_sml(e_ + 1)
            for ct in range(2):
                S.add("pool", lambda e, ct=ct, e_=e_: e.indirect_dma_start(
                    out=xres_d, out_offset=bass.IndirectOffsetOnAxis(ap=idxT[:, ct, e_:e_ + 1], axis=0),
                    in_=yo[ct], in_offset=None, compute_op=ALU.add),
                    r=[("yo", ct), "idxT"], w=["xres_all"], dma=("sc", ct))
        S.barrier()
        A.reset(m0)

    def stage_mixer1():
        m0 = A.mark()
        hT = A.alloc([16, S_LEN], BF)
        kT = A.alloc([4, MEM], BF)
        V = A.alloc([2, 512], BF)
        Ct = A.alloc([S_LEN], F32)
        St = A.alloc([S_LEN], F32)
        invf = A.alloc([2], F32)
        rotT = A.alloc([128], F32)
        stage_norm_hT(xres_d, 2, hT)
        m1 = A.mark()
        posi = A.alloc([S_LEN], I32)
        posf = A.alloc([S_LEN], F32)
        dma("sp", posi, pos_d.partition_broadcast(128), [], ["posi"], "posi")
        dma("sp", invf, cinvf_d, [], ["_invf"], "c5")
        dma("sp", rotT, crot_d, [], ["_rotT"], "c6")
        S.add("dve", lambda e: e.tensor_copy(out=posf, in_=posi), r=["posi"], w=["posf"])
        ki = A.alloc([S_LEN], I32)
        mk = A.alloc([S_LEN], F32)
        SC = 6.283179

        def reduce_(T, res):
            S.add("dve", lambda e: e.tensor_copy(out=ki, in_=T), r=[res], w=["ki"])
            S.add("dve", lambda e: e.tensor_copy(out=mk, in_=ki), r=["ki"], w=["mk"])
            S.add("dve", lambda e: e.tensor_tensor(out=T, in0=T, in1=mk, op=ALU.subtract), r=[res, "mk"], w=[res])
            S.add("dve", lambda e: e.tensor_scalar(out=mk, in0=T, scalar1=0.5, scalar2=None, op0=ALU.is_gt), r=[res], w=["mk"])
            S.add("dve", lambda e: e.tensor_tensor(out=T, in0=T, in1=mk, op=ALU.subtract), r=[res, "mk"], w=[res])
            S.add("dve", lambda e: e.tensor_scalar(out=mk, in0=T, scalar1=-0.5, scalar2=None, op0=ALU.is_lt), r=[res], w=["mk"])
            S.add("dve", lambda e: e.tensor_tensor(out=T, in0=T, in1=mk, op=ALU.add), r=[res, "mk"], w=[res])
            S.add("act", lambda e: e.activation(out=T, in_=T, func=AF.Sin, scale=SC), r=[res], w=[res])
        S.add("dve", lambda e: e.tensor_scalar(out=St, in0=posf, scalar1=invf[:, 0:1], scalar2=None, op0=ALU.mult),
              r=["posf", "_invf"], w=["St"])
        S.add("dve", lambda e: e.tensor_scalar(out=Ct, in0=posf, scalar1=invf[:, 0:1], scalar2=0.25, op0=ALU.mult, op1=ALU.add),
              r=["posf", "_invf"], w=["Ct"])
        reduce_(St, "St")
        reduce_(Ct, "Ct")
        S.add("dve", lambda e: e.tensor_scalar(out=St, in0=St, scalar1=invf[:, 1:2], scalar2=None, op0=ALU.mult), r=["St", "_invf"], w=["St"])
        S.add("dve", lambda e: e.tensor_scalar(out=Ct, in0=Ct, scalar1=-1.0, scalar2=invf[:, 1:2], op0=ALU.add, op1=ALU.mult), r=["Ct", "_invf"], w=["Ct"])
        S.add("dve", lambda e: e.tensor_scalar(out=Ct, in0=Ct, scalar1=1.0, scalar2=None, op0=ALU.add), r=["Ct"], w=["Ct"])
        S.barrier()
        A.reset(m1)
        mem_kv(1, kT, V)
        wbufs = [A.alloc([16, 128], BF) for _ in range(3)]
        wv = A.alloc([16, 192], BF)
        v_tm = A.alloc([NT, 192], BF)
        kT2 = A.alloc([3, S_LEN], BF)
        qf = A.alloc([S_LEN], F32)
        t1 = A.alloc([512], F32)
        qTb = [A.alloc([S_LEN], BF) for _ in range(2)]
        mask = A.alloc([2, 384], BF)
        pb_ = [A.alloc([2, 384], BF) for _ in range(2)]
        pm_ = [A.alloc([2, 384], BF) for _ in range(2)]
        pTs = [A.alloc([6, 128], BF) for _ in range(2)]
        otm = [A.alloc([128], BF) for _ in range(2)]
        cst = [A.alloc([S_LEN], BF) for _ in range(2)]
        qx = [A.alloc([S_LEN], BF) for _ in range(2)]
        dma("sp", mask, cmask_d.rearrange("p (a b) -> p a b", a=2), [], ["_mask"], "c7")
        dma("pool", wv, wv1_d.rearrange("p (a b) -> p a b", a=16), [], ["_wv"], "wv")
        hT_all = [(("hT", t), h) for t in range(NT) for h in range(2)]
        for t in range(NT):
            bank = t % 2

            def mm(e, t=t, bank=bank):
                last = None
                for dc in range(16):
                    last = e.matmul(psb[bank][:, 0:192], hT[:, dc, t * 128:(t + 1) * 128], wv[:, dc, :], start=(dc == 0), stop=(dc == 15))
                return last
            S.add("pe", mm, r=["_wv", (("hT", t), 0), (("hT", t), 1)], w=[PS(bank)])
            S.add("act", lambda e, t=t, bank=bank: e.copy(out=v_tm[:, t, :], in_=psb[bank][:, 0:192]), r=[PS(bank)], w=[("v_tm", t)])

        def rotary_sub(dst, dst_res, ts, hh):
            c0 = ts * 512 + hh * 256
            S.add("pe", lambda e: e.matmul(psb[2][:, 0:256], rotT, qf[:, c0:c0 + 256], start=True, stop=True),
                  r=[("qf", ts), "_rotT"], w=[PS(2)])
            S.add("dve", lambda e: e.tensor_tensor(out=t1[:, 0:256], in0=psb[2][:, 0:256], in1=St[:, c0:c0 + 256], op=ALU.mult),
                  r=[PS(2), "St"], w=["t1a"])
            S.add("pool", lambda e: e.tensor_tensor(out=t1[:, 256:512], in0=qf[:, c0:c0 + 256], in1=Ct[:, c0:c0 + 256], op=ALU.mult),
                  r=[("qf", ts), "Ct"], w=["t1b"])
            S.add("dve", lambda e: e.tensor_tensor(out=dst[:, c0:c0 + 256], in0=t1[:, 0:256], in1=t1[:, 256:512], op=ALU.add),
                  r=["t1a", "t1b"], w=[dst_res])

        def rotary_chunk(dst, dst_res):
            for ts in range(4):
                for hh in range(2):
                    rotary_sub(dst, dst_res, ts, hh)

        nxt = {}

        def next_chunk_item(j1, k):
            if k == 0:
                b = ci[0] % len(wbufs)
                ci[0] += 1
                nxt["wb"] = (wbufs[b], b)
                dma("pool", wbufs[b], win1_d[3 + j1].rearrange("p (a b) -> p a b", a=16), [], [("wb", b)], ("wb", b))
            elif 2 <= k <= 5:
                ts = k - 2
                wb, b = nxt["wb"]

                def mm(e):
                    last = None
                    for dc in range(16):
                        last = e.matmul(psb[2][:, :], wb[:, dc, :], hT[:, dc, ts * 512:(ts + 1) * 512],
                                        start=(dc == 0), stop=(dc == 15))
                    return last
                S.add("pe", mm, r=[("wb", b)], w=[PS(2)])
                evac_qf(ts, 2)
            elif 6 <= k <= 13:
                rotary_sub(qTb[j1 % 2], ("qTb", j1 % 2), (k - 6) // 2, (k - 6) % 2)

        def evac_qf(ts, bank):
            S.add("act", lambda e, ts=ts, bank=bank: e.copy(out=qf[:, ts * 512:(ts + 1) * 512], in_=psb[bank][:, :]),
                  r=[PS(bank)], w=[("qf", ts)])

        ci = [0]
        for g in range(3):
            proj_chunk(win1_d[g], hT, wbufs, ci[0], evac_qf, (0, 1))
            ci[0] += 1
            rotary_chunk(kT2[:, g, :], ("kT2", g))
        cat_n = [0]

        def store_cat(chunk, buf_i):
            dma("sp", cat_d[chunk], cst[buf_i], [("cst", buf_i)], [("catd", chunk)], ("cstst", buf_i))

        NQ = 12 * NT

        def geom(q):
            j, n = q // NT, q % NT
            kb0 = max(n - 1, 0)
            kb1 = min(n + 1, NT - 1)
            nk = (kb1 - kb0 + 1) * 128
            moff = 0 if n > 0 else 128
            return j, n, j // 4, kb0, nk, moff

        def st_cols(q):
            i4 = q % 4
            base = 24 + 6 * i4
            return stat[:, base:base + 2], stat[:, base + 2:base + 4], stat[:, base + 4:base + 6]

        def ph0(q):
            j, n, g, kb0, nk, moff = geom(q)
            ib = q % 2
            i4 = q % 4
            qb = qTb[j % 2]
            qres = ("qTb", j % 2)
            sb0 = 3 if ib == 0 else 0
            mx, sm, es_ = st_cols(q)
            for hh in range(2):
                def qk(e, hh=hh):
                    e.matmul(psb[sb0 + hh][:, 0:nk], qb[hh * 64:(hh + 1) * 64, n * 128:(n + 1) * 128],
                             kT2[hh * 64:(hh + 1) * 64, g, kb0 * 128:kb0 * 128 + nk], start=True, stop=False)
                    return e.matmul(psb[sb0 + hh][:, 0:nk], identb, mask[:, 0, moff:moff + nk], start=False, stop=True)
                S.add("pe", qk, r=[qres, ("kT2", g), "_mask", "_identb"], w=[PS(sb0 + hh)])
            for hh in range(2):
                S.add("dve", lambda e, hh=hh: e.tensor_reduce(out=mx[:, hh:hh + 1], in_=psb[sb0 + hh][:, 0:nk], axis=AX.X, op=ALU.max),
                      r=[PS(sb0 + hh)], w=[("amx", i4, hh)])
            S.add("dve", lambda e: e.scalar_tensor_tensor(out=mx, in0=mx, scalar=-0.125, in1=nsink[:, 2 * j:2 * j + 2], op0=ALU.mult, op1=ALU.min),
                  r=[("amx", i4, 0), ("amx", i4, 1), "_nsink"], w=[("anm", i4)])
            S.add("dve", lambda e: e.tensor_tensor(out=es_, in0=mx, in1=nsink[:, 2 * j:2 * j + 2], op=ALU.subtract),
                  r=[("anm", i4), "_nsink"], w=[("aes", i4)])
            for hh in range(2):
                S.add("act", lambda e, hh=hh: e.activation(
                    out=pm_[ib][:, hh, 0:nk], in_=psb[sb0 + hh][:, 0:nk], func=AF.Exp, bias=mx[:, hh:hh + 1], scale=0.125,
                    accum_out=sm[:, hh:hh + 1]),
                    r=[PS(sb0 + hh), ("anm", i4)], w=[("apm", ib, hh), ("asm", i4, hh)])
            S.add("act", lambda e: e.activation(out=es_, in_=es_, func=AF.Exp), r=[("aes", i4)], w=[("aes", i4)])

        def ph1(q):
            j, n, g, kb0, nk, moff = geom(q)
            ib = q % 2
            i4 = q % 4
            nkb = nk // 128
            mx, sm, es_ = st_cols(q)
            S.add("dve", lambda e: e.tensor_tensor(out=sm, in0=sm, in1=es_, op=ALU.add),
                  r=[("aes", i4), ("asm", i4, 0), ("asm", i4, 1)], w=[("ard", i4)])
            S.add("dve", lambda e: e.reciprocal(out=sm, in_=sm), r=[("ard", i4)], w=[("ard", i4)])
            pv = psbf(7)[:, 0:768].rearrange("p (a b) -> p a b", a=6)

            def tr(e):
                last = None
                for hh in range(2):
                    for kb in range(nkb):
                        last = e.transpose(out=pv[:, hh * 3 + kb, :], in_=pm_[ib][:, hh, kb * 128:(kb + 1) * 128], identity=identb)
                return last
            S.add("pe", tr, r=[("apm", ib, 0), ("apm", ib, 1), "_identb"], w=[(PS(7), 0)])
            if nkb == 3:
                S.add("act", lambda e: e.copy(out=pTs[ib], in_=pv), r=[(PS(7), 0)], w=[("apT", ib)])
            else:
                for hh in range(2):
                    S.add("act", lambda e, hh=hh: e.copy(out=pTs[ib][:, hh * 3:hh * 3 + nkb, :], in_=pv[:, hh * 3:hh * 3 + nkb, :]),
                          r=[(PS(7), 0)], w=[("apT", ib)])

        def ph2(q):
            j, n, g, kb0, nk, moff = geom(q)
            ib = q % 2
            i4 = q % 4
            nkb = nk // 128
            mx, sm, es_ = st_cols(q)
            po = psb[5][:, 0:128]

            def pvm(e):
                last = None
                for hh in range(2):
                    for kb in range(nkb):
                        last = e.matmul(po[:, hh * 64:(hh + 1) * 64], pTs[ib][:, hh * 3 + kb, :], v_tm[:, kb0 + kb, g * 64:(g + 1) * 64],
                                        start=(kb == 0), stop=(kb == nkb - 1))
                return last
            S.add("pe", pvm, r=[("apT", ib)] + [("v_tm", kb0 + kb) for kb in range(nkb)], w=[PS(5)])
            for hh in range(2):
                S.add("act", lambda e, hh=hh: e.mul(
                    out=otm[ib][:, hh * 64:(hh + 1) * 64], in_=po[:, hh * 64:(hh + 1) * 64], mul=sm[:, hh:hh + 1]),
                    r=[PS(5), ("ard", i4)], w=[("otm", ib)])

        def ph3(q):
            j, n, g, kb0, nk, moff = geom(q)
            ib = q % 2
            bi = j % 2
            ft = psbf(6)[:, 0:128]
            S.add("pe", lambda e: e.transpose(out=ft, in_=otm[ib], identity=identb), r=[("otm", ib), "_identb"], w=[PS(6)])
            S.add("dve", lambda e: e.tensor_copy(out=cst[bi][:, n * 128:(n + 1) * 128], in_=ft), r=[PS(6)], w=[("cst", bi)])
            if n == NT - 1:
                store_cat(j, bi)

        proj_chunk(win1_d[3], hT, wbufs, ci[0], evac_qf, (0, 1))
        ci[0] += 1
        rotary_chunk(qTb[0], ("qTb", 0))
        for s_ in range(NQ + 3):
            if s_ < NQ:
                ph0(s_)
                j1 = s_ // NT + 1
                if j1 < 12:
                    next_chunk_item(j1, s_ % NT)
            if 0 <= s_ - 1 < NQ:
                ph1(s_ - 1)
            if 0 <= s_ - 2 < NQ:
                ph2(s_ - 2)
            if 0 <= s_ - 3 < NQ:
                ph3(s_ - 3)
        cat_n[0] = 12
        for h in range(4):
            qb = qx[h % 2]

            def evac(ts, bank, qb=qb, h=h):
                S.add("act", lambda e, ts=ts, bank=bank: e.copy(out=qb[:, ts * 512:(ts + 1) * 512], in_=psb[bank][:, :]),
                      r=[PS(bank)], w=[("qx", h % 2)])
            proj_chunk(win1_d[15 + h], hT, wbufs, ci[0], evac, (0, 1))
            ci[0] += 1
            bi = cat_n[0] % 2
            cat_n[0] += 1
            xattn_head(h, qb, ("qx", h % 2), kT, V, cst[bi], ("cst", bi), (3, 4, 7, 5))
            store_cat(12 + h, bi)
        S.barrier()
        A.reset(m0)

    def stage_final():
        m0 = A.mark()
        xts = [A.alloc([D], F32) for _ in range(4)]
        outs = [A.alloc([D], F32) for _ in range(4)]
        junk = A.alloc([D], BF)
        load_gain(5)
        for t in range(NT):
            b = t % 4
            dma("sp", xts[b], xres_d[t * 128:(t + 1) * 128, :], [], [("xt", b)], ("xt", b))
            norm_tile(xts[b], ("xt", b), outs[b], ("of", b), junk, 2 * b)
            dma("sp", y_d[t * 128:(t + 1) * 128, :], outs[b], [("of", b)], ["y"], ("yst", b))
        A.reset(m0)

    stages = [
        ("mem", stage_mem),
        ("mix0", stage_mixer0),
        ("out0", lambda: stage_outproj(0, x_d)),
        ("ex0", lambda: stage_moe(0)),
        ("mix1", stage_mixer1),
        ("out1", lambda: stage_outproj(1, xres_d)),
        ("ex1", lambda: stage_moe(1)),
        ("final", stage_final),
    ]
    for name, fn in stages:
        if only is not None and name not in only:
            continue
        fn()
        if stop_after == name:
            break
    if stop_after is not None and stop_after != "final":
        S.barrier()
    finals = [k for k in S.dma_cnt if isinstance(k, tuple) and k[0] in ("yst", "xost", "cstst", "sc")]
    S.emit(es, final_dma_keys=finals)
    es.close()
    return nc


def _consts():
    identb = np.eye(128, dtype=np.float32).astype(ml_dtypes.bfloat16)
    identf = np.eye(128, dtype=np.float32)
    rot = np.zeros((128, 128), np.float32)
    for b in (0, 64):
        for r in range(8):
            rot[b + r + 8, b + r] = -1.0
            rot[b + r, b + r + 8] = 1.0
    inv_freq = (500000.0 ** (-np.arange(0, 16, 2, dtype=np.float32) / 16)).astype(np.float32)
    invf = np.zeros((128, 2), np.float32)
    for p in range(128):
        r = p % 64
        if r < 16:
            invf[p, 0] = np.float32(inv_freq[r % 8] / (2.0 * np.pi))
            invf[p, 1] = 1.0
    qq = np.arange(128)[:, None]
    kk = np.arange(128)[None, :]
    m = np.concatenate([(kk >= qq), np.ones((128, 128), bool), (kk <= qq)], axis=1)
    m = np.where(m, 0.0, -30000.0).astype(np.float32)
    mask = np.concatenate([m, m], axis=1).astype(ml_dtypes.bfloat16)
    pedge = np.zeros((4, 16), np.float32)
    t = np.arange(S_LEN)
    for g, w in enumerate((2, 4, 8, 16)):
        lo = np.maximum(t - w // 2, 0)
        hi = np.minimum(t + w // 2 - 1, S_LEN - 1)
        cnt = (hi - lo + 1).astype(np.float32)
        pedge[g, 0:8] = 1.0 / cnt[0:8]
        pedge[g, 8:16] = 1.0 / cnt[S_LEN - 8:]
    pedge = np.broadcast_to(pedge.reshape(1, 64), (128, 64)).copy()
    return identb, identf, rot, invf, mask, pedge


def _pieces(w, pc):
    R, C = w.shape
    n = R // 128
    return np.ascontiguousarray(w.reshape(n, 128, C // pc, pc).transpose(2, 1, 0, 3)).reshape(C // pc, 128, n * pc)


def _host_layout(inp, slim=False):
    f = lambda a: np.ascontiguousarray(np.asarray(a, dtype=np.float32))
    shared = {}
    shared["gains"] = np.stack([f(inp["norm_mix_g"])[0], f(inp["norm_ffn_g"])[0], f(inp["norm_mix_g"])[1],
                                f(inp["norm_ffn_g"])[1], f(inp["mem_norm_g"]), f(inp["final_g"])], axis=0)
    wkv = f(inp["mem_w_kv"])
    shared["wkv"] = np.ascontiguousarray(wkv.reshape(2, 4, 4, 128, 1024).transpose(0, 1, 3, 2, 4)).reshape(2, 4, 128, 4096)
    shared["win0"] = _pieces(f(inp["pool_w_in"])[0], 128)
    gw = f(inp["pool_group_w"])[0]
    shared["wgrp"] = np.ascontiguousarray(gw.reshape(4, 3, 128, 384).transpose(0, 2, 1, 3)).reshape(4, 128, 1152)
    shared["pscale"] = np.ascontiguousarray(f(inp["pool_scale"])[0].reshape(12, 128).T)
    shared["wout"] = np.stack([f(inp["pool_w_out"])[0].reshape(16, 128, 2048), f(inp["attn_w_out"])[0].reshape(16, 128, 2048)], axis=0)
    w1 = f(inp["attn_w_in"])[0]
    kcols = []
    for g in range(3):
        kc = w1[:, 1536 + g * 64:1536 + (g + 1) * 64]
        kcols.append(np.concatenate([kc, kc], axis=1))
    w1c = np.concatenate(kcols + [w1[:, 0:1536], w1[:, 1920:2432]], axis=1)
    shared["win1"] = _pieces(w1c, 128)
    wv = w1[:, 1728:1920]
    shared["wv1"] = np.ascontiguousarray(wv.reshape(16, 128, 192).transpose(1, 0, 2)).reshape(128, 16 * 192)
    shared["nsink"] = np.ascontiguousarray(np.broadcast_to(-f(inp["attn_sink"])[0][None, :], (128, 24)))
    wr = f(inp["router_w"])
    shared["wr"] = np.ascontiguousarray(wr.reshape(2, 16, 128, 16).transpose(0, 2, 1, 3)).reshape(2, 128, 256)
    wg = inp["exp_w_gate"]
    wu = inp["exp_w_up"]
    wd = inp["exp_w_down"]
    if slim:
        wg, wu, wd = wg[:slim[0], :slim[1]], wu[:slim[0], :slim[1]], wd[:slim[0], :slim[1]]
    wg, wu, wd = f(wg), f(wu), f(wd)
    nl_, ne_ = wg.shape[0], wg.shape[1]
    shared["wg"] = np.ascontiguousarray(wg.reshape(nl_, ne_, 16, 128, 2, 512).transpose(0, 1, 4, 3, 2, 5)).reshape(nl_, ne_, 2, 128, 8192)
    shared["wu"] = np.ascontiguousarray(wu.reshape(nl_, ne_, 16, 128, 2, 512).transpose(0, 1, 4, 3, 2, 5)).reshape(nl_, ne_, 2, 128, 8192)
    shared["wd"] = np.ascontiguousarray(wd.reshape(nl_, ne_, 8, 128, 4, 512).transpose(0, 1, 4, 3, 2, 5)).reshape(nl_, ne_, 4, 128, 4096)
    identb, identf, rot, invf, mask, pedge = _consts()
    shared["c_identb"] = identb
    shared["c_identf"] = identf
    shared["c_rot"] = rot
    shared["c_invf"] = invf
    shared["c_mask"] = mask
    shared["c_pedge"] = pedge
    return shared


_NC_CACHE = {}


def kernel(x, mem, positions, norm_mix_g, norm_ffn_g, mem_norm_g, final_g, mem_w_kv,
           pool_w_in, pool_group_w, pool_scale, pool_w_out,
           attn_w_in, attn_sink, attn_w_out,
           router_w, exp_w_gate, exp_w_up, exp_w_down):
    inp = dict(norm_mix_g=norm_mix_g, norm_ffn_g=norm_ffn_g, mem_norm_g=mem_norm_g, final_g=final_g,
               mem_w_kv=mem_w_kv, pool_w_in=pool_w_in, pool_group_w=pool_group_w, pool_scale=pool_scale,
               pool_w_out=pool_w_out, attn_w_in=attn_w_in, attn_sink=attn_sink, attn_w_out=attn_w_out,
               router_w=router_w, exp_w_gate=exp_w_gate, exp_w_up=exp_w_up, exp_w_down=exp_w_down)
    shared = _host_layout(inp)
    x = np.asarray(x, dtype=np.float32)
    mem = np.asarray(mem, dtype=np.float32)
    positions = np.asarray(positions, dtype=np.int32)
    if "nc" not in _NC_CACHE:
        _NC_CACHE["nc"] = build()
    nc = _NC_CACHE["nc"]
    n = 8
    in_maps = []
    for b in range(n):
        m = dict(shared)
        m["x"] = np.ascontiguousarray(x[b])
        m["mem"] = np.ascontiguousarray(mem[b])
        m["pos"] = np.ascontiguousarray(positions[b].reshape(1, S_LEN))
        in_maps.append(m)
    res = run_bass_kernel_spmd(nc, in_maps, core_ids=list(range(n)))
    return np.stack([r["y"] for r in res.results], axis=0)
```
